# Optimizing a Trainium2 kernel written in Bass

```python
import jax
import jax.numpy as jnp
from jax import lax
import numpy as np

D_MODEL = 2048
BATCH = 4
SEQ = 2048
DEPTH = 1

GRID_W = 64
CTX_LEN = 256

RET_HEADS = 8
RET_DK = 128
RET_DV = 128
RET_CHUNK = 128
RET_W = RET_HEADS * RET_DV

MLA_HEADS = 8
MLA_Q_LORA = 512
MLA_KV_LORA = 256
MLA_NOPE = 128
MLA_ROPE = 64
MLA_DV = 128
MLA_W = MLA_HEADS * MLA_DV
Q_BLOCK = 128

MIX_W = RET_W + MLA_W
IN_SIZES = (RET_HEADS * RET_DK, RET_HEADS * RET_DK, RET_W, RET_W, MLA_Q_LORA, MLA_KV_LORA, MLA_ROPE)
IN_W = sum(IN_SIZES)

N_GROUPS = 4
EXPERTS_PER_GROUP = 8
TOP_K = 2
D_EXPERT = 512

ROPE_BASE = 10000.0
EPS = 1e-6
DEEPNORM_ALPHA = (2.0 * DEPTH) ** 0.25
DEEPNORM_BETA = (8.0 * DEPTH) ** -0.25
F32 = jnp.float32

kernel_name = 'hybrid_retention_mla_hmoe_diffusion_block'


def plain_norm(x):
    xf = x.astype(F32)
    mu = jnp.mean(xf, -1, keepdims=True)
    var = jnp.mean(jnp.square(xf - mu), -1, keepdims=True)
    return (xf - mu) * lax.rsqrt(var + EPS)


def layer_norm(x, w, b):
    return (plain_norm(x) * w.astype(F32) + b.astype(F32)).astype(x.dtype)


def rms_norm(x, w):
    xf = x.astype(F32)
    y = xf * lax.rsqrt(jnp.mean(jnp.square(xf), -1, keepdims=True) + EPS)
    return (y * w.astype(F32)).astype(x.dtype)


def modulate(x, shift, scale):
    y = plain_norm(x) * (1.0 + scale[:, None].astype(F32)) + shift[:, None].astype(F32)
    return y.astype(x.dtype)


def to_heads(t, n_heads):
    b, l, w = t.shape
    return t.reshape(b, l, n_heads, w // n_heads).transpose(0, 2, 1, 3)


def from_heads(t):
    b, h, l, d = t.shape
    return t.transpose(0, 2, 1, 3).reshape(b, l, h * d)


def split_projection(p):
    offs = np.cumsum(np.array(IN_SIZES))[:-1].tolist()
    return jnp.split(p, offs, axis=-1)


def axial_rope(x, rows, cols):
    half = x.shape[-1] // 2
    inv_freq = ROPE_BASE ** (-jnp.arange(0, half, 2, dtype=F32) / half)

    def rotate(xa, pos):
        ang = pos.astype(F32)[:, None] * inv_freq[None, :]
        cos, sin = jnp.cos(ang), jnp.sin(ang)
        x1, x2 = jnp.split(xa.astype(F32), 2, axis=-1)
        return jnp.concatenate([x1 * cos - x2 * sin, x1 * sin + x2 * cos], -1)

    return jnp.concatenate([rotate(x[..., :half], rows), rotate(x[..., half:], cols)], -1).astype(x.dtype)


def retention_chunkwise(q, k, v, log_gamma, state0):
    b, h, l, dk = q.shape
    dv = v.shape[-1]
    n = l // RET_CHUNK
    qc = q.reshape(b, h, n, RET_CHUNK, dk).astype(F32)
    kc = k.reshape(b, h, n, RET_CHUNK, dk).astype(F32)
    vc = v.reshape(b, h, n, RET_CHUNK, dv).astype(F32)
    idx = jnp.arange(RET_CHUNK, dtype=F32)
    lg = log_gamma[:, None]
    diff = idx[:, None] - idx[None, :]
    decay_in = jnp.where(diff >= 0, jnp.exp(log_gamma[:, None, None] * jnp.maximum(diff, 0.0)), 0.0)
    zeta = jnp.exp(lg * (RET_CHUNK - 1.0 - idx))
    xi = jnp.exp(lg * (idx + 1.0))
    chunk_decay = jnp.exp(log_gamma * RET_CHUNK)[None, :, None, None]
    scores = jnp.einsum('bhncd,bhnsd->bhncs', qc, kc) * decay_in[None, :, None]
    o_inner = jnp.einsum('bhncs,bhnse->bhnce', scores, vc)
    chunk_kv = jnp.einsum('bhnsd,bhnse->nbhde', kc * zeta[None, :, None, :, None], vc)

    def step(s, u):
        return chunk_decay * s + u, s

    _, s_prev = lax.scan(step, state0.astype(F32), chunk_kv)
    o_cross = jnp.einsum('bhncd,nbhde->bhnce', qc, s_prev) * xi[None, :, None, :, None]
    return (o_inner + o_cross).reshape(b, h, l, dv)


def retention_final_state(k, v, log_gamma):
    l = k.shape[2]
    pw = jnp.exp(log_gamma[:, None] * (l - 1.0 - jnp.arange(l, dtype=F32)))
    return jnp.einsum('bhld,bhle->bhde', k.astype(F32) * pw[None, :, :, None], v.astype(F32))


def context_retention_states(k, v, log_gamma):
    return (retention_final_state(k, v, log_gamma[0]),
            retention_final_state(jnp.flip(k, 2), jnp.flip(v, 2), log_gamma[1]))


def bidirectional_retention(q, k, v, log_gamma, s_fwd, s_bwd):
    o_fwd = retention_chunkwise(q, k, v, log_gamma[0], s_fwd)
    o_bwd = retention_chunkwise(jnp.flip(q, 2), jnp.flip(k, 2), jnp.flip(v, 2), log_gamma[1], s_bwd)
    return o_fwd + jnp.flip(o_bwd, 2)


def retention_output(o, g, gn_w):
    y = from_heads(plain_norm(o)) * gn_w.astype(F32)
    return (jax.nn.silu(g.astype(F32)) * y).astype(g.dtype)


def mla_queries(c_q, q_norm_w, w_uq):
    return to_heads(rms_norm(c_q, q_norm_w) @ w_uq, MLA_HEADS)


def mla_keys_values(c_kv, k_pe, kv_norm_w, w_ukv):
    kv = to_heads(rms_norm(c_kv, kv_norm_w) @ w_ukv, MLA_HEADS)
    k_nope, v = kv[..., :MLA_NOPE], kv[..., MLA_NOPE:]
    b, h, l, _ = k_nope.shape
    k_pe_h = jnp.broadcast_to(k_pe[:, None], (b, h, l, MLA_ROPE)).astype(k_nope.dtype)
    return jnp.concatenate([k_nope, k_pe_h], -1), v


def block_attention(q, k, v):
    b, h, lq, dq = q.shape
    nb = lq // Q_BLOCK
    qb = jnp.moveaxis(q.reshape(b, h, nb, Q_BLOCK, dq), 2, 0)
    scale = dq ** -0.5

    def one_block(qblk):
        s = jnp.einsum('bhqd,bhkd->bhqk', qblk, k).astype(F32) * scale
        p = jax.nn.softmax(s, axis=-1)
        return jnp.einsum('bhqk,bhkd->bhqd', p.astype(v.dtype), v)

    o = lax.map(one_block, qb)
    return jnp.moveaxis(o, 0, 2).reshape(b, h, lq, v.shape[-1])


def hier_moe(x, w_group, b_group, w_route, b_route, w_gate, w_up, w_down):
    t = x.reshape(-1, x.shape[-1])
    g_logits = (t @ w_group).astype(F32) + b_group.astype(F32)
    g_onehot = jax.nn.one_hot(jnp.argmax(g_logits, -1), N_GROUPS, dtype=F32)
    g_prob = jnp.sum(jax.nn.softmax(g_logits, -1) * g_onehot, -1, keepdims=True)
    e_logits = jnp.einsum('td,gde->tge', t, w_route).astype(F32) + b_route.astype(F32)
    e_logits = jnp.einsum('tge,tg->te', e_logits, g_onehot)
    top_v, top_i = lax.top_k(e_logits, TOP_K)
    top_w = jax.nn.softmax(top_v, -1) * g_prob
    e_w = jnp.sum(jax.nn.one_hot(top_i, EXPERTS_PER_GROUP, dtype=F32) * top_w[..., None], 1)
    gate = g_onehot[:, :, None] * e_w[:, None, :]
    y = jnp.zeros(t.shape, F32)
    for gi in range(N_GROUPS):
        hid = jax.nn.silu(jnp.einsum('td,edf->tef', t, w_gate[gi]).astype(F32)) * jnp.einsum('td,edf->tef', t, w_up[gi]).astype(F32)
        y = y + jnp.einsum('tef,efd->td', hid * gate[:, gi, :, None], w_down[gi].astype(F32))
    return y.reshape(x.shape).astype(x.dtype)


def layer_forward(x, ctx, c, c_ctx, rows, cols, w_ada, b_ada, w_in, ret_decay, ret_gn_w,
                  mla_q_norm, mla_kv_norm, w_uq, w_ukv, w_o, ln1_w, ln1_b,
                  router_group_w, router_group_b, router_expert_w, router_expert_b,
                  expert_w_gate, expert_w_up, expert_w_down, ln2_w, ln2_b, update_ctx):
    def moe(t):
        return hier_moe(t, router_group_w, router_group_b, router_expert_w, router_expert_b,
                        expert_w_gate, expert_w_up, expert_w_down)

    mod_x = jnp.split(jax.nn.silu(c) @ w_ada + b_ada, 6, axis=-1)
    mod_c = jnp.split(jax.nn.silu(c_ctx)[None] @ w_ada + b_ada, 6, axis=-1)
    log_gamma = jax.nn.log_sigmoid(ret_decay.astype(F32))
    k_scale = RET_DK ** -0.5

    qr, kr, vr, gr, cq, ckv, kpe = split_projection(modulate(x, mod_x[0], mod_x[1]) @ w_in)
    qr_c, kr_c, vr_c, gr_c, cq_c, ckv_c, kpe_c = split_projection(modulate(ctx, mod_c[0], mod_c[1]) @ w_in)

    kh_c = to_heads(kr_c, RET_HEADS) * k_scale
    vh_c = to_heads(vr_c, RET_HEADS)
    s_fwd, s_bwd = context_retention_states(kh_c, vh_c, log_gamma)
    k_ctx, v_ctx = mla_keys_values(ckv_c, kpe_c, mla_kv_norm, w_ukv)

    qh = axial_rope(to_heads(qr, RET_HEADS), rows, cols)
    kh = axial_rope(to_heads(kr, RET_HEADS), rows, cols) * k_scale
    o_ret = bidirectional_retention(qh, kh, to_heads(vr, RET_HEADS), log_gamma, s_fwd, s_bwd)
    ret = retention_output(o_ret, gr, ret_gn_w)

    q = mla_queries(cq, mla_q_norm, w_uq)
    q = jnp.concatenate([q[..., :MLA_NOPE], axial_rope(q[..., MLA_NOPE:], rows, cols)], -1)
    k_lat, v_lat = mla_keys_values(ckv, axial_rope(kpe, rows, cols), mla_kv_norm, w_ukv)
    att = block_attention(q, jnp.concatenate([k_ctx, k_lat], 2), jnp.concatenate([v_ctx, v_lat], 2))

    mix = jnp.concatenate([ret, from_heads(att)], -1) @ w_o
    h = layer_norm(DEEPNORM_ALPHA * x + mod_x[2][:, None] * mix, ln1_w, ln1_b)
    y = moe(modulate(h, mod_x[3], mod_x[4]))
    x_out = layer_norm(DEEPNORM_ALPHA * h + mod_x[5][:, None] * y, ln2_w, ln2_b)

    if not update_ctx:
        return x_out, ctx

    z = jnp.zeros((ctx.shape[0], RET_HEADS, RET_DK, RET_DV), F32)
    o_ret_c = bidirectional_retention(to_heads(qr_c, RET_HEADS), kh_c, vh_c, log_gamma, z, z)
    ret_c = retention_output(o_ret_c, gr_c, ret_gn_w)
    att_c = block_attention(mla_queries(cq_c, mla_q_norm, w_uq), k_ctx, v_ctx)
    mix_c = jnp.concatenate([ret_c, from_heads(att_c)], -1) @ w_o
    hc = layer_norm(DEEPNORM_ALPHA * ctx + mod_c[2][:, None] * mix_c, ln1_w, ln1_b)
    yc = moe(modulate(hc, mod_c[3], mod_c[4]))
    ctx_out = layer_norm(DEEPNORM_ALPHA * hc + mod_c[5][:, None] * yc, ln2_w, ln2_b)
    return x_out, ctx_out


def setup_inputs(seed: int = 0) -> dict:
    key = jax.random.key(seed)
    ks = jax.random.split(key, 25)

    def nrm(k, shape, s):
        return jax.random.normal(k, shape, F32) * s

    L = DEPTH
    E = EXPERTS_PER_GROUP
    base_decay = jnp.log(2.0 ** (5.0 + jnp.arange(RET_HEADS, dtype=F32)) - 1.0)
    return {
        'x': nrm(ks[0], (BATCH, SEQ, D_MODEL), 1.0),
        'c': nrm(ks[1], (BATCH, D_MODEL), 1.0),
        'ctx': nrm(ks[2], (BATCH, CTX_LEN, D_MODEL), 1.0),
        'c_ctx': nrm(ks[3], (D_MODEL,), 1.0),
        'w_ada': nrm(ks[4], (L, D_MODEL, 6 * D_MODEL), 0.5 * D_MODEL ** -0.5),
        'b_ada': nrm(ks[5], (L, 6 * D_MODEL), 0.02),
        'w_in': nrm(ks[6], (L, D_MODEL, IN_W), D_MODEL ** -0.5),
        'ret_decay': base_decay + nrm(ks[7], (L, 2, RET_HEADS), 0.1),
        'ret_gn_w': 1.0 + nrm(ks[8], (L, RET_W), 0.02),
        'mla_q_norm': 1.0 + nrm(ks[9], (L, MLA_Q_LORA), 0.02),
        'mla_kv_norm': 1.0 + nrm(ks[10], (L, MLA_KV_LORA), 0.02),
        'w_uq': nrm(ks[11], (L, MLA_Q_LORA, MLA_HEADS * (MLA_NOPE + MLA_ROPE)), MLA_Q_LORA ** -0.5),
        'w_ukv': nrm(ks[12], (L, MLA_KV_LORA, MLA_HEADS * (MLA_NOPE + MLA_DV)), MLA_KV_LORA ** -0.5),
        'w_o': nrm(ks[13], (L, MIX_W, D_MODEL), DEEPNORM_BETA * MIX_W ** -0.5),
        'ln1_w': 1.0 + nrm(ks[14], (L, D_MODEL), 0.02),
        'ln1_b': nrm(ks[15], (L, D_MODEL), 0.02),
        'router_group_w': nrm(ks[16], (L, D_MODEL, N_GROUPS), D_MODEL ** -0.5),
        'router_group_b': nrm(ks[17], (L, N_GROUPS), 0.01),
        'router_expert_w': nrm(ks[18], (L, N_GROUPS, D_MODEL, E), D_MODEL ** -0.5),
        'router_expert_b': nrm(ks[19], (L, N_GROUPS, E), 0.01),
        'expert_w_gate': nrm(ks[20], (L, N_GROUPS, E, D_MODEL, D_EXPERT), D_MODEL ** -0.5),
        'expert_w_up': nrm(ks[21], (L, N_GROUPS, E, D_MODEL, D_EXPERT), D_MODEL ** -0.5),
        'expert_w_down': nrm(ks[22], (L, N_GROUPS, E, D_EXPERT, D_MODEL), DEEPNORM_BETA * D_EXPERT ** -0.5),
        'ln2_w': 1.0 + nrm(ks[23], (L, D_MODEL), 0.02),
        'ln2_b': nrm(ks[24], (L, D_MODEL), 0.02),
    }


def reference(x, c, ctx, c_ctx, w_ada, b_ada, w_in, ret_decay, ret_gn_w, mla_q_norm, mla_kv_norm,
              w_uq, w_ukv, w_o, ln1_w, ln1_b, router_group_w, router_group_b, router_expert_w,
              router_expert_b, expert_w_gate, expert_w_up, expert_w_down, ln2_w, ln2_b):
    n_rows = x.shape[1] // GRID_W
    rows = jnp.repeat(jnp.arange(n_rows, dtype=jnp.int32), GRID_W)
    cols = jnp.tile(jnp.arange(GRID_W, dtype=jnp.int32), n_rows)
    for l in range(DEPTH):
        x, ctx = layer_forward(
            x, ctx, c, c_ctx, rows, cols, w_ada[l], b_ada[l], w_in[l], ret_decay[l], ret_gn_w[l],
            mla_q_norm[l], mla_kv_norm[l], w_uq[l], w_ukv[l], w_o[l], ln1_w[l], ln1_b[l],
            router_group_w[l], router_group_b[l], router_expert_w[l], router_expert_b[l],
            expert_w_gate[l], expert_w_up[l], expert_w_down[l], ln2_w[l], ln2_b[l],
            l < DEPTH - 1)
    return x
```

```python
import math
import os
from contextlib import ExitStack

import numpy as np
import concourse.bass as bass
import concourse.mybir as mybir
from concourse.bass_utils import run_bass_kernel_spmd

F32 = mybir.dt.float32
BF16 = mybir.dt.bfloat16
AF = mybir.ActivationFunctionType
ALU = mybir.AluOpType
AX = mybir.AxisListType

D = 2048
NTOK = 1024
NT = 8
GRID_W = 64
CTX = 256
IN_W = 4928
ALPHA = 2.0 ** 0.25
EPS = 1e-6
K_SCALE = 128.0 ** -0.5
ATT_SCALE = 192.0 ** -0.5
COMPUTE = ("pe", "act", "dve", "pool")
NDMA_SEM = 24
ARENA_KB = 200
T_D1, T_MK1, T_D2, T_MK2, T_CP1, T_C128, T_Z1, T_Z2, T_EF, T_MF, T_EB, T_MB = (
    0, 128, 256, 384, 512, 640, 768, 769, 770, 780, 790, 800)
NTAB = 810


class _I:
    __slots__ = ("eng", "fn", "dma", "deps", "signal", "sem", "val", "waits")

    def __init__(self, eng, fn, dma):
        self.eng = eng
        self.fn = fn
        self.dma = dma
        self.deps = ()
        self.signal = False
        self.sem = None
        self.val = 0
        self.waits = ()


class _Rec:
    def __init__(self):
        self.calls = []

    def __getattr__(self, name):
        def f(*args, **kw):
            self.calls.append((name, args, kw))
            return None
        return f


class Prog:
    def __init__(self):
        self.marks = []
        self.ins = []
        self.last_w = {}
        self.readers = {}
        self.seen = set()
        self.tile_pending = {}
        self.tile_acc = {}

    def _touch(self, k, i):
        base = k if isinstance(k, str) else k[0]
        if k not in self.seen:
            self.seen.add(k)
            p = self.tile_pending.get(base)
            if p:
                self.readers[k] = list(p)
        acc = self.tile_acc.setdefault(base, {"c": {}, "d": set()})
        x = self.ins[i] if i < len(self.ins) else None
        return acc

    def add(self, eng, fn, reads=(), writes=(), dma=False):
        if fn is not None:
            rec = _Rec()
            fn(rec)
            assert len(rec.calls) == 1
            fn = rec.calls[0]
        i = len(self.ins)
        ins = _I(eng, fn, dma)
        self.ins.append(ins)
        for k in tuple(reads) + tuple(writes):
            acc = self._touch(k, i)
            if dma:
                acc["d"].add(i)
            else:
                acc["c"][eng] = i
        deps = set()
        for k in reads:
            w = self.last_w.get(k)
            if w is not None:
                deps.add(w)
        for k in writes:
            w = self.last_w.get(k)
            if w is not None:
                deps.add(w)
            deps.update(self.readers.get(k, ()))
        for k in reads:
            self.readers.setdefault(k, []).append(i)
        for k in writes:
            self.last_w[k] = i
            self.readers[k] = []
        deps.discard(i)
        ins.deps = deps
        return i

    def pe(self, fn, r=(), w=()):
        return self.add("pe", fn, r, w)

    def act(self, fn, r=(), w=()):
        return self.add("act", fn, r, w)

    def dve(self, fn, r=(), w=()):
        return self.add("dve", fn, r, w)

    def pool(self, fn, r=(), w=()):
        return self.add("pool", fn, r, w)

    def dma(self, q, out, in_, r=(), w=()):
        return self.add(q, lambda e: e.dma_start(out=out, in_=in_), r, w, dma=True)

    def finalize(self, sems, dma_sems, fence_aps=None):
        ins = self.ins

        def prune(x):
            best = {}
            dmas = []
            for d in x.deps:
                p = ins[d]
                if p.dma:
                    dmas.append(d)
                else:
                    if p.eng == "pe" and x.eng == "pe" and not x.dma:
                        continue
                    if p.eng not in best or best[p.eng] < d:
                        best[p.eng] = d
            return sorted(list(best.values()) + dmas)

        for x in ins:
            x.deps = prune(x)
        if fence_aps and os.environ.get("K_FENCE"):
            F = set()
            for x in ins:
                if x.eng == "pe" and not x.dma:
                    for d in x.deps:
                        p = ins[d]
                        if not p.dma and p.eng in fence_aps:
                            F.add(d)
            new = []
            remap = {}
            fence_of = {}
            for i, x in enumerate(ins):
                remap[i] = len(new)
                new.append(x)
                if i in F:
                    if x.eng == "act":
                        f = _I(x.eng, ("activation", (), dict(out=fence_aps[x.eng], in_=fence_aps[x.eng], func=AF.Copy)), False)
                    else:
                        f = _I(x.eng, ("memset", (fence_aps[x.eng], 0.0), {}), False)
                    f.deps = [i]
                    fence_of[i] = len(new)
                    new.append(f)
            for j, x in enumerate(new):
                is_pe = (x.eng == "pe" and not x.dma)
                nd = []
                for d in x.deps:
                    if is_pe and d in fence_of:
                        nd.append(fence_of[d])
                    else:
                        nd.append(remap[d])
                x.deps = nd
            self.ins = ins = new
        prev = {}
        for i, x in enumerate(ins):
            if x.dma or x.fn is None or x.eng == "pe" or not os.environ.get("K_CHAIN"):
                continue
            p = prev.get(x.eng)
            if p is not None:
                x.deps = sorted(set(x.deps) | {p})
            prev[x.eng] = i
        for x in ins:
            x.deps = prune(x)
            for d in x.deps:
                ins[d].signal = True
        cnt = {e: 0 for e in COMPUTE}
        dcnt = {q: 0 for q in dma_sems}
        for x in ins:
            if x.dma:
                j = dcnt[x.eng]
                dcnt[x.eng] += 1
                pool = dma_sems[x.eng]
                x.sem = pool[j % len(pool)]
                x.val = 16 * (j // len(pool) + 1)
                x.signal = True
            elif x.signal:
                cnt[x.eng] += 1
                x.sem = sems[x.eng]
                x.val = cnt[x.eng]
        waited = {}
        for x in ins:
            ws = []
            if x.dma and x.val > 16:
                ws.append((x.sem, x.val - 16))
            for d in x.deps:
                p = ins[d]
                ws.append((p.sem, p.val))
            out = []
            for sem, val in ws:
                key = (x.eng, id(sem))
                if waited.get(key, 0) >= val:
                    continue
                waited[key] = val
                out.append((sem, val))
            x.waits = out

    def emit(self, eng_name, e):
        for x in self.ins:
            if x.eng != eng_name:
                continue
            for sem, val in x.waits:
                e.wait_ge(sem, val)
            if x.fn is None:
                continue
            name, args, kw = x.fn
            bi = getattr(e, name)(*args, **kw)
            if x.signal:
                bi.then_inc(x.sem, 16 if x.dma else 1)


class Tile:
    def __init__(self, name, ap, start, end):
        self.name = name
        self.ap = ap
        self.start = start
        self.end = end

    def __getitem__(self, key):
        return self.ap[key]


class Arena:
    def __init__(self, prog, a32, a16, nbytes):
        self.P = prog
        self.a32 = a32
        self.a16 = a16
        self.nbytes = nbytes
        self.used = []
        self.freed = []
        self.n = 0
        self.peak = 0

    def alloc(self, name, shape, dt, parts=None):
        esz = 4 if dt == F32 else 2
        free = 1
        for s in shape[1:]:
            free *= s
        nb = (free * esz + 511) // 512 * 512
        self.used.sort()
        pos = 0
        for (s, e, _) in self.used:
            if s - pos >= nb:
                break
            pos = max(pos, e)
        if pos + nb > self.nbytes:
            raise RuntimeError(f"arena full allocating {name} ({nb} B); used={[(u[2], u[1]-u[0]) for u in self.used]}")
        self.n += 1
        uname = f"{name}#{self.n}"
        self.used.append((pos, pos + nb, uname))
        self.peak = max(self.peak, pos + nb)
        pend = {}
        dm = set()
        keep = []
        for (s, e, acc) in self.freed:
            if s < pos + nb and e > pos:
                for k, v in acc["c"].items():
                    if pend.get(k, -1) < v:
                        pend[k] = v
                dm |= acc["d"]
                if s >= pos and e <= pos + nb:
                    continue
            keep.append((s, e, acc))
        self.freed = keep
        self.P.tile_pending[uname] = list(pend.values()) + sorted(dm)
        handle = self.a32 if dt == F32 else self.a16
        rowlen = self.nbytes // esz
        dims = [[rowlen, shape[0]]]
        st = free
        for s in shape[1:]:
            st //= s
            dims.append([st, s])
        ap = bass.AP(handle, pos // esz, dims)
        return Tile(uname, ap, pos, pos + nb)

    def free(self, *tiles):
        for t in tiles:
            self.used = [u for u in self.used if u[2] != t.name]
            acc = self.P.tile_acc.get(t.name, {"c": {}, "d": set()})
            self.freed.append((t.start, t.end, {"c": dict(acc["c"]), "d": set(acc["d"])}))


def bc(ap_tensor, offset, dims):
    return bass.AP(ap_tensor, offset, [list(d) for d in dims])


def build(stop=None, taps=(), n_exp=32):
    nc = bass.Bass("TRN2", target_bir_lowering=False)
    P = Prog()

    def din(name, shape):
        return nc.dram_tensor(name, list(shape), F32, kind="ExternalInput")

    x_own = din("x_own", [NTOK, D])
    x_oth = din("x_oth", [NTOK, D])
    ctx_in = din("ctx", [CTX, D])
    cT = din("cT", [128, 32])
    w_ada = din("w_ada", [D, 6 * D])
    badaT = din("badaT", [128, 96])
    b_ada = din("b_ada", [1, 6 * D])
    w_in = din("w_in", [D, IN_W])
    ret_decay = din("ret_decay", [1, 16])
    ret_gn_w = din("ret_gn_w", [1, 1024])
    qnT = din("qnT", [128, 4])
    kvnT = din("kvnT", [128, 2])
    w_uq = din("w_uq", [512, 1536])
    w_ckp = din("w_ckp", [D, 320])
    w_uqr = din("w_uqr", [512, 512])
    w_ukv = din("w_ukv", [256, 2048])
    w_o = din("w_o", [D, D])
    ln1_w = din("ln1_w", [1, D])
    ln1_b = din("ln1_b", [1, D])
    ln2_w = din("ln2_w", [1, D])
    ln2_b = din("ln2_b", [1, D])
    w_rt = din("w_rt", [D, 36])
    b_rt = din("b_rt", [1, 36])
    if n_exp:
        ew_gate = din("ew_gate", [32, D, 512])
        ew_up = din("ew_up", [32, D, 512])
        ew_down = din("ew_down", [32, 512, D])
    ident_in = din("ident", [128, 128])
    rope_r_own = din("rope_r_own", [128, 2048])
    rope_r_oth = din("rope_r_oth", [128, 2048])
    rope_m_own = din("rope_m_own", [128, 1024])
    rope_m_oth = din("rope_m_oth", [128, 1024])
    tabs = din("tabs", [128, NTAB])
    out = nc.dram_tensor("out", [NTOK, D], F32, kind="ExternalOutput")
    tap_out = {}
    taps = list(taps)
    if any(t[0] == "xmTo" for t in taps):
        taps += [("xmTo%d" % q4, [128, 16, 256]) for q4 in range(4)]
    for (tname, tshape) in taps:
        tap_out[tname] = nc.dram_tensor("tap_" + tname, [tshape[0], int(np.prod(tshape[1:]))], F32, kind="ExternalOutput")

    st = ExitStack()
    with st:
        a32 = st.enter_context(nc.sbuf_tensor("arena", [128, ARENA_KB * 256], F32))
        a16 = a32.bitcast(BF16)
        ps = [st.enter_context(nc.psum_tensor(f"ps{i}", [128, 512], F32)) for i in range(8)]
        psb = [p.bitcast(BF16) for p in ps]
        sems = {e: st.enter_context(nc.semaphore(f"s_{e}")) for e in COMPUTE}
        dsems = {q: [st.enter_context(nc.semaphore(f"d_{q}{i}")) for i in range(NDMA_SEM)] for q in ("sp", "pool")}
        A = Arena(P, a32, a16, ARENA_KB * 1024)
        PSK = [f"ps{i}" for i in range(8)]
        out_keys = []

        def tap(name, src_ap, rkeys, shape):
            if name not in tap_out:
                return
            t = A.alloc("tapst", list(shape), F32)
            idx = tuple([slice(0, shape[0])] + [slice(None)] * (len(shape) - 1))
            P.dve(lambda e: e.tensor_copy(out=t[idx], in_=src_ap), r=rkeys, w=[t.name])
            flat = 1
            for d_ in shape[1:]:
                flat *= d_
            src = bass.AP(t.ap.tensor, t.ap.offset, [[t.ap.ap[0][0], shape[0]], [1, flat]])
            P.dma("sp", tap_out[name].ap(), src, r=[t.name], w=["tap_" + name])
            out_keys.append("tap_" + name)
            A.free(t)

        FEN = A.alloc("fence", [128, 4], F32)
        ident_bf = A.alloc("ident_bf", [128, 128], BF16)
        ident_f = A.alloc("ident_f", [128, 128], F32)
        ones_bf = A.alloc("ones_bf", [128, 128], BF16)
        TB = A.alloc("tabs", [128, NTAB], F32)
        P.dma("pool", ident_bf[:, :], ident_in.ap(), w=[ident_bf.name])
        P.dma("sp", ident_f[:, :], ident_in.ap(), w=[ident_f.name])
        P.dma("sp", TB[:, :], tabs.ap(), w=[TB.name])
        P.dve(lambda e: e.memset(ones_bf[:, :], 1.0), w=[ones_bf.name])
        kvn = A.alloc("kvn", [128, 128], F32)
        qn = A.alloc("qn", [128, 128], F32)
        P.dma("sp", kvn[:, 0:2], kvnT.ap(), w=[kvn.name])
        P.dma("sp", qn[:, 0:4], qnT.ap(), w=[qn.name])

        cT_sb = A.alloc("cT", [128, 32], F32)
        sT = A.alloc("sT", [128, 32], F32)
        sT_bf = A.alloc("sT_bf", [128, 16, 2], BF16)
        s_bc = A.alloc("s_bc", [128, 16, 128], BF16)
        modsT = A.alloc("modsT", [128, 8, 16], F32)
        badaT_sb = A.alloc("badaT", [128, 96], F32)
        P.dma("sp", cT_sb[:, :], cT.ap(), w=[cT_sb.name])
        P.dma("sp", badaT_sb[:, :], badaT.ap(), w=[badaT_sb.name])
        P.act(lambda e: e.activation(out=sT[:, :], in_=cT_sb[:, :], func=AF.Silu), r=[cT_sb.name], w=[sT.name])
        P.dve(lambda e: e.tensor_copy(out=sT_bf[:, :, :], in_=sT[:, :].rearrange("p (k r) -> p k r", r=2)), r=[sT.name], w=[sT_bf.name])
        P.dve(lambda e: e.tensor_copy(out=s_bc[:, :, :], in_=bc(sT_bf.ap.tensor, sT_bf.ap.offset, [[A.nbytes // 2, 128], [2, 16], [0, 128]])),
              r=[sT_bf.name], w=[s_bc.name])

        def load_wada(blk):
            buf = A.alloc("wada", [128, 16, 512], BF16)
            src = bc(w_ada, blk * 512, [[6 * D, 128], [128 * 6 * D, 16], [1, 512]])
            P.dma("pool", buf[:, :, :], src, w=[buf.name])
            return buf

        def mods_fm_multi(specs):
            blocks = [(j_src, q, rows, p1) for (j_src, rows, p1) in specs for q in range(4)]
            nxt = load_wada(blocks[0][0] * 4 + blocks[0][1])
            for bi, (j_src, q, rows, plus_one) in enumerate(blocks):
                buf = nxt
                if bi + 1 < len(blocks):
                    nxt = load_wada(blocks[bi + 1][0] * 4 + blocks[bi + 1][1])
                for cc in range(4):
                    for kc in range(16):
                        P.pe(lambda e: e.matmul(
                            ps[0][:, cc * 2:cc * 2 + 2], lhsT=buf[:, kc, cc * 128:(cc + 1) * 128], rhs=sT_bf[:, kc, :],
                            start=(kc == 0), stop=(kc == 15)), r=[buf.name, sT_bf.name], w=[PSK[0]])
                for (row, jd) in rows:
                    dst = modsT[:, jd, q * 4:(q + 1) * 4]
                    src = bc(ps[0], row, [[512, 128], [2, 4]])
                    bsrc = badaT_sb[:, j_src * 16 + q * 4: j_src * 16 + q * 4 + 4]
                    P.dve(lambda e: e.scalar_tensor_tensor(
                        out=dst, in0=src, scalar=(1.0 if plus_one else 0.0), in1=bsrc, op0=ALU.add, op1=ALU.add),
                        r=[PSK[0], badaT_sb.name], w=[modsT.name])
                A.free(buf)

        mods_fm_multi([(0, [(0, 0), (1, 4)], False), (1, [(0, 1), (1, 5)], True)])

        if "modsT" in tap_out:
            tap("modsT", modsT[:, :, :], [modsT.name], [128, 8, 16])

        if stop == "mods":
            pass
        else:
          class Ref:
              def __init__(self, tensor, off, row):
                  self.t, self.o, self.r = tensor, off, row

              def v(self, off, dims, parts=128):
                  return bass.AP(self.t, self.o + off, [[self.r, parts]] + [list(d) for d in dims])

          def tref(tile):
              return Ref(tile.ap.tensor, tile.ap.offset, tile.ap.ap[0][0])

          def pref(i, bf=False):
              return Ref(psb[i] if bf else ps[i], 0, 1024 if bf else 512)

          def pe_fence(b):
              if os.environ.get("DBG_NOFENCE"):
                  return
              P.pe(lambda e: e.matmul(ps[b][0:1, 510:512], lhsT=ident_bf[:, 0:1], rhs=ident_bf[:, 0:2], start=True, stop=True), r=[ident_bf.name], w=[PSK[b]])

          bank_rr = [0]

          def next_bank(lo=0, hi=8):
              b = lo + (bank_rr[0] % (hi - lo))
              bank_rr[0] += 1
              return b

          TBr = tref(TB)
          statn = [0]
          STT = A.alloc("stats", [128, 16, 8], F32)

          def new_stat():
              s = statn[0] % 16
              statn[0] += 1
              key = (STT.name, s)
              P.dve(lambda e: e.memset(STT[:, s, :], 0.0), w=[key])
              return s, key

          def finish_rstd(s, key, n, c_sum, c_sq, c_mean, c_rstd, c_nmr, width=1):
              inv = 1.0 / n
              sl = lambda c: STT[:, s, c:c + width]
              P.dve(lambda e: e.tensor_scalar(out=sl(c_mean), in0=sl(c_sum), scalar1=inv, scalar2=None, op0=ALU.mult), r=[key], w=[key])
              P.dve(lambda e: e.tensor_tensor(out=sl(c_rstd), in0=sl(c_mean), in1=sl(c_mean), op=ALU.mult), r=[key], w=[key])
              P.dve(lambda e: e.tensor_scalar(out=sl(c_rstd), in0=sl(c_rstd), scalar1=-EPS, scalar2=None, op0=ALU.add), r=[key], w=[key])
              P.dve(lambda e: e.scalar_tensor_tensor(out=sl(c_rstd), in0=sl(c_sq), scalar=inv, in1=sl(c_rstd), op0=ALU.mult, op1=ALU.subtract), r=[key], w=[key])
              P.act(lambda e: e.activation(out=sl(c_rstd), in_=sl(c_rstd), func=AF.Sqrt), r=[key], w=[key])
              P.dve(lambda e: e.reciprocal(out=sl(c_rstd), in_=sl(c_rstd)), r=[key], w=[key])
              if c_nmr is not None:
                  P.dve(lambda e: e.scalar_tensor_tensor(out=sl(c_nmr), in0=sl(c_mean), scalar=-1.0, in1=sl(c_rstd), op0=ALU.mult, op1=ALU.mult), r=[key], w=[key])

          def rms_rstd(s, key, n, c_sq, c_rstd):
              P.dve(lambda e: e.tensor_scalar(out=STT[:, s, c_rstd:c_rstd + 1], in0=STT[:, s, c_sq:c_sq + 1], scalar1=1.0 / n, scalar2=EPS, op0=ALU.mult, op1=ALU.add), r=[key], w=[key])
              P.act(lambda e: e.activation(out=STT[:, s, c_rstd:c_rstd + 1], in_=STT[:, s, c_rstd:c_rstd + 1], func=AF.Sqrt), r=[key], w=[key])
              P.dve(lambda e: e.reciprocal(out=STT[:, s, c_rstd:c_rstd + 1], in_=STT[:, s, c_rstd:c_rstd + 1]), r=[key], w=[key])

          JUNK = A.alloc("junk", [128, 2048], BF16)

          def ln_stats_multi(srcs):
              n = len(srcs)
              LS = A.alloc("lnst", [128, 5, n], F32)
              key = LS.name
              P.dve(lambda e: e.memset(LS[:, :, :], 0.0), w=[key])
              for i, (ap_, keys_) in enumerate(srcs):
                  P.dve(lambda e: e.tensor_reduce(out=LS[:, 0, i:i + 1], in_=ap_, axis=AX.X, op=ALU.add), r=list(keys_) + [key], w=[key])
              for i, (ap_, keys_) in enumerate(srcs):
                  P.act(lambda e: e.activation(out=JUNK[:, :], in_=ap_, func=AF.Square, accum_out=LS[:, 1, i:i + 1]), r=list(keys_) + [key], w=[JUNK.name, key])
              inv = 1.0 / 2048.0
              P.dve(lambda e: e.tensor_scalar(out=LS[:, 2, :], in0=LS[:, 0, :], scalar1=inv, scalar2=None, op0=ALU.mult), r=[key], w=[key])
              P.dve(lambda e: e.tensor_tensor(out=LS[:, 3, :], in0=LS[:, 2, :], in1=LS[:, 2, :], op=ALU.mult), r=[key], w=[key])
              P.dve(lambda e: e.tensor_scalar(out=LS[:, 3, :], in0=LS[:, 3, :], scalar1=-EPS, scalar2=None, op0=ALU.add), r=[key], w=[key])
              P.dve(lambda e: e.scalar_tensor_tensor(out=LS[:, 3, :], in0=LS[:, 1, :], scalar=inv, in1=LS[:, 3, :], op0=ALU.mult, op1=ALU.subtract), r=[key], w=[key])
              P.act(lambda e: e.activation(out=LS[:, 3, :], in_=LS[:, 3, :], func=AF.Sqrt), r=[key], w=[key])
              P.dve(lambda e: e.reciprocal(out=LS[:, 3, :], in_=LS[:, 3, :]), r=[key], w=[key])
              P.dve(lambda e: e.scalar_tensor_tensor(out=LS[:, 4, :], in0=LS[:, 2, :], scalar=-1.0, in1=LS[:, 3, :], op0=ALU.mult, op1=ALU.mult), r=[key], w=[key])
              return LS, [(LS[:, 3, i:i + 1], LS[:, 4, i:i + 1], key) for i in range(n)]

          def ln_apply(st, src_ap, src_keys, dst_ap, dst_keys):
              rstd_ap, nmr_ap, key = st
              P.act(lambda e: e.activation(out=dst_ap, in_=src_ap, func=AF.Identity, scale=rstd_ap, bias=nmr_ap), r=list(src_keys) + [key], w=list(dst_keys))

          def ln_stats(src_ap, src_keys):
              s, key = new_stat()
              P.dve(lambda e: e.tensor_reduce(out=STT[:, s, 0:1], in_=src_ap, axis=AX.X, op=ALU.add), r=list(src_keys) + [key], w=[key])
              P.act(lambda e: e.activation(out=JUNK[:, :], in_=src_ap, func=AF.Square, accum_out=STT[:, s, 1:2]), r=list(src_keys) + [key], w=[JUNK.name, key])
              finish_rstd(s, key, 2048.0, 0, 1, 2, 3, 4)
              return STT[:, s, 3:4], STT[:, s, 4:5], key

          def ln_norm_tile(src_ap, src_keys, dst_ap, dst_keys):
              ln_apply(ln_stats(src_ap, src_keys), src_ap, src_keys, dst_ap, dst_keys)

          def transpose_block(src_tile, ntile, dst_tile, dst_col0, j_scale, j_shift, eng_toggle):
              n = ntile * 128
              for kc in range(16):
                  b = next_bank()
                  for j in range(ntile):
                      P.pe(lambda e, b=b, j=j, kc=kc: e.transpose(psb[b][:, j * 128:(j + 1) * 128], src_tile[:, j, kc * 128:(kc + 1) * 128], ident_bf[:, :]),
                           r=[src_tile.name, ident_bf.name], w=[PSK[b]])
                  dst = dst_tile[:, kc, dst_col0:dst_col0 + n]
                  if (kc + eng_toggle) % 2 == 0:
                      P.act(lambda e, b=b, dst=dst, kc=kc: e.activation(out=dst, in_=psb[b][:, 0:n], func=AF.Identity, scale=modsT[:, j_scale, kc:kc + 1], bias=modsT[:, j_shift, kc:kc + 1]),
                            r=[PSK[b], modsT.name], w=[(dst_tile.name, kc)])
                  else:
                      P.dve(lambda e, b=b, dst=dst, kc=kc: e.tensor_scalar(out=dst, in0=psb[b][:, 0:n], scalar1=modsT[:, j_scale, kc:kc + 1], scalar2=modsT[:, j_shift, kc:kc + 1], op0=ALU.mult, op1=ALU.add),
                            r=[PSK[b], modsT.name], w=[(dst_tile.name, kc)])

          P.marks.append(("A", sum(1 for x_ in P.ins if x_.eng == "pe")))
          xmT_own = A.alloc("xmT_own", [128, 16, 1024], BF16)
          xmT_oth = A.alloc("xmT_oth", [128, 16, 1024], BF16)
          xmT_ctx = A.alloc("xmT_ctx", [128, 16, 256], BF16)
          XK = lambda t: [(t.name, kc) for kc in range(16)]

          def phase_a(src_dram, ntiles_total, dst_tile, j_scale, j_shift):
              loaded = {}

              def load(ti):
                  if ti < ntiles_total and ti not in loaded:
                      xt = A.alloc("xt", [128, 2048], F32)
                      P.dma("sp", xt[:, :], src_dram.ap()[ti * 128:(ti + 1) * 128, :], w=[xt.name])
                      loaded[ti] = xt
              blk = 0
              t0 = 0
              while t0 < ntiles_total:
                  nt = min(4, ntiles_total - t0)
                  for j in range(nt):
                      load(t0 + j)
                  load(t0 + nt)
                  load(t0 + nt + 1)
                  xn = A.alloc("xn", [128, nt, 2048], BF16)
                  xts = [loaded.pop(t0 + j) for j in range(nt)]
                  LSx, sts = ln_stats_multi([(xt[:, :], [xt.name]) for xt in xts])
                  for j in range(nt):
                      ln_apply(sts[j], xts[j][:, :], [xts[j].name], xn[:, j, :], [xn.name])
                  A.free(LSx, *xts)
                  transpose_block(xn, nt, dst_tile, t0 * 128, j_scale, j_shift, blk)
                  A.free(xn)
                  t0 += nt
                  blk += 1

          phase_a(x_oth, 8, xmT_oth, 1, 0)
          phase_a(ctx_in, 2, xmT_ctx, 5, 4)
          phase_a(x_own, 8, xmT_own, 1, 0)
          if "xmTo" in tap_out:
              for q4 in range(4):
                  tap("xmTo%d" % q4, xmT_oth[:, :, q4 * 256:(q4 + 1) * 256], XK(xmT_oth), [128, 16, 256])
          if "xmT" in tap_out:
              tap("xmT", xmT_own[:, :, 0:256], XK(xmT_own), [128, 16, 256])
              tap("xmTc", xmT_ctx[:, :, :], XK(xmT_ctx), [128, 16, 256])

          P.marks.append(("0c", sum(1 for x_ in P.ins if x_.eng == "pe")))
          LG = A.alloc("LG", [128, 16], F32)
          G128 = A.alloc("G128", [128, 16], F32)
          KZ = A.alloc("KZ", [128, 16], F32)
          lgt = A.alloc("lgt", [128, 16], F32)
          P.dma("sp", LG[:, :], bc(ret_decay, 0, [[0, 128], [1, 16]]), w=[LG.name])
          P.act(lambda e: e.activation(out=lgt[:, :], in_=LG[:, :], func=AF.Exp, scale=-1.0), r=[LG.name], w=[lgt.name])
          P.dve(lambda e: e.tensor_scalar(out=LG[:, :], in0=lgt[:, :], scalar1=-1.0 / 7.0, scalar2=1.0 / 6.0, op0=ALU.mult, op1=ALU.add), r=[lgt.name], w=[LG.name])
          for kk in (5, 4, 3, 2, 1):
              P.dve(lambda e: e.tensor_tensor(out=LG[:, :], in0=LG[:, :], in1=lgt[:, :], op=ALU.mult), r=[LG.name, lgt.name], w=[LG.name])
              P.dve(lambda e, kk=kk: e.tensor_scalar(out=LG[:, :], in0=LG[:, :], scalar1=-1.0, scalar2=1.0 / kk, op0=ALU.mult, op1=ALU.add), r=[LG.name], w=[LG.name])
          P.dve(lambda e: e.scalar_tensor_tensor(out=LG[:, :], in0=LG[:, :], scalar=-1.0, in1=lgt[:, :], op0=ALU.mult, op1=ALU.mult), r=[LG.name, lgt.name], w=[LG.name])
          P.act(lambda e: e.activation(out=G128[:, :], in_=LG[:, :], func=AF.Exp, scale=128.0), r=[LG.name], w=[G128.name])
          P.dve(lambda e: e.tensor_scalar(out=lgt[:, 0:8], in0=LG[:, 0:8], scalar1=TB[:, T_Z1:T_Z1 + 1], scalar2=None, op0=ALU.mult), r=[LG.name, TB.name], w=[lgt.name])
          P.dve(lambda e: e.tensor_scalar(out=lgt[:, 8:16], in0=LG[:, 8:16], scalar1=TB[:, T_Z2:T_Z2 + 1], scalar2=None, op0=ALU.mult), r=[LG.name, TB.name], w=[lgt.name])
          P.act(lambda e: e.activation(out=KZ[:, :], in_=lgt[:, :], func=AF.Exp), r=[lgt.name], w=[KZ.name])
          P.dve(lambda e: e.tensor_scalar(out=KZ[:, :], in0=KZ[:, :], scalar1=K_SCALE, scalar2=None, op0=ALU.mult), r=[KZ.name], w=[KZ.name])
          MT = A.alloc("MT", [128, 8, 128], F32)
          XiF = A.alloc("XiF", [128, 8, 128], BF16)
          XiB = A.alloc("XiB", [128, 8, 128], BF16)
          WF = A.alloc("WF", [128, 10, 8], F32)
          WB = A.alloc("WB", [128, 10, 8], F32)
          mtmp = A.alloc("mtmp", [128, 128], F32)
          for h in range(8):
              P.act(lambda e, h=h: e.activation(out=MT[:, h, :], in_=TB[:, T_D1:T_D1 + 128], func=AF.Exp, scale=LG[:, h:h + 1]), r=[LG.name, TB.name], w=[(MT.name, h)])
              P.dve(lambda e, h=h: e.scalar_tensor_tensor(out=MT[:, h, :], in0=MT[:, h, :], scalar=K_SCALE, in1=TB[:, T_MK1:T_MK1 + 128], op0=ALU.mult, op1=ALU.mult), r=[(MT.name, h), TB.name], w=[(MT.name, h)])
              P.act(lambda e, h=h: e.activation(out=mtmp[:, :], in_=TB[:, T_D2:T_D2 + 128], func=AF.Exp, scale=LG[:, 8 + h:9 + h]), r=[LG.name, TB.name], w=[mtmp.name])
              P.dve(lambda e, h=h: e.scalar_tensor_tensor(out=mtmp[:, :], in0=mtmp[:, :], scalar=K_SCALE, in1=TB[:, T_MK2:T_MK2 + 128], op0=ALU.mult, op1=ALU.mult), r=[mtmp.name, TB.name], w=[mtmp.name])
              P.dve(lambda e, h=h: e.tensor_tensor(out=MT[:, h, :], in0=MT[:, h, :], in1=mtmp[:, :], op=ALU.add), r=[(MT.name, h), mtmp.name], w=[(MT.name, h)])
              P.act(lambda e, h=h: e.activation(out=XiF[:, h, :], in_=TB[:, T_CP1:T_CP1 + 128], func=AF.Exp, scale=LG[:, h:h + 1]), r=[LG.name, TB.name], w=[(XiF.name, h)])
              P.act(lambda e, h=h: e.activation(out=XiB[:, h, :], in_=TB[:, T_C128:T_C128 + 128], func=AF.Exp, scale=LG[:, 8 + h:9 + h]), r=[LG.name, TB.name], w=[(XiB.name, h)])
              P.act(lambda e, h=h: e.activation(out=WF[:, :, h], in_=TB[:, T_EF:T_EF + 10], func=AF.Exp, scale=LG[:, h:h + 1]), r=[LG.name, TB.name], w=[WF.name])
              P.act(lambda e, h=h: e.activation(out=WB[:, :, h], in_=TB[:, T_EB:T_EB + 10], func=AF.Exp, scale=LG[:, 8 + h:9 + h]), r=[LG.name, TB.name], w=[WB.name])
          P.dve(lambda e: e.scalar_tensor_tensor(out=WF[:, :, :], in0=WF[:, :, :], scalar=K_SCALE, in1=TBr.v(T_MF, [[1, 10], [0, 8]]), op0=ALU.mult, op1=ALU.mult), r=[WF.name, TB.name], w=[WF.name])
          P.dve(lambda e: e.scalar_tensor_tensor(out=WB[:, :, :], in0=WB[:, :, :], scalar=K_SCALE, in1=TBr.v(T_MB, [[1, 10], [0, 8]]), op0=ALU.mult, op1=ALU.mult), r=[WB.name, TB.name], w=[WB.name])
          A.free(mtmp, lgt)
          MTK = [(MT.name, h) for h in range(8)]
          if "decay" in tap_out:
              tap("LG", LG[:, :], [LG.name], [128, 16])
              tap("MT", MT[:, :, :], MTK, [128, 8, 128])
              tap("WF", WF[:, :, :], [WF.name], [128, 10, 8])
              tap("WB", WB[:, :, :], [WB.name], [128, 10, 8])

          def load_win(col0, ncols, name="w"):
              t = A.alloc(name, [128, 16, ncols], BF16)
              P.dma("pool", t[:, :, :], bc(w_in, col0, [[IN_W, 128], [128 * IN_W, 16], [1, ncols]]), w=[t.name])
              return t

          def proj_tile(xT, col0, wt, ncols, bank, wcol0=0, wkeys=None):
              for kc in range(16):
                  P.pe(lambda e, kc=kc: e.matmul(ps[bank][:, 0:ncols], lhsT=xT[:, kc, col0:col0 + 128], rhs=wt[:, kc, wcol0:wcol0 + ncols], start=(kc == 0), stop=(kc == 15)),
                       r=[(xT.name, kc)] + (wkeys or [wt.name]), w=[PSK[bank]])

          def rope(dst_ref, src_ref, nh, W, cos_ap_ref, sin_ap_ref, rkeys, wkeys, tmpname="ropetmp", tab_stride=0):
              q = W // 4
              CH = ["ropechain"] if os.environ.get("DBG_ROPE_CHAIN") else []
              rkeys = list(rkeys)
              t1 = A.alloc(tmpname, [128, nh, W], F32)
              t2 = A.alloc(tmpname, [128, nh, W], F32)
              t1r, t2r = tref(t1), tref(t2)
              hdim = [[W, nh]] if nh > 1 else []
              bdim = [[tab_stride, nh]] if nh > 1 else []
              full = hdim + [[1, W]]
              P.dve(lambda e: e.tensor_tensor(out=t1r.v(0, full), in0=src_ref.v(0, full), in1=cos_ap_ref.v(0, bdim + [[1, W]]), op=ALU.mult),
                    r=rkeys + CH, w=[t1.name] + CH)
              if W == 64:
                  q = W // 2
                  half = hdim + [[1, q]]
                  bhalf = bdim + [[1, q]]
              else:
                  half = hdim + [[2 * q, 2], [1, q]]
                  bhalf = bdim + [[2 * q, 2], [1, q]]
              P.dve(lambda e: e.tensor_tensor(out=t2r.v(0, half), in0=src_ref.v(q, half), in1=sin_ap_ref.v(0, bhalf), op=ALU.mult),
                    r=rkeys + CH, w=[(t2.name, 0)] + CH)
              P.dve(lambda e: e.tensor_tensor(out=t2r.v(q, half), in0=src_ref.v(0, half), in1=sin_ap_ref.v(q, bhalf), op=ALU.mult),
                    r=rkeys + CH, w=[(t2.name, 1)] + CH)
              P.add(os.environ.get("DBG_ROPE_ENG", "dve"), lambda e: e.tensor_tensor(out=dst_ref.v(0, full), in0=t1r.v(0, full), in1=t2r.v(0, full), op=ALU.add),
                    [t1.name, (t2.name, 0), (t2.name, 1)], wkeys)
              A.free(t1, t2)

          RR_OTH = A.alloc("rr_oth", [128, 2, 8, 128], F32)
          P.dma("sp", RR_OTH[:, :, :, :].rearrange("p a b c -> p (a b c)"), rope_r_oth.ap(), w=[RR_OTH.name])
          RRo = tref(RR_OTH)

          P.marks.append(("B", sum(1 for x_ in P.ins if x_.eng == "pe")))
          SF0 = A.alloc("SF0", [128, 8, 128], F32)
          SB0 = A.alloc("SB0", [128, 8, 128], F32)
          for hg in range(2):
              wk = load_win(1024 + hg * 512, 512, "wk")
              wv = load_win(2048 + hg * 512, 512, "wv")
              kf_all = A.alloc("kf_all", [128, 10, 512], BF16)
              kb_all = A.alloc("kb_all", [128, 10, 512], BF16)
              v_all = A.alloc("v_all", [128, 10, 512], BF16)
              WFr, WBr = tref(WF), tref(WB)
              for t in range(10):
                  xT, c0 = (xmT_oth, t * 128) if t < 8 else (xmT_ctx, (t - 8) * 128)
                  b = next_bank()
                  proj_tile(xT, c0, wk, 512, b)
                  if t < 8:
                      kr = A.alloc("kr", [128, 4, 128], F32)
                      rope(tref(kr), pref(b), 4, 128, Ref(RRo.t, RRo.o + t * 128, RRo.r), Ref(RRo.t, RRo.o + 1024 + t * 128, RRo.r), [PSK[b], RR_OTH.name], [kr.name])
                      ksrc, kkeys = tref(kr), [kr.name]
                  else:
                      ksrc, kkeys = pref(b), [PSK[b]]
                  full = [[128, 4], [1, 128]]
                  P.dve(lambda e, ksrc=ksrc, t=t: e.tensor_tensor(out=tref(kf_all).v(t * 512, full), in0=ksrc.v(0, full), in1=WFr.v(t * 8 + hg * 4, [[1, 4], [0, 128]]), op=ALU.mult),
                        r=kkeys + [WF.name], w=[(kf_all.name, t)])
                  P.dve(lambda e, ksrc=ksrc, t=t: e.tensor_tensor(out=tref(kb_all).v(t * 512, full), in0=ksrc.v(0, full), in1=WBr.v(t * 8 + hg * 4, [[1, 4], [0, 128]]), op=ALU.mult),
                        r=kkeys + [WB.name], w=[(kb_all.name, t)])
                  if t < 8:
                      A.free(kr)
                  b2 = next_bank()
                  proj_tile(xT, c0, wv, 512, b2)
                  P.act(lambda e, b2=b2, t=t: e.activation(out=v_all[:, t, :], in_=ps[b2][:, :], func=AF.Copy), r=[PSK[b2]], w=[(v_all.name, t)])
              A.free(wk, wv)
              for (kall, S0) in ((kf_all, SF0), (kb_all, SB0)):
                  b = next_bank()
                  for h in range(4):
                      for t in range(10):
                          P.pe(lambda e, b=b, h=h, t=t, kall=kall: e.matmul(ps[b][:, h * 128:(h + 1) * 128], lhsT=kall[:, t, h * 128:(h + 1) * 128], rhs=v_all[:, t, h * 128:(h + 1) * 128], start=(t == 0), stop=(t == 9)),
                               r=[(kall.name, t), (v_all.name, t)], w=[PSK[b]])
                  P.dve(lambda e, b=b, S0=S0: e.tensor_copy(out=S0[:, hg * 4:(hg + 1) * 4, :], in_=ps[b][:, :].rearrange("p (h e) -> p h e", h=4)), r=[PSK[b]], w=[(S0.name, hg)])
              A.free(kf_all, kb_all, v_all)
          A.free(RR_OTH)
          if "SF0" in tap_out:
              tap("SF0", SF0[:, :, :], [(SF0.name, 0), (SF0.name, 1)], [128, 8, 128])
              tap("SB0", SB0[:, :, :], [(SB0.name, 0), (SB0.name, 1)], [128, 8, 128])

          nb6 = lambda: next_bank(0, 6)
          P.marks.append(("C", sum(1 for x_ in P.ins if x_.eng == "pe")))
          ckvT = A.alloc("ckvT", [128, 2, 2304], BF16)
          kpeT = A.alloc("kpeT", [64, 2304], BF16)
          RM_OWN = A.alloc("rm_own", [128, 2, 8, 64], F32)
          RM_OTH = A.alloc("rm_oth", [128, 2, 8, 64], F32)
          P.dma("sp", RM_OWN[:, :, :, :].rearrange("p a b c -> p (a b c)"), rope_m_own.ap(), w=[RM_OWN.name])
          P.dma("sp", RM_OTH[:, :, :, :].rearrange("p a b c -> p (a b c)"), rope_m_oth.ap(), w=[RM_OTH.name])
          wc = A.alloc("wc", [128, 16, 320], BF16)
          P.dma("pool", wc[:, :, :], bc(w_ckp, 0, [[320, 128], [128 * 320, 16], [1, 320]]), w=[wc.name])
          CK = lambda lo, hi: [(ckvT.name, i) for i in range(lo // 128, (hi + 127) // 128)]
          PK = lambda lo, hi: [(kpeT.name, i) for i in range(lo // 128, (hi + 127) // 128)]
          for (xT, ntiles, key0, rtab) in ((xmT_oth, 8, 1024, RM_OTH), (xmT_ctx, 2, 2048, None), (xmT_own, 8, 0, RM_OWN)):
              t0 = 0
              while t0 < ntiles:
                  nt = min(4, ntiles - t0)
                  n = nt * 128
                  ckvn = A.alloc("ckvn", [128, nt, 256], BF16)
                  kper = A.alloc("kper", [128, nt, 64], BF16)
                  kps = A.alloc("kps", [128, nt, 64], F32)
                  for j in range(nt):
                      t = t0 + j
                      b = nb6()
                      proj_tile(xT, t * 128, wc, 320, b)
                      s, key = new_stat()
                      P.act(lambda e: e.activation(out=JUNK[:, 0:256], in_=ps[b][:, 0:256], func=AF.Square, accum_out=STT[:, s, 0:1]), r=[PSK[b], key], w=[JUNK.name, key])
                      rms_rstd(s, key, 256.0, 0, 1)
                      P.act(lambda e: e.activation(out=ckvn[:, j, :], in_=ps[b][:, 0:256], func=AF.Copy, scale=STT[:, s, 1:2]), r=[PSK[b], key], w=[(ckvn.name, j)])
                      if rtab is not None:
                          P.act(lambda e: e.activation(out=kps[:, j, :], in_=ps[b][:, 256:320], func=AF.Copy), r=[PSK[b]], w=[(kps.name, j)])
                      else:
                          P.act(lambda e: e.activation(out=kper[:, j, :], in_=ps[b][:, 256:320], func=AF.Copy), r=[PSK[b]], w=[(kper.name, j)])
                  if rtab is not None:
                      rt = tref(rtab)
                      rope(tref(kper), tref(kps), nt, 64, Ref(rt.t, rt.o + t0 * 64, rt.r), Ref(rt.t, rt.o + 512 + t0 * 64, rt.r),
                           [(kps.name, j) for j in range(nt)] + [rtab.name], [(kper.name, j) for j in range(nt)], tab_stride=64)
                  A.free(kps)
                  k0 = key0 + t0 * 128
                  for lc in range(2):
                      b = nb6()
                      for j in range(nt):
                          P.pe(lambda e: e.transpose(psb[b][:, j * 128:(j + 1) * 128], ckvn[:, j, lc * 128:(lc + 1) * 128], ident_bf[:, :]), r=[(ckvn.name, j), ident_bf.name], w=[PSK[b]])
                      pe_fence(b)
                      P.dve(lambda e: e.tensor_scalar(out=ckvT[:, lc, k0:k0 + n], in0=psb[b][:, 0:n], scalar1=kvn[:, lc:lc + 1], scalar2=None, op0=ALU.mult), r=[PSK[b], kvn.name], w=CK(k0, k0 + n))
                  b = nb6()
                  if not os.environ.get("DBG_NOKPE") and not os.environ.get("DBG_NOKPET"):
                      for j in range(nt):
                          P.pe(lambda e: e.transpose(psb[b][0:64, j * 128:(j + 1) * 128], kper[:, j, :], ident_bf[:, :]), r=[(kper.name, j), ident_bf.name], w=[PSK[b]])
                      pe_fence(b)
                      P.act(lambda e: e.activation(out=kpeT[0:64, k0:k0 + n], in_=psb[b][0:64, 0:n], func=AF.Copy), r=[PSK[b]], w=PK(k0, k0 + n))
                  A.free(ckvn, kper)
                  t0 += nt
          A.free(wc, xmT_oth, xmT_ctx, RM_OTH)
          if "ckvT" in tap_out:
              tap("ckvT", ckvT[:, :, :], CK(0, 2304), [128, 2, 2304])
              tap("kpeT", kpeT[0:64, :], PK(0, 2304), [64, 2304])

          P.marks.append(("D", sum(1 for x_ in P.ins if x_.eng == "pe")))
          mixT = A.alloc("mixT", [128, 16, 1024], BF16)
          NHP = 0 if stop == "C" else (1 if stop == "D1" else 4)
          GW = A.alloc("GW", [128, 1024], F32)
          P.dma("sp", GW[:, :], bc(ret_gn_w, 0, [[0, 128], [1, 1024]]), w=[GW.name])
          RR_OWN = A.alloc("rr_own", [128, 2, 8, 128], F32)
          P.dma("sp", RR_OWN[:, :, :, :].rearrange("p a b c -> p (a b c)"), rope_r_own.ap(), w=[RR_OWN.name])
          RRw = tref(RR_OWN)
          KZr, G128r, MTr, XiFr, XiBr, GWr = tref(KZ), tref(G128), tref(MT), tref(XiF), tref(XiB), tref(GW)
          hd = [[128, 2], [1, 128]]
          for hp in range(NHP):
              wq = load_win(hp * 256, 256, "wq")
              wk = load_win(1024 + hp * 256, 256, "wk")
              wv = load_win(2048 + hp * 256, 256, "wv")
              wg = load_win(3072 + hp * 256, 256, "wg")
              qT = A.alloc("qT", [128, 2, 1024], BF16)
              kT = A.alloc("kT", [128, 2, 1024], BF16)
              kzf = A.alloc("kzf", [128, 8, 256], BF16)
              kzb = A.alloc("kzb", [128, 8, 256], BF16)
              vt = A.alloc("vt", [128, 8, 256], BF16)
              sg = A.alloc("sg", [128, 8, 256], BF16)
              pend = []

              def flush():
                  for th in pend:
                      th()
                  del pend[:]

              for t in range(8):
                  blk, j = t // 4, t % 4
                  bq = nb6()
                  proj_tile(xmT_own, t * 128, wq, 256, bq)
                  bk = nb6()
                  proj_tile(xmT_own, t * 128, wk, 256, bk)
                  bv = nb6()
                  proj_tile(xmT_own, t * 128, wv, 256, bv)
                  bg = nb6()
                  proj_tile(xmT_own, t * 128, wg, 256, bg)
                  flush()
                  qr = A.alloc("qr", [128, 2, 128], BF16)
                  rope(tref(qr), pref(bq), 2, 128, Ref(RRw.t, RRw.o + t * 128, RRw.r), Ref(RRw.t, RRw.o + 1024 + t * 128, RRw.r), [PSK[bq], RR_OWN.name], [qr.name])
                  kr = A.alloc("kr", [128, 2, 128], F32)
                  rope(tref(kr), pref(bk), 2, 128, Ref(RRw.t, RRw.o + t * 128, RRw.r), Ref(RRw.t, RRw.o + 1024 + t * 128, RRw.r), [PSK[bk], RR_OWN.name], [kr.name])
                  P.act(lambda e: e.activation(out=vt[:, t, :], in_=ps[bv][:, 0:256], func=AF.Copy), r=[PSK[bv]], w=[(vt.name, t)])
                  P.act(lambda e: e.activation(out=sg[:, t, :], in_=ps[bg][:, 0:256], func=AF.Silu), r=[PSK[bg]], w=[(sg.name, t)])
                  P.dve(lambda e: e.tensor_tensor(out=tref(kzf).v(t * 256, hd), in0=kr[:, :, :], in1=KZr.v(2 * hp, [[1, 2], [0, 128]]), op=ALU.mult), r=[kr.name, KZ.name], w=[(kzf.name, t)])
                  P.pool(lambda e: e.tensor_tensor(out=tref(kzb).v(t * 256, hd), in0=kr[:, :, :], in1=KZr.v(8 + 2 * hp, [[1, 2], [0, 128]]), op=ALU.mult), r=[kr.name, KZ.name], w=[(kzb.name, t)])
                  krb = A.alloc("krb", [128, 2, 128], BF16)
                  P.act(lambda e: e.activation(out=krb[:, :, :], in_=kr[:, :, :], func=AF.Copy), r=[kr.name], w=[krb.name])

                  def tr(qr=qr, krb=krb, kr=kr, blk=blk, j=j):
                      for h in range(2):
                          P.pe(lambda e: e.transpose(psb[6][:, h * 512 + j * 128:h * 512 + (j + 1) * 128], qr[:, h, :], ident_bf[:, :]), r=[qr.name, ident_bf.name], w=[PSK[6]])
                      for h in range(2):
                          P.pe(lambda e: e.transpose(psb[7][:, h * 512 + j * 128:h * 512 + (j + 1) * 128], krb[:, h, :], ident_bf[:, :]), r=[krb.name, ident_bf.name], w=[PSK[7]])
                      A.free(qr, kr, krb)
                      if j == 3:
                          P.dve(lambda e: e.tensor_copy(out=qT[:, :, blk * 512:(blk + 1) * 512], in_=psb[6][:, :].rearrange("p (h n) -> p h n", h=2)), r=[PSK[6]], w=[(qT.name, blk)])
                          P.act(lambda e: e.activation(out=kT[:, :, blk * 512:(blk + 1) * 512], in_=psb[7][:, :].rearrange("p (h n) -> p h n", h=2), func=AF.Copy), r=[PSK[7]], w=[(kT.name, blk)])
                  pend.append(tr)
              flush()
              A.free(wq, wk, wv, wg)
              SF = A.alloc("SF", [128, 8, 2, 128], F32)
              SBk = A.alloc("SBk", [128, 8, 2, 128], F32)
              SFb = A.alloc("SFb", [128, 8, 2, 128], BF16)
              SBb = A.alloc("SBb", [128, 8, 2, 128], BF16)
              P.dve(lambda e: e.tensor_copy(out=SF[:, 0, :, :], in_=SF0[:, 2 * hp:2 * hp + 2, :]), r=[(SF0.name, hp // 2)], w=[(SF.name, 0)])
              P.dve(lambda e: e.tensor_copy(out=SBk[:, 7, :, :], in_=SB0[:, 2 * hp:2 * hp + 2, :]), r=[(SB0.name, hp // 2)], w=[(SBk.name, 7)])
              for i in range(7):
                  b = nb6()
                  for h in range(2):
                      P.pe(lambda e: e.matmul(ps[b][:, h * 128:(h + 1) * 128], lhsT=kzf[:, i, h * 128:(h + 1) * 128], rhs=vt[:, i, h * 128:(h + 1) * 128], start=True, stop=True),
                           r=[(kzf.name, i), (vt.name, i)], w=[PSK[b]])
                      P.pe(lambda e: e.matmul(ps[b][:, 256 + h * 128:256 + (h + 1) * 128], lhsT=kzb[:, 7 - i, h * 128:(h + 1) * 128], rhs=vt[:, 7 - i, h * 128:(h + 1) * 128], start=True, stop=True),
                           r=[(kzb.name, 7 - i), (vt.name, 7 - i)], w=[PSK[b]])
                  for h in range(2):
                      P.dve(lambda e: e.scalar_tensor_tensor(out=SF[:, i + 1, h, :], in0=SF[:, i, h, :], scalar=G128[:, 2 * hp + h:2 * hp + h + 1], in1=ps[b][:, h * 128:(h + 1) * 128], op0=ALU.mult, op1=ALU.add),
                            r=[(SF.name, i), G128.name, PSK[b]], w=[(SF.name, i + 1)])
                      P.dve(lambda e: e.scalar_tensor_tensor(out=SBk[:, 6 - i, h, :], in0=SBk[:, 7 - i, h, :], scalar=G128[:, 8 + 2 * hp + h:8 + 2 * hp + h + 1], in1=ps[b][:, 256 + h * 128:256 + (h + 1) * 128], op0=ALU.mult, op1=ALU.add),
                            r=[(SBk.name, 7 - i), G128.name, PSK[b]], w=[(SBk.name, 6 - i)])
              P.act(lambda e: e.activation(out=SFb[:, :, :, :].rearrange("p a b c -> p (a b c)"), in_=SF[:, :, :, :].rearrange("p a b c -> p (a b c)"), func=AF.Copy), r=[(SF.name, i) for i in range(8)], w=[SFb.name])
              P.act(lambda e: e.activation(out=SBb[:, :, :, :].rearrange("p a b c -> p (a b c)"), in_=SBk[:, :, :, :].rearrange("p a b c -> p (a b c)"), func=AF.Copy), r=[(SBk.name, i) for i in range(8)], w=[SBb.name])
              if hp == 0 and "SF" in tap_out:
                  tap("SF", SF[:, :, :, :].rearrange("p a b c -> p (a b c)"), [(SF.name, i) for i in range(8)], [128, 2048])
                  tap("SBk", SBk[:, :, :, :].rearrange("p a b c -> p (a b c)"), [(SBk.name, i) for i in range(8)], [128, 2048])
              A.free(SF, SBk)

              def scores(t):
                  blk = t // 4
                  b = nb6()
                  for h in range(2):
                      P.pe(lambda e: e.matmul(ps[b][:, h * 128:(h + 1) * 128], lhsT=kT[:, h, t * 128:(t + 1) * 128], rhs=qT[:, h, t * 128:(t + 1) * 128], start=True, stop=True),
                           r=[(kT.name, blk), (qT.name, blk)], w=[PSK[b]])
                  PT = A.alloc("PT", [128, 2, 128], BF16)
                  P.dve(lambda e: e.tensor_tensor(out=PT[:, :, :], in0=ps[b][:, 0:256].rearrange("p (h n) -> p h n", h=2), in1=MT[:, 2 * hp:2 * hp + 2, :], op=ALU.mult),
                        r=[PSK[b], (MT.name, 2 * hp), (MT.name, 2 * hp + 1)], w=[PT.name])
                  qf = A.alloc("qf", [128, 2, 128], BF16)
                  qb_ = A.alloc("qb", [128, 2, 128], BF16)
                  P.pool(lambda e: e.tensor_tensor(out=qf[:, :, :], in0=qT[:, :, t * 128:(t + 1) * 128], in1=XiF[:, 2 * hp:2 * hp + 2, :], op=ALU.mult),
                         r=[(qT.name, blk), (XiF.name, 2 * hp), (XiF.name, 2 * hp + 1)], w=[qf.name])
                  P.pool(lambda e: e.tensor_tensor(out=qb_[:, :, :], in0=qT[:, :, t * 128:(t + 1) * 128], in1=XiB[:, 2 * hp:2 * hp + 2, :], op=ALU.mult),
                         r=[(qT.name, blk), (XiB.name, 2 * hp), (XiB.name, 2 * hp + 1)], w=[qb_.name])
                  return PT, qf, qb_

              cur = scores(0)
              for t in range(8):
                  blk, j = t // 4, t % 4
                  nxt = scores(t + 1) if t + 1 < 8 else None
                  PT, qf, qb_ = cur
                  bo = nb6()
                  for h in range(2):
                      oreg = ps[bo][:, h * 128:(h + 1) * 128]
                      P.pe(lambda e: e.matmul(oreg, lhsT=PT[:, h, :], rhs=vt[:, t, h * 128:(h + 1) * 128], start=True, stop=False), r=[PT.name, (vt.name, t)], w=[PSK[bo]])
                      P.pe(lambda e: e.matmul(oreg, lhsT=qf[:, h, :], rhs=SFb[:, t, h, :], start=False, stop=False), r=[qf.name, SFb.name], w=[PSK[bo]])
                      P.pe(lambda e: e.matmul(oreg, lhsT=qb_[:, h, :], rhs=SBb[:, t, h, :], start=False, stop=True), r=[qb_.name, SBb.name], w=[PSK[bo]])
                  flush()
                  s_, key = new_stat()
                  P.dve(lambda e: e.tensor_reduce(out=STT[:, s_, 0:2], in_=ps[bo][:, 0:256].rearrange("p (h n) -> p h n", h=2), axis=AX.X, op=ALU.add), r=[PSK[bo], key], w=[key])
                  for h in range(2):
                      P.act(lambda e: e.activation(out=JUNK[:, 0:128], in_=ps[bo][:, h * 128:(h + 1) * 128], func=AF.Square, accum_out=STT[:, s_, 2 + h:3 + h]), r=[PSK[bo], key], w=[JUNK.name, key])
                  finish_rstd(s_, key, 128.0, 0, 2, 4, 6, None, width=2)
                  y = A.alloc("y", [128, 2, 128], F32)
                  for h in range(2):
                      P.dve(lambda e: e.tensor_scalar(out=y[:, h, :], in0=ps[bo][:, h * 128:(h + 1) * 128], scalar1=STT[:, s_, 4 + h:5 + h], scalar2=STT[:, s_, 6 + h:7 + h], op0=ALU.subtract, op1=ALU.mult),
                            r=[PSK[bo], key], w=[(y.name, h)])
                  P.pool(lambda e: e.tensor_tensor(out=y[:, :, :], in0=y[:, :, :], in1=GWr.v(hp * 256, hd), op=ALU.mult), r=[(y.name, 0), (y.name, 1), GW.name], w=[(y.name, 0), (y.name, 1)])
                  ret = A.alloc("ret", [128, 2, 128], BF16)
                  P.pool(lambda e: e.tensor_tensor(out=ret[:, :, :], in0=y[:, :, :], in1=tref(sg).v(t * 256, hd), op=ALU.mult), r=[(y.name, 0), (y.name, 1), (sg.name, t)], w=[ret.name])

                  def tr2(ret=ret, PT=PT, qf=qf, qb_=qb_, y=y, blk=blk, j=j):
                      for h in range(2):
                          P.pe(lambda e: e.transpose(psb[6][:, h * 512 + j * 128:h * 512 + (j + 1) * 128], ret[:, h, :], ident_bf[:, :]), r=[ret.name, ident_bf.name], w=[PSK[6]])
                      A.free(PT, qf, qb_, y, ret)
                      if j == 3:
                          P.dve(lambda e: e.tensor_copy(out=mixT[:, 2 * hp:2 * hp + 2, blk * 512:(blk + 1) * 512], in_=psb[6][:, :].rearrange("p (h n) -> p h n", h=2)), r=[PSK[6]],
                                w=[(mixT.name, 2 * hp), (mixT.name, 2 * hp + 1)])
                  pend.append(tr2)
                  cur = nxt
              flush()
              A.free(qT, kT, kzf, kzb, vt, sg, SFb, SBb)
          A.free(RR_OWN, GW, SF0, SB0, MT, XiF, XiB, WF, WB)
          if "ret" in tap_out:
              tap("ret", mixT[:, 0:8, 0:256], [(mixT.name, i) for i in range(8)], [128, 8, 256])

          nb4 = lambda: next_bank(0, 4)
          MK = lambda lo, hi: [(mixT.name, i) for i in range(lo, hi)]
          if stop not in ("C", "D1", "D"):
              P.marks.append(("E", sum(1 for x_ in P.ins if x_.eng == "pe")))
              wcq = load_win(4096, 512, "wcq")
              cqT = A.alloc("cqT", [128, 4, 1024], BF16)
              for blk in range(2):
                  cqn = A.alloc("cqn", [128, 4, 512], BF16)
                  for j in range(4):
                      t = blk * 4 + j
                      b = nb6()
                      proj_tile(xmT_own, t * 128, wcq, 512, b)
                      s, key = new_stat()
                      P.act(lambda e: e.activation(out=JUNK[:, 0:512], in_=ps[b][:, :], func=AF.Square, accum_out=STT[:, s, 0:1]), r=[PSK[b], key], w=[JUNK.name, key])
                      rms_rstd(s, key, 512.0, 0, 1)
                      P.act(lambda e: e.activation(out=cqn[:, j, :], in_=ps[b][:, :], func=AF.Copy, scale=STT[:, s, 1:2]), r=[PSK[b], key], w=[(cqn.name, j)])
                  for lc in range(4):
                      b = nb6()
                      for j in range(4):
                          P.pe(lambda e: e.transpose(psb[b][:, j * 128:(j + 1) * 128], cqn[:, j, lc * 128:(lc + 1) * 128], ident_bf[:, :]), r=[(cqn.name, j), ident_bf.name], w=[PSK[b]])
                      P.dve(lambda e: e.tensor_scalar(out=cqT[:, lc, blk * 512:(blk + 1) * 512], in0=psb[b][:, 0:512], scalar1=qn[:, lc:lc + 1], scalar2=None, op0=ALU.mult), r=[PSK[b], qn.name], w=[(cqT.name, blk)])
                  A.free(cqn)
              A.free(wcq, xmT_own)
              CQK = [(cqT.name, 0), (cqT.name, 1)]
              wqr = A.alloc("wqr", [128, 4, 512], BF16)
              P.dma("pool", wqr[:, :, :], bc(w_uqr, 0, [[512, 128], [128 * 512, 4], [1, 512]]), w=[wqr.name])
              qrT = A.alloc("qrT", [64, 8, 1024], BF16)
              RMw = tref(RM_OWN)
              for t in range(8):
                  b = nb6()
                  for lc in range(4):
                      P.pe(lambda e: e.matmul(ps[b][:, :], lhsT=cqT[:, lc, t * 128:(t + 1) * 128], rhs=wqr[:, lc, :], start=(lc == 0), stop=(lc == 3)), r=[(cqT.name, t // 4), wqr.name], w=[PSK[b]])
                  qrr = A.alloc("qrr", [128, 8, 64], BF16)
                  rope(tref(qrr), pref(b), 8, 64, Ref(RMw.t, RMw.o + t * 64, RMw.r), Ref(RMw.t, RMw.o + 512 + t * 64, RMw.r), [PSK[b], RM_OWN.name], [qrr.name])
                  b2 = nb6()
                  for h in range(8):
                      P.pe(lambda e: e.transpose(psb[b2][0:64, h * 128:(h + 1) * 128], qrr[:, h, :], ident_bf[:, :]), r=[qrr.name, ident_bf.name], w=[PSK[b2]])
                  P.act(lambda e: e.activation(out=qrT[0:64, :, t * 128:(t + 1) * 128], in_=psb[b2][0:64, :].rearrange("p (h n) -> p h n", h=8), func=AF.Copy), r=[PSK[b2]], w=[(qrT.name, t // 4)])
                  A.free(qrr)
              A.free(wqr, RM_OWN)
              wqn = A.alloc("wqn", [128, 4, 8, 128], BF16)
              for lc in range(4):
                  P.dma("pool", wqn[:, lc, :, :], bc(w_uq, lc * 128 * 1536, [[1536, 128], [192, 8], [1, 128]]), w=[wqn.name])
              wkv = A.alloc("wkv", [128, 2, 2048], BF16)
              P.dma("pool", wkv[:, :, :], bc(w_ukv, 0, [[2048, 128], [128 * 2048, 2], [1, 2048]]), w=[wkv.name])
              late = []

              def fm_block_thunks(j_src, jd, plus_one):
                  for q in range(4):
                      def th(q=q):
                          buf = load_wada(j_src * 4 + q)
                          def run():
                              bb = next_bank(0, 4)
                              for cc in range(4):
                                  for kc in range(16):
                                      P.pe(lambda e: e.matmul(ps[bb][:, cc * 2:cc * 2 + 2], lhsT=buf[:, kc, cc * 128:(cc + 1) * 128], rhs=sT_bf[:, kc, :], start=(kc == 0), stop=(kc == 15)), r=[buf.name, sT_bf.name], w=[PSK[bb]])
                              dst = modsT[:, jd, q * 4:(q + 1) * 4]
                              src = bc(ps[bb], 0, [[512, 128], [2, 4]])
                              bsrc = badaT_sb[:, j_src * 16 + q * 4: j_src * 16 + q * 4 + 4]
                              P.dve(lambda e: e.scalar_tensor_tensor(out=dst, in0=src, scalar=(1.0 if plus_one else 0.0), in1=bsrc, op0=ALU.add, op1=ALU.add), r=[PSK[bb], badaT_sb.name], w=[modsT.name])
                              A.free(buf)
                          return run
                      late.append(th)

              Gt = {}

              def gate_block_thunks(j_src, name):
                  def alloc_g():
                      G = A.alloc(name, [128, 2048], F32)
                      P.dma("sp", G[:, :], bc(b_ada, j_src * 2048, [[0, 128], [1, 2048]]), w=[G.name])
                      Gt[name] = G
                  for q in range(4):
                      def th(q=q):
                          if name not in Gt:
                              alloc_g()
                          G = Gt[name]
                          buf = load_wada(j_src * 4 + q)
                          def run():
                              bb = next_bank(0, 4)
                              for kc in range(16):
                                  P.pe(lambda e: e.matmul(ps[bb][:, :], lhsT=s_bc[:, kc, :], rhs=buf[:, kc, :], start=(kc == 0), stop=(kc == 15)), r=[s_bc.name, buf.name], w=[PSK[bb]])
                              P.dve(lambda e: e.tensor_tensor(out=G[:, q * 512:(q + 1) * 512], in0=ps[bb][:, :], in1=G[:, q * 512:(q + 1) * 512], op=ALU.add), r=[PSK[bb], G.name], w=[G.name])
                              A.free(buf)
                          return run
                      late.append(th)

              fm_block_thunks(3, 2, False)
              fm_block_thunks(4, 3, True)
              gate_block_thunks(2, "G1")
              gate_block_thunks(5, "G2")
              pending_runs = []

              def late_step(n):
                  runs = list(pending_runs)
                  del pending_runs[:]
                  for _ in range(n):
                      if late:
                          pending_runs.append(late.pop(0)())
                  for r_ in runs:
                      r_()

              PTs = [A.alloc("PTt", [128, 512], BF16) for _ in range(4)]
              ptn = [0]
              kblocks = [(0, 512), (512, 512), (1024, 512), (1536, 512), (2048, 256)]
              hq = 0
              for h in range(8):
                  late_step(1)
                  qnT_h = A.alloc("qnT_h", [128, 1024], BF16)
                  for qb in range(2):
                      b = nb4()
                      for lc in range(4):
                          P.pe(lambda e: e.matmul(ps[b][:, :], lhsT=wqn[:, lc, h, :], rhs=cqT[:, lc, qb * 512:(qb + 1) * 512], start=(lc == 0), stop=(lc == 3)), r=[wqn.name, (cqT.name, qb)], w=[PSK[b]])
                      P.dve(lambda e: e.tensor_copy(out=qnT_h[:, qb * 512:(qb + 1) * 512], in_=ps[b][:, :]), r=[PSK[b]], w=[(qnT_h.name, qb)])
                  knT_h = A.alloc("knT_h", [128, 2304], BF16)
                  for (k0, n) in kblocks:
                      b = nb4()
                      for lc in range(2):
                          P.pe(lambda e: e.matmul(ps[b][:, 0:n], lhsT=wkv[:, lc, h * 256:h * 256 + 128], rhs=ckvT[:, lc, k0:k0 + n], start=(lc == 0), stop=(lc == 1)), r=[wkv.name] + CK(k0, k0 + n), w=[PSK[b]])
                      P.act(lambda e: e.activation(out=knT_h[:, k0:k0 + n], in_=ps[b][:, 0:n], func=AF.Copy), r=[PSK[b]], w=[(knT_h.name, k0 // 512)])
                  v_h = A.alloc("v_h", [128, 18, 128], BF16)
                  for g in range(5):
                      ntl = 4 if g < 4 else 2
                      b = nb4()
                      for j in range(ntl):
                          kt = g * 4 + j
                          for lc in range(2):
                              P.pe(lambda e: e.matmul(ps[b][:, j * 128:(j + 1) * 128], lhsT=ckvT[:, lc, kt * 128:(kt + 1) * 128], rhs=wkv[:, lc, h * 256 + 128:h * 256 + 256], start=(lc == 0), stop=(lc == 1)), r=[wkv.name, (ckvT.name, kt)], w=[PSK[b]])
                      P.dve(lambda e: e.tensor_copy(out=v_h[:, g * 4:g * 4 + ntl, :], in_=ps[b][:, 0:ntl * 128].rearrange("p (j n) -> p j n", j=ntl)), r=[PSK[b]], w=[(v_h.name, g)])
                  for qb in range(2):
                      if qb == 1:
                          late_step(1)
                      bO, bS = (4, 5) if hq % 2 == 0 else (6, 7)
                      hq += 1
                      def qk(kt):
                          b = nb4()
                          P.pe(lambda e: e.matmul(ps[b][:, :], lhsT=knT_h[:, kt * 128:(kt + 1) * 128], rhs=qnT_h[:, qb * 512:(qb + 1) * 512], start=True, stop=False), r=[(knT_h.name, kt // 4), (qnT_h.name, qb)], w=[PSK[b]])
                          P.pe(lambda e: e.matmul(ps[b][:, :], lhsT=kpeT[0:64, kt * 128:(kt + 1) * 128], rhs=qrT[0:64, h, qb * 512:(qb + 1) * 512], start=False, stop=True), r=[(kpeT.name, kt), (qrT.name, qb)], w=[PSK[b]])
                          return b
                      banks = {0: qk(0), 1: qk(1)}
                      for kt in range(18):
                          if kt + 2 < 18:
                              banks[kt + 2] = qk(kt + 2)
                          b = banks.pop(kt)
                          PTt = PTs[ptn[0] % 4]
                          ptn[0] += 1
                          P.act(lambda e: e.activation(out=PTt[:, :], in_=ps[b][:, :], func=AF.Exp, scale=ATT_SCALE), r=[PSK[b]], w=[PTt.name])
                          P.pe(lambda e: e.matmul(ps[bO][:, :], lhsT=v_h[:, kt, :], rhs=PTt[:, :], start=(kt == 0), stop=(kt == 17)), r=[(v_h.name, kt // 4), PTt.name], w=[PSK[bO]])
                          P.pe(lambda e: e.matmul(ps[bS][:, :], lhsT=ones_bf[:, :], rhs=PTt[:, :], start=(kt == 0), stop=(kt == 17)), r=[ones_bf.name, PTt.name], w=[PSK[bS]])
                      rec = A.alloc("rec", [128, 512], F32)
                      P.dve(lambda e: e.reciprocal(out=rec[:, :], in_=ps[bS][:, :]), r=[PSK[bS]], w=[rec.name])
                      P.dve(lambda e: e.tensor_tensor(out=mixT[:, 8 + h, qb * 512:(qb + 1) * 512], in0=ps[bO][:, :], in1=rec[:, :], op=ALU.mult), r=[PSK[bO], rec.name], w=[(mixT.name, 8 + h)])
                      A.free(rec)
                  A.free(qnT_h, knT_h, v_h)
              late_step(0)
              assert not late and not pending_runs
              A.free(wqn, wkv, cqT, qrT, ckvT, kpeT, *PTs)
              G1, G2 = Gt["G1"], Gt["G2"]
              if "att" in tap_out:
                  tap("att", mixT[:, 8:16, 0:256], MK(8, 16), [128, 8, 256])

              P.marks.append(("mods2", sum(1 for x_ in P.ins if x_.eng == "pe")))
              def bcast_row(dram, name):
                  T_ = A.alloc(name, [128, 2048], F32)
                  P.dma("sp", T_[:, :], bc(dram, 0, [[0, 128], [1, 2048]]), w=[T_.name])
                  return T_

              P.marks.append(("F", sum(1 for x_ in P.ins if x_.eng == "pe")))
              acc = A.alloc("acc", [128, 8, 2048], F32)
              wos = [A.alloc("wo", [128, 16, 512], BF16) for _ in range(2)]
              P.dma("pool", wos[0][:, :, :], bc(w_o, 0, [[D, 128], [128 * D, 16], [1, 512]]), w=[wos[0].name])
              def load_xr(i):
                  cb_, t_ = i // 8, i % 8
                  xr_ = A.alloc("xr", [128, 512], F32)
                  P.dma("sp", xr_[:, :], x_own.ap()[t_ * 128:(t_ + 1) * 128, cb_ * 512:(cb_ + 1) * 512], w=[xr_.name])
                  return xr_
              xq = [load_xr(i) for i in range(3)]
              tmp_prev = [None]
              for cb in range(4):
                  wo = wos[cb % 2]
                  if cb < 3:
                      wn = wos[(cb + 1) % 2]
                      P.dma("pool", wn[:, :, :], bc(w_o, (cb + 1) * 512, [[D, 128], [128 * D, 16], [1, 512]]), w=[wn.name])
                  for t in range(8):
                      b = next_bank()
                      for kc in range(16):
                          P.pe(lambda e: e.matmul(ps[b][:, :], lhsT=mixT[:, kc, t * 128:(t + 1) * 128], rhs=wo[:, kc, :], start=(kc == 0), stop=(kc == 15)), r=[(mixT.name, kc), wo.name], w=[PSK[b]])
                      xr = xq.pop(0)
                      nxt_i = cb * 8 + t + 3
                      if nxt_i < 32:
                          xq.append(load_xr(nxt_i))
                      tmp = A.alloc("tmpo", [128, 512], F32)
                      if tmp_prev[0] is not None:
                          A.free(tmp_prev[0])
                      tmp_prev[0] = tmp
                      P.dve(lambda e: e.tensor_tensor(out=tmp[:, :], in0=ps[b][:, :], in1=G1[:, cb * 512:(cb + 1) * 512], op=ALU.mult), r=[PSK[b], G1.name], w=[tmp.name])
                      P.dve(lambda e: e.scalar_tensor_tensor(out=acc[:, t, cb * 512:(cb + 1) * 512], in0=xr[:, :], scalar=ALPHA, in1=tmp[:, :], op0=ALU.mult, op1=ALU.add), r=[xr.name, tmp.name], w=[(acc.name, t)])
                      A.free(xr)
              A.free(mixT, G1, tmp_prev[0], *wos)
              if "pre1" in tap_out:
                  tap("pre1", acc[:, 0, :], [(acc.name, 0)], [128, 2048])
              L1W = bcast_row(ln1_w, "L1W")
              L1B = bcast_row(ln1_b, "L1B")
              h2T = A.alloc("h2T", [128, 16, 1024], BF16)
              hn_old = []
              for blk in range(2):
                  hn2 = A.alloc("hn2", [128, 4, 2048], BF16)
                  for pr in range(2):
                      tl = [blk * 4 + pr * 2, blk * 4 + pr * 2 + 1]
                      hnl = []
                      for t in tl:
                          hn = A.alloc("hn", [128, 2048], F32)
                          hnl.append(hn)
                      for ohn in hn_old:
                          A.free(ohn)
                      hn_old = list(hnl)
                      LSa, st1 = ln_stats_multi([(acc[:, t, :], [(acc.name, t)]) for t in tl])
                      for t, hn, st_ in zip(tl, hnl, st1):
                          ln_apply(st_, acc[:, t, :], [(acc.name, t)], hn[:, :], [hn.name])
                          P.dve(lambda e: e.tensor_tensor(out=hn[:, :], in0=hn[:, :], in1=L1W[:, :], op=ALU.mult), r=[hn.name, L1W.name], w=[hn.name])
                          P.add("pool" if t % 2 == 0 else "dve", lambda e: e.tensor_tensor(out=hn[:, :], in0=hn[:, :], in1=L1B[:, :], op=ALU.add), [hn.name, L1B.name], [hn.name])
                      LSb, st2 = ln_stats_multi([(hn[:, :], [hn.name]) for hn in hnl])
                      for t, hn, st_ in zip(tl, hnl, st2):
                          j = t % 4
                          ln_apply(st_, hn[:, :], [hn.name], hn2[:, j, :], [hn2.name])
                          P.act(lambda e: e.activation(out=acc[:, t, :], in_=hn[:, :], func=AF.Copy, scale=ALPHA), r=[hn.name], w=[(acc.name, t)])
                      A.free(LSa, LSb)
                  transpose_block(hn2, 4, h2T, blk * 512, 3, 2, blk)
                  A.free(hn2)
              A.free(L1W, L1B, *hn_old)
              H2K = [(h2T.name, kc) for kc in range(16)]
              if "h2T" in tap_out:
                  tap("h2T", h2T[:, :, 0:256], H2K, [128, 16, 256])

              P.marks.append(("G", sum(1 for x_ in P.ins if x_.eng == "pe")))
              wr = A.alloc("wr", [128, 16, 36], BF16)
              P.dma("pool", wr[:, :, :], bc(w_rt, 0, [[36, 128], [128 * 36, 16], [1, 36]]), w=[wr.name])
              brt = A.alloc("brt", [128, 36], F32)
              P.dma("sp", brt[:, :], bc(b_rt, 0, [[0, 128], [1, 36]]), w=[brt.name])
              LGT = A.alloc("LGT", [128, 8, 36], F32)
              for t in range(8):
                  b = next_bank()
                  for kc in range(16):
                      P.pe(lambda e: e.matmul(ps[b][:, 0:36], lhsT=h2T[:, kc, t * 128:(t + 1) * 128], rhs=wr[:, kc, :], start=(kc == 0), stop=(kc == 15)), r=[(h2T.name, kc), wr.name], w=[PSK[b]])
                  P.dve(lambda e: e.tensor_tensor(out=LGT[:, t, :], in0=ps[b][:, 0:36], in1=brt[:, :], op=ALU.add), r=[PSK[b], brt.name], w=[LGT.name])
              RT = A.alloc("RT", [128, 1024], F32)
              RTr = tref(RT)
              LGr = tref(LGT)
              def rv(off, dims):
                  return RTr.v(off, dims)
              o_gmax, o_ohg, o_eg, o_gsum, o_gprob, o_tmpe, o_esel, o_m1, o_oh1, o_e2, o_m2, o_oh2, o_dd, o_w1, o_w2, o_t1, o_t2, o_ew = (
                  0, 8, 40, 72, 80, 88, 344, 408, 416, 480, 544, 552, 616, 624, 632, 640, 704, 768)
              GATE = A.alloc("GATE", [128, 8, 32], F32)
              GL = LGr.v(0, [[36, 8], [1, 4]])
              EL = LGr.v(4, [[36, 8], [8, 4], [1, 8]])
              RK = [RT.name]
              def rdve(fn, extra_r=()):
                  P.dve(fn, r=RK + list(extra_r), w=RK)
              rdve(lambda e: e.tensor_reduce(out=rv(o_gmax, [[1, 8]]), in_=GL, axis=AX.X, op=ALU.max), [LGT.name])
              rdve(lambda e: e.tensor_tensor(out=rv(o_ohg, [[4, 8], [1, 4]]), in0=GL, in1=rv(o_gmax, [[1, 8], [0, 4]]), op=ALU.is_equal), [LGT.name])
              rdve(lambda e: e.tensor_tensor(out=rv(o_eg, [[4, 8], [1, 4]]), in0=GL, in1=rv(o_gmax, [[1, 8], [0, 4]]), op=ALU.subtract), [LGT.name])
              P.act(lambda e: e.activation(out=rv(o_eg, [[1, 32]]), in_=rv(o_eg, [[1, 32]]), func=AF.Exp), r=RK, w=RK)
              rdve(lambda e: e.tensor_reduce(out=rv(o_gsum, [[1, 8]]), in_=rv(o_eg, [[4, 8], [1, 4]]), axis=AX.X, op=ALU.add))
              rdve(lambda e: e.reciprocal(out=rv(o_gprob, [[1, 8]]), in_=rv(o_gsum, [[1, 8]])))
              rdve(lambda e: e.tensor_tensor(out=rv(o_tmpe, [[32, 8], [8, 4], [1, 8]]), in0=EL, in1=rv(o_ohg, [[4, 8], [1, 4], [0, 8]]), op=ALU.mult), [LGT.name])
              rdve(lambda e: e.tensor_reduce(out=rv(o_esel, [[8, 8], [1, 8]]), in_=rv(o_tmpe, [[32, 8], [1, 8], [8, 4]]), axis=AX.X, op=ALU.add))
              rdve(lambda e: e.tensor_reduce(out=rv(o_m1, [[1, 8]]), in_=rv(o_esel, [[8, 8], [1, 8]]), axis=AX.X, op=ALU.max))
              rdve(lambda e: e.tensor_tensor(out=rv(o_oh1, [[8, 8], [1, 8]]), in0=rv(o_esel, [[8, 8], [1, 8]]), in1=rv(o_m1, [[1, 8], [0, 8]]), op=ALU.is_equal))
              rdve(lambda e: e.scalar_tensor_tensor(out=rv(o_e2, [[1, 64]]), in0=rv(o_oh1, [[1, 64]]), scalar=-1.0e30, in1=rv(o_esel, [[1, 64]]), op0=ALU.mult, op1=ALU.add))
              rdve(lambda e: e.tensor_reduce(out=rv(o_m2, [[1, 8]]), in_=rv(o_e2, [[8, 8], [1, 8]]), axis=AX.X, op=ALU.max))
              rdve(lambda e: e.tensor_tensor(out=rv(o_oh2, [[8, 8], [1, 8]]), in0=rv(o_e2, [[8, 8], [1, 8]]), in1=rv(o_m2, [[1, 8], [0, 8]]), op=ALU.is_equal))
              rdve(lambda e: e.tensor_tensor(out=rv(o_dd, [[1, 8]]), in0=rv(o_m2, [[1, 8]]), in1=rv(o_m1, [[1, 8]]), op=ALU.subtract))
              P.act(lambda e: e.activation(out=rv(o_dd, [[1, 8]]), in_=rv(o_dd, [[1, 8]]), func=AF.Exp), r=RK, w=RK)
              rdve(lambda e: e.tensor_scalar(out=rv(o_w1, [[1, 8]]), in0=rv(o_dd, [[1, 8]]), scalar1=1.0, scalar2=None, op0=ALU.add))
              rdve(lambda e: e.reciprocal(out=rv(o_w1, [[1, 8]]), in_=rv(o_w1, [[1, 8]])))
              rdve(lambda e: e.tensor_tensor(out=rv(o_w2, [[1, 8]]), in0=rv(o_dd, [[1, 8]]), in1=rv(o_w1, [[1, 8]]), op=ALU.mult))
              rdve(lambda e: e.tensor_tensor(out=rv(o_w1, [[1, 8]]), in0=rv(o_w1, [[1, 8]]), in1=rv(o_gprob, [[1, 8]]), op=ALU.mult))
              rdve(lambda e: e.tensor_tensor(out=rv(o_w2, [[1, 8]]), in0=rv(o_w2, [[1, 8]]), in1=rv(o_gprob, [[1, 8]]), op=ALU.mult))
              rdve(lambda e: e.tensor_tensor(out=rv(o_t1, [[8, 8], [1, 8]]), in0=rv(o_oh1, [[8, 8], [1, 8]]), in1=rv(o_w1, [[1, 8], [0, 8]]), op=ALU.mult))
              rdve(lambda e: e.tensor_tensor(out=rv(o_t2, [[8, 8], [1, 8]]), in0=rv(o_oh2, [[8, 8], [1, 8]]), in1=rv(o_w2, [[1, 8], [0, 8]]), op=ALU.mult))
              rdve(lambda e: e.tensor_tensor(out=rv(o_ew, [[1, 64]]), in0=rv(o_t1, [[1, 64]]), in1=rv(o_t2, [[1, 64]]), op=ALU.add))
              P.dve(lambda e: e.tensor_tensor(out=tref(GATE).v(0, [[32, 8], [8, 4], [1, 8]]), in0=rv(o_ohg, [[4, 8], [1, 4], [0, 8]]), in1=rv(o_ew, [[8, 8], [0, 4], [1, 8]]), op=ALU.mult), r=RK, w=[GATE.name])
              A.free(wr, brt, LGT, RT)
              if "gate" in tap_out:
                  tap("gate", GATE[:, :, :], [GATE.name], [128, 8, 32])

              P.marks.append(("H", sum(1 for x_ in P.ins if x_.eng == "pe")))
              NEXP = int(os.environ.get("DBG_NEXP", "32")) if n_exp else 0
              items = []
              for ge in range(NEXP):
                  for fh in range(2):
                      items.append(("gu", ge, fh))
                  items.append(("wd", ge, 0))
              loaded = {}

              def load_item(i):
                  kind, ge, fh = items[i]
                  if kind == "gu":
                      wg = A.alloc("wg", [128, 16, 256], BF16)
                      wu = A.alloc("wu", [128, 16, 256], BF16)
                      P.dma("pool", wg[:, :, :], bc(ew_gate, ge * D * 512 + fh * 256, [[512, 128], [128 * 512, 16], [1, 256]]), w=[wg.name])
                      P.dma("pool", wu[:, :, :], bc(ew_up, ge * D * 512 + fh * 256, [[512, 128], [128 * 512, 16], [1, 256]]), w=[wu.name])
                      loaded[i] = (wg, wu)
                  else:
                      wd = A.alloc("wd", [128, 4, 2048], BF16)
                      P.dma("pool", wd[:, :, :], bc(ew_down, ge * 512 * D, [[D, 128], [128 * D, 4], [1, 2048]]), w=[wd.name])
                      P.pool(lambda e: e.tensor_tensor(out=wd[:, :, :], in0=wd[:, :, :], in1=tref(G2).v(0, [[0, 4], [1, 2048]]), op=ALU.mult), r=[wd.name, G2.name], w=[wd.name])
                      loaded[i] = (wd,)

              def get_item(i):
                  if i not in loaded:
                      load_item(i)
                  if i + 1 < len(items) and (i + 1) not in loaded:
                      load_item(i + 1)
                  return loaded[i]

              ii = 0
              for ge in range(NEXP):
                  hid = A.alloc("hid", [128, 4, 1024], BF16)
                  for fh in range(2):
                      wg, wu = get_item(ii)
                      for fq in range(2):
                          fc = fh * 2 + fq
                          for tb in range(2):
                              bg = next_bank()
                              for kc in range(16):
                                  P.pe(lambda e: e.matmul(ps[bg][:, :], lhsT=wg[:, kc, fq * 128:(fq + 1) * 128], rhs=h2T[:, kc, tb * 512:(tb + 1) * 512], start=(kc == 0), stop=(kc == 15)), r=[wg.name, (h2T.name, kc)], w=[PSK[bg]])
                              bu = next_bank()
                              for kc in range(16):
                                  P.pe(lambda e: e.matmul(ps[bu][:, :], lhsT=wu[:, kc, fq * 128:(fq + 1) * 128], rhs=h2T[:, kc, tb * 512:(tb + 1) * 512], start=(kc == 0), stop=(kc == 15)), r=[wu.name, (h2T.name, kc)], w=[PSK[bu]])
                              sgt = A.alloc("sgt", [128, 512], F32)
                              P.act(lambda e: e.activation(out=sgt[:, :], in_=ps[bg][:, :], func=AF.Silu), r=[PSK[bg]], w=[sgt.name])
                              P.dve(lambda e: e.tensor_tensor(out=hid[:, fc, tb * 512:(tb + 1) * 512], in0=ps[bu][:, :], in1=sgt[:, :], op=ALU.mult), r=[PSK[bu], sgt.name], w=[(hid.name, fc)])
                              A.free(sgt)
                      A.free(wg, wu)
                      del loaded[ii]
                      ii += 1
                  (wd,) = get_item(ii)
                  for t in range(8):
                      for nbk in range(4):
                          b = next_bank()
                          for fc in range(4):
                              P.pe(lambda e: e.matmul(ps[b][:, :], lhsT=hid[:, fc, t * 128:(t + 1) * 128], rhs=wd[:, fc, nbk * 512:(nbk + 1) * 512], start=(fc == 0), stop=(fc == 3)), r=[(hid.name, fc), wd.name], w=[PSK[b]])
                          P.dve(lambda e: e.scalar_tensor_tensor(out=acc[:, t, nbk * 512:(nbk + 1) * 512], in0=ps[b][:, :], scalar=GATE[:, t, ge:ge + 1], in1=acc[:, t, nbk * 512:(nbk + 1) * 512], op0=ALU.mult, op1=ALU.add),
                                r=[PSK[b], GATE.name, (acc.name, t)], w=[(acc.name, t)])
                  A.free(wd, hid)
                  del loaded[ii]
                  ii += 1
              A.free(h2T, G2)

              P.marks.append(("LN2", sum(1 for x_ in P.ins if x_.eng == "pe")))
              L2W = bcast_row(ln2_w, "L2W")
              L2B = bcast_row(ln2_b, "L2B")
              hns = [A.alloc("hn", [128, 2048], F32) for _ in range(3)]
              LS2, sts = ln_stats_multi([(acc[:, t, :], [(acc.name, t)]) for t in range(8)])
              for t in range(8):
                  hn = hns[t % 3]
                  ln_apply(sts[t], acc[:, t, :], [(acc.name, t)], hn[:, :], [hn.name])
                  P.dve(lambda e: e.tensor_tensor(out=hn[:, :], in0=hn[:, :], in1=L2W[:, :], op=ALU.mult), r=[hn.name, L2W.name], w=[hn.name])
                  P.add("pool" if t % 2 == 0 else "dve", lambda e: e.tensor_tensor(out=hn[:, :], in0=hn[:, :], in1=L2B[:, :], op=ALU.add), [hn.name, L2B.name], [hn.name])
                  P.dma("sp", out.ap()[t * 128:(t + 1) * 128, :], hn[:, :], r=[hn.name], w=[("out", t)])
                  out_keys.append(("out", t))

        P.add("sp", None, reads=out_keys)
        P.finalize(sems, dsems, {"act": FEN[:, 0:1], "dve": FEN[:, 1:2], "pool": FEN[:, 2:3]} if not os.environ.get("DBG_NOFENCE2") else None)
        import collections
        print("MARKS", P.marks, flush=True)
        print("instr counts", collections.Counter(x.eng for x in P.ins), "signals", collections.Counter(x.eng for x in P.ins if x.signal and not x.dma), "arena peak", A.peak, flush=True)
        with nc.Block() as block0:
            @block0.sync
            def _(e):
                for s_ in list(sems.values()) + [x_ for l_ in dsems.values() for x_ in l_]:
                    e.sem_clear(s_)

        with nc.Block() as block:
            @block.tensor
            def _(e):
                P.emit("pe", e)

            @block.scalar
            def _(e):
                P.emit("act", e)

            @block.vector
            def _(e):
                P.emit("dve", e)

            @block.gpsimd
            def _(e):
                P.emit("pool", e)

            @block.sync
            def _(e):
                P.emit("sp", e)
    return nc


def _rope_tables(pos, dim):
    half = dim // 2
    inv = 10000.0 ** (-np.arange(0, half, 2, dtype=np.float32) / np.float32(half))
    inv = inv.astype(np.float32)
    rows = (pos // GRID_W).astype(np.float32)
    cols = (pos % GRID_W).astype(np.float32)
    ar = (rows[:, None] * inv[None, :]).astype(np.float32)
    ac = (cols[:, None] * inv[None, :]).astype(np.float32)
    cr, sr, cc, sc = np.cos(ar), np.sin(ar), np.cos(ac), np.sin(ac)
    cosx = np.concatenate([cr, cr, cc, cc], 1).astype(np.float32)
    sinx = np.concatenate([-sr, sr, -sc, sc], 1).astype(np.float32)
    return cosx, sinx


_PERM64 = np.concatenate([np.arange(0, 16), np.arange(32, 48), np.arange(16, 32), np.arange(48, 64)])


def _tile_major(a):
    n = a.shape[0] // 128
    return np.ascontiguousarray(a.reshape(n, 128, a.shape[1]).transpose(1, 0, 2).reshape(128, -1))


def _core_tables(hf):
    own = np.arange(NTOK) + hf * NTOK
    oth = np.arange(NTOK) + (1 - hf) * NTOK
    t = {}
    for nm, pos in (("own", own), ("oth", oth)):
        c, s = _rope_tables(pos, 128)
        t["rope_r_" + nm] = np.concatenate([_tile_major(c), _tile_major(s)], 1)
        c, s = _rope_tables(pos, 64)
        c, s = c[:, _PERM64], s[:, _PERM64]
        t["rope_m_" + nm] = np.concatenate([_tile_major(c), _tile_major(s)], 1)
    tb = np.zeros((128, NTAB), np.float32)
    sidx = np.arange(128, dtype=np.float32)[:, None]
    cidx = np.arange(128, dtype=np.float32)[None, :]
    tb[:, T_D1:T_D1 + 128] = np.maximum(cidx - sidx, 0)
    tb[:, T_MK1:T_MK1 + 128] = (cidx >= sidx)
    tb[:, T_D2:T_D2 + 128] = np.maximum(sidx - cidx, 0)
    tb[:, T_MK2:T_MK2 + 128] = (sidx >= cidx)
    tb[:, T_CP1:T_CP1 + 128] = cidx + 1
    tb[:, T_C128:T_C128 + 128] = 128 - cidx
    tb[:, T_Z1] = 127 - sidx[:, 0]
    tb[:, T_Z2] = sidx[:, 0]
    j = np.arange(NTOK, dtype=np.float32)
    m = np.arange(CTX, dtype=np.float32)
    if hf == 0:
        ef_o, mf_o = np.zeros(NTOK), np.zeros(NTOK)
        ef_c, mf_c = CTX - 1 - m, np.ones(CTX)
        eb_o, mb_o = j, np.ones(NTOK)
        eb_c, mb_c = NTOK + m, np.ones(CTX)
    else:
        ef_o, mf_o = NTOK - 1 - j, np.ones(NTOK)
        ef_c, mf_c = NTOK + CTX - 1 - m, np.ones(CTX)
        eb_o, mb_o = np.zeros(NTOK), np.zeros(NTOK)
        eb_c, mb_c = m, np.ones(CTX)
    def tm(o, c):
        return np.concatenate([o.reshape(8, 128).T, c.reshape(2, 128).T], 1)
    tb[:, T_EF:T_EF + 10] = tm(ef_o, ef_c)
    tb[:, T_MF:T_MF + 10] = tm(mf_o, mf_c)
    tb[:, T_EB:T_EB + 10] = tm(eb_o, eb_c)
    tb[:, T_MB:T_MB + 10] = tm(mb_o, mb_c)
    t["tabs"] = tb
    return t


def _colT(v, n):
    return np.ascontiguousarray(np.asarray(v, np.float32).reshape(n, 128).T)


def prep(inputs, cores=None, n_exp=32):
    f = lambda k: np.asarray(inputs[k], np.float32)
    x, c, ctx, c_ctx = f("x"), f("c"), f("ctx"), f("c_ctx")
    shared = {
        "w_ada": np.ascontiguousarray(f("w_ada")[0]),
        "b_ada": np.ascontiguousarray(f("b_ada")[0][None]),
        "badaT": _colT(f("b_ada")[0], 96),
        "w_in": np.ascontiguousarray(f("w_in")[0]),
        "ret_decay": np.ascontiguousarray(f("ret_decay")[0].reshape(1, 16)),
        "ret_gn_w": np.ascontiguousarray(f("ret_gn_w")[0][None]),
        "qnT": _colT(f("mla_q_norm")[0], 4),
        "kvnT": _colT(f("mla_kv_norm")[0], 2),
        "w_uq": np.ascontiguousarray(f("w_uq")[0]),
        "w_ckp": np.ascontiguousarray(np.concatenate([f("w_in")[0][:, 4608:4864], f("w_in")[0][:, 4864:4928][:, _PERM64]], 1)),
        "w_uqr": np.ascontiguousarray(np.concatenate(
            [f("w_uq")[0][:, h * 192 + 128:h * 192 + 192][:, _PERM64] for h in range(8)], 1)),
        "w_ukv": np.ascontiguousarray(f("w_ukv")[0]),
        "w_o": np.ascontiguousarray(f("w_o")[0]),
        "ln1_w": f("ln1_w")[0][None], "ln1_b": f("ln1_b")[0][None],
        "ln2_w": f("ln2_w")[0][None], "ln2_b": f("ln2_b")[0][None],
        "w_rt": np.ascontiguousarray(np.concatenate(
            [f("router_group_w")[0]] + [f("router_expert_w")[0][g] for g in range(4)], 1)),
        "b_rt": np.ascontiguousarray(np.concatenate(
            [f("router_group_b")[0].reshape(-1), f("router_expert_b")[0].reshape(-1)])[None]),
        "ew_gate": f("expert_w_gate")[0].reshape(32, D, 512),
        "ew_up": f("expert_w_up")[0].reshape(32, D, 512),
        "ew_down": f("expert_w_down")[0].reshape(32, 512, D),
        "ident": np.eye(128, dtype=np.float32),
    }
    if not n_exp:
        for k in ("ew_gate", "ew_up", "ew_down"):
            del shared[k]
    tables = [_core_tables(0), _core_tables(1)]
    maps = []
    for core in (range(8) if cores is None else cores):
        b, hf = core // 2, core % 2
        m = dict(shared)
        m["x_own"] = np.ascontiguousarray(x[b, hf * NTOK:(hf + 1) * NTOK])
        m["x_oth"] = np.ascontiguousarray(x[b, (1 - hf) * NTOK:(2 - hf) * NTOK])
        m["ctx"] = np.ascontiguousarray(ctx[b])
        cv = np.stack([c[b], c_ctx], 0)
        m["cT"] = np.ascontiguousarray(cv.reshape(2, 16, 128).transpose(2, 1, 0).reshape(128, 32))
        m.update(tables[hf])
        maps.append(m)
    return maps


_NC_CACHE = {}


def kernel(**inputs):
    maps = prep(inputs)
    if "nc" not in _NC_CACHE:
        _NC_CACHE["nc"] = build()
    res = run_bass_kernel_spmd(_NC_CACHE["nc"], maps, core_ids=list(range(8)))
    outp = np.empty((4, 2048, D), np.float32)
    for core in range(8):
        b, hf = core // 2, core % 2
        outp[b, hf * NTOK:(hf + 1) * NTOK] = res.results[core]["out"]
    return outp
```

```python
import math
import os
from contextlib import ExitStack

import numpy as np
import concourse.bass as bass
import concourse.mybir as mybir
from concourse.bass_utils import run_bass_kernel_spmd

F32 = mybir.dt.float32
BF16 = mybir.dt.bfloat16
AF = mybir.ActivationFunctionType
ALU = mybir.AluOpType
AX = mybir.AxisListType

D = 2048
NTOK = 1024
NT = 8
GRID_W = 64
CTX = 256
IN_W = 4928
ALPHA = 2.0 ** 0.25
EPS = 1e-6
K_SCALE = 128.0 ** -0.5
ATT_SCALE = 192.0 ** -0.5
COMPUTE = ("pe", "act", "dve", "pool")
NDMA_SEM = 24
ARENA_KB = 200
T_D1, T_MK1, T_D2, T_MK2, T_CP1, T_C128, T_Z1, T_Z2, T_EF, T_MF, T_EB, T_MB = (
    0, 128, 256, 384, 512, 640, 768, 769, 770, 780, 790, 800)
NTAB = 810


class _I:
    __slots__ = ("eng", "fn", "dma", "deps", "signal", "sem", "val", "waits")

    def __init__(self, eng, fn, dma):
        self.eng = eng
        self.fn = fn
        self.dma = dma
        self.deps = ()
        self.signal = False
        self.sem = None
        self.val = 0
        self.waits = ()


class _Rec:
    def __init__(self):
        self.calls = []

    def __getattr__(self, name):
        def f(*args, **kw):
            self.calls.append((name, args, kw))
            return None
        return f


class Prog:
    def __init__(self):
        self.marks = []
        self.ins = []
        self.last_w = {}
        self.readers = {}
        self.seen = set()
        self.tile_pending = {}
        self.tile_acc = {}

    def _touch(self, k, i):
        base = k if isinstance(k, str) else k[0]
        if k not in self.seen:
            self.seen.add(k)
            p = self.tile_pending.get(base)
            if p:
                self.readers[k] = list(p)
        acc = self.tile_acc.setdefault(base, {"c": {}, "d": set()})
        x = self.ins[i] if i < len(self.ins) else None
        return acc

    def add(self, eng, fn, reads=(), writes=(), dma=False):
        if fn is not None:
            rec = _Rec()
            fn(rec)
            assert len(rec.calls) == 1
            fn = rec.calls[0]
        i = len(self.ins)
        ins = _I(eng, fn, dma)
        self.ins.append(ins)
        for k in tuple(reads) + tuple(writes):
            acc = self._touch(k, i)
            if dma:
                acc["d"].add(i)
            else:
                acc["c"][eng] = i
        deps = set()
        for k in reads:
            w = self.last_w.get(k)
            if w is not None:
                deps.add(w)
        for k in writes:
            w = self.last_w.get(k)
            if w is not None:
                deps.add(w)
            deps.update(self.readers.get(k, ()))
        for k in reads:
            self.readers.setdefault(k, []).append(i)
        for k in writes:
            self.last_w[k] = i
            self.readers[k] = []
        deps.discard(i)
        ins.deps = deps
        return i

    def pe(self, fn, r=(), w=()):
        return self.add("pe", fn, r, w)

    def act(self, fn, r=(), w=()):
        return self.add("act", fn, r, w)

    def dve(self, fn, r=(), w=()):
        return self.add("dve", fn, r, w)

    def pool(self, fn, r=(), w=()):
        return self.add("pool", fn, r, w)

    def dma(self, q, out, in_, r=(), w=()):
        return self.add(q, lambda e: e.dma_start(out=out, in_=in_), r, w, dma=True)

    def finalize(self, sems, dma_sems, fence_aps=None):
        ins = self.ins

        def prune(x):
            best = {}
            dmas = []
            for d in x.deps:
                p = ins[d]
                if p.dma:
                    dmas.append(d)
                else:
                    if p.eng == "pe" and x.eng == "pe" and not x.dma:
                        continue
                    if p.eng not in best or best[p.eng] < d:
                        best[p.eng] = d
            return sorted(list(best.values()) + dmas)

        for x in ins:
            x.deps = prune(x)
        if fence_aps and os.environ.get("K_FENCE"):
            F = set()
            for x in ins:
                if x.eng == "pe" and not x.dma:
                    for d in x.deps:
                        p = ins[d]
                        if not p.dma and p.eng in fence_aps:
                            F.add(d)
            new = []
            remap = {}
            fence_of = {}
            for i, x in enumerate(ins):
                remap[i] = len(new)
                new.append(x)
                if i in F:
                    if x.eng == "act":
                        f = _I(x.eng, ("activation", (), dict(out=fence_aps[x.eng], in_=fence_aps[x.eng], func=AF.Copy)), False)
                    else:
                        f = _I(x.eng, ("memset", (fence_aps[x.eng], 0.0), {}), False)
                    f.deps = [i]
                    fence_of[i] = len(new)
                    new.append(f)
            for j, x in enumerate(new):
                is_pe = (x.eng == "pe" and not x.dma)
                nd = []
                for d in x.deps:
                    if is_pe and d in fence_of:
                        nd.append(fence_of[d])
                    else:
                        nd.append(remap[d])
                x.deps = nd
            self.ins = ins = new
        prev = {}
        for i, x in enumerate(ins):
            if x.dma or x.fn is None or x.eng == "pe" or not os.environ.get("K_CHAIN"):
                continue
            p = prev.get(x.eng)
            if p is not None:
                x.deps = sorted(set(x.deps) | {p})
            prev[x.eng] = i
        for x in ins:
            x.deps = prune(x)
            for d in x.deps:
                ins[d].signal = True
        cnt = {e: 0 for e in COMPUTE}
        dcnt = {q: 0 for q in dma_sems}
        for x in ins:
            if x.dma:
                j = dcnt[x.eng]
                dcnt[x.eng] += 1
                pool = dma_sems[x.eng]
                x.sem = pool[j % len(pool)]
                x.val = 16 * (j // len(pool) + 1)
                x.signal = True
            elif x.signal:
                cnt[x.eng] += 1
                x.sem = sems[x.eng]
                x.val = cnt[x.eng]
        waited = {}
        for x in ins:
            ws = []
            if x.dma and x.val > 16:
                ws.append((x.sem, x.val - 16))
            for d in x.deps:
                p = ins[d]
                ws.append((p.sem, p.val))
            out = []
            for sem, val in ws:
                key = (x.eng, id(sem))
                if waited.get(key, 0) >= val:
                    continue
                waited[key] = val
                out.append((sem, val))
            x.waits = out

    def emit(self, eng_name, e):
        for x in self.ins:
            if x.eng != eng_name:
                continue
            for sem, val in x.waits:
                e.wait_ge(sem, val)
            if x.fn is None:
                continue
            name, args, kw = x.fn
            bi = getattr(e, name)(*args, **kw)
            if x.signal:
                bi.then_inc(x.sem, 16 if x.dma else 1)


class Tile:
    def __init__(self, name, ap, start, end):
        self.name = name
        self.ap = ap
        self.start = start
        self.end = end

    def __getitem__(self, key):
        return self.ap[key]


class Arena:
    def __init__(self, prog, a32, a16, nbytes):
        self.P = prog
        self.a32 = a32
        self.a16 = a16
        self.nbytes = nbytes
        self.used = []
        self.freed = []
        self.n = 0
        self.peak = 0

    def alloc(self, name, shape, dt, parts=None):
        esz = 4 if dt == F32 else 2
        free = 1
        for s in shape[1:]:
            free *= s
        nb = (free * esz + 511) // 512 * 512
        self.used.sort()
        pos = 0
        for (s, e, _) in self.used:
            if s - pos >= nb:
                break
            pos = max(pos, e)
        if pos + nb > self.nbytes:
            raise RuntimeError(f"arena full allocating {name} ({nb} B); used={[(u[2], u[1]-u[0]) for u in self.used]}")
        self.n += 1
        uname = f"{name}#{self.n}"
        self.used.append((pos, pos + nb, uname))
        self.peak = max(self.peak, pos + nb)
        pend = {}
        dm = set()
        keep = []
        for (s, e, acc) in self.freed:
            if s < pos + nb and e > pos:
                for k, v in acc["c"].items():
                    if pend.get(k, -1) < v:
                        pend[k] = v
                dm |= acc["d"]
                if s >= pos and e <= pos + nb:
                    continue
            keep.append((s, e, acc))
        self.freed = keep
        self.P.tile_pending[uname] = list(pend.values()) + sorted(dm)
        handle = self.a32 if dt == F32 else self.a16
        rowlen = self.nbytes // esz
        dims = [[rowlen, shape[0]]]
        st = free
        for s in shape[1:]:
            st //= s
            dims.append([st, s])
        ap = bass.AP(handle, pos // esz, dims)
        return Tile(uname, ap, pos, pos + nb)

    def free(self, *tiles):
        for t in tiles:
            self.used = [u for u in self.used if u[2] != t.name]
            acc = self.P.tile_acc.get(t.name, {"c": {}, "d": set()})
            self.freed.append((t.start, t.end, {"c": dict(acc["c"]), "d": set(acc["d"])}))


def bc(ap_tensor, offset, dims):
    return bass.AP(ap_tensor, offset, [list(d) for d in dims])


def build(stop=None, taps=(), n_exp=32):
    nc = bass.Bass("TRN2", target_bir_lowering=False)
    P = Prog()

    def din(name, shape):
        return nc.dram_tensor(name, list(shape), F32, kind="ExternalInput")

    x_own = din("x_own", [NTOK, D])
    x_oth = din("x_oth", [NTOK, D])
    ctx_in = din("ctx", [CTX, D])
    cT = din("cT", [128, 32])
    w_ada = din("w_ada", [D, 6 * D])
    badaT = din("badaT", [128, 96])
    b_ada = din("b_ada", [1, 6 * D])
    w_in = din("w_in", [D, IN_W])
    ret_decay = din("ret_decay", [1, 16])
    ret_gn_w = din("ret_gn_w", [1, 1024])
    qnT = din("qnT", [128, 4])
    kvnT = din("kvnT", [128, 2])
    w_uq = din("w_uq", [512, 1536])
    w_ckp = din("w_ckp", [D, 320])
    w_uqr = din("w_uqr", [512, 512])
    w_ukv = din("w_ukv", [256, 2048])
    w_o = din("w_o", [D, D])
    ln1_w = din("ln1_w", [1, D])
    ln1_b = din("ln1_b", [1, D])
    ln2_w = din("ln2_w", [1, D])
    ln2_b = din("ln2_b", [1, D])
    w_rt = din("w_rt", [D, 36])
    b_rt = din("b_rt", [1, 36])
    if n_exp:
        ew_gate = din("ew_gate", [32, D, 512])
        ew_up = din("ew_up", [32, D, 512])
        ew_down = din("ew_down", [32, 512, D])
    ident_in = din("ident", [128, 128])
    rope_r_own = din("rope_r_own", [128, 2048])
    rope_r_oth = din("rope_r_oth", [128, 2048])
    rope_m_own = din("rope_m_own", [128, 1024])
    rope_m_oth = din("rope_m_oth", [128, 1024])
    tabs = din("tabs", [128, NTAB])
    out = nc.dram_tensor("out", [NTOK, D], F32, kind="ExternalOutput")
    tap_out = {}
    taps = list(taps)
    if any(t[0] == "xmTo" for t in taps):
        taps += [("xmTo%d" % q4, [128, 16, 256]) for q4 in range(4)]
    for (tname, tshape) in taps:
        tap_out[tname] = nc.dram_tensor("tap_" + tname, [tshape[0], int(np.prod(tshape[1:]))], F32, kind="ExternalOutput")

    st = ExitStack()
    with st:
        a32 = st.enter_context(nc.sbuf_tensor("arena", [128, ARENA_KB * 256], F32))
        a16 = a32.bitcast(BF16)
        ps = [st.enter_context(nc.psum_tensor(f"ps{i}", [128, 512], F32)) for i in range(8)]
        psb = [p.bitcast(BF16) for p in ps]
        sems = {e: st.enter_context(nc.semaphore(f"s_{e}")) for e in COMPUTE}
        dsems = {q: [st.enter_context(nc.semaphore(f"d_{q}{i}")) for i in range(NDMA_SEM)] for q in ("sp", "pool")}
        A = Arena(P, a32, a16, ARENA_KB * 1024)
        PSK = [f"ps{i}" for i in range(8)]
        out_keys = []

        def tap(name, src_ap, rkeys, shape):
            if name not in tap_out:
                return
            t = A.alloc("tapst", list(shape), F32)
            idx = tuple([slice(0, shape[0])] + [slice(None)] * (len(shape) - 1))
            P.dve(lambda e: e.tensor_copy(out=t[idx], in_=src_ap), r=rkeys, w=[t.name])
            flat = 1
            for d_ in shape[1:]:
                flat *= d_
            src = bass.AP(t.ap.tensor, t.ap.offset, [[t.ap.ap[0][0], shape[0]], [1, flat]])
            P.dma("sp", tap_out[name].ap(), src, r=[t.name], w=["tap_" + name])
            out_keys.append("tap_" + name)
            A.free(t)

        FEN = A.alloc("fence", [128, 4], F32)
        ident_bf = A.alloc("ident_bf", [128, 128], BF16)
        ident_f = A.alloc("ident_f", [128, 128], F32)
        ones_bf = A.alloc("ones_bf", [128, 128], BF16)
        TB = A.alloc("tabs", [128, NTAB], F32)
        P.dma("pool", ident_bf[:, :], ident_in.ap(), w=[ident_bf.name])
        P.dma("sp", ident_f[:, :], ident_in.ap(), w=[ident_f.name])
        P.dma("sp", TB[:, :], tabs.ap(), w=[TB.name])
        P.dve(lambda e: e.memset(ones_bf[:, :], 1.0), w=[ones_bf.name])
        kvn = A.alloc("kvn", [128, 128], F32)
        qn = A.alloc("qn", [128, 128], F32)
        P.dma("sp", kvn[:, 0:2], kvnT.ap(), w=[kvn.name])
        P.dma("sp", qn[:, 0:4], qnT.ap(), w=[qn.name])

        cT_sb = A.alloc("cT", [128, 32], F32)
        sT = A.alloc("sT", [128, 32], F32)
        sT_bf = A.alloc("sT_bf", [128, 16, 2], BF16)
        s_bc = A.alloc("s_bc", [128, 16, 128], BF16)
        modsT = A.alloc("modsT", [128, 8, 16], F32)
        badaT_sb = A.alloc("badaT", [128, 96], F32)
        P.dma("sp", cT_sb[:, :], cT.ap(), w=[cT_sb.name])
        P.dma("sp", badaT_sb[:, :], badaT.ap(), w=[badaT_sb.name])
        P.act(lambda e: e.activation(out=sT[:, :], in_=cT_sb[:, :], func=AF.Silu), r=[cT_sb.name], w=[sT.name])
        P.dve(lambda e: e.tensor_copy(out=sT_bf[:, :, :], in_=sT[:, :].rearrange("p (k r) -> p k r", r=2)), r=[sT.name], w=[sT_bf.name])
        P.dve(lambda e: e.tensor_copy(out=s_bc[:, :, :], in_=bc(sT_bf.ap.tensor, sT_bf.ap.offset, [[A.nbytes // 2, 128], [2, 16], [0, 128]])),
              r=[sT_bf.name], w=[s_bc.name])

        def load_wada(blk):
            buf = A.alloc("wada", [128, 16, 512], BF16)
            src = bc(w_ada, blk * 512, [[6 * D, 128], [128 * 6 * D, 16], [1, 512]])
            P.dma("pool", buf[:, :, :], src, w=[buf.name])
            return buf

        def mods_fm_multi(specs):
            blocks = [(j_src, q, rows, p1) for (j_src, rows, p1) in specs for q in range(4)]
            nxt = load_wada(blocks[0][0] * 4 + blocks[0][1])
            for bi, (j_src, q, rows, plus_one) in enumerate(blocks):
                buf = nxt
                if bi + 1 < len(blocks):
                    nxt = load_wada(blocks[bi + 1][0] * 4 + blocks[bi + 1][1])
                for cc in range(4):
                    for kc in range(16):
                        P.pe(lambda e: e.matmul(
                            ps[0][:, cc * 2:cc * 2 + 2], lhsT=buf[:, kc, cc * 128:(cc + 1) * 128], rhs=sT_bf[:, kc, :],
                            start=(kc == 0), stop=(kc == 15)), r=[buf.name, sT_bf.name], w=[PSK[0]])
                for (row, jd) in rows:
                    dst = modsT[:, jd, q * 4:(q + 1) * 4]
                    src = bc(ps[0], row, [[512, 128], [2, 4]])
                    bsrc = badaT_sb[:, j_src * 16 + q * 4: j_src * 16 + q * 4 + 4]
                    P.dve(lambda e: e.scalar_tensor_tensor(
                        out=dst, in0=src, scalar=(1.0 if plus_one else 0.0), in1=bsrc, op0=ALU.add, op1=ALU.add),
                        r=[PSK[0], badaT_sb.name], w=[modsT.name])
                A.free(buf)

        mods_fm_multi([(0, [(0, 0), (1, 4)], False), (1, [(0, 1), (1, 5)], True)])

        if "modsT" in tap_out:
            tap("modsT", modsT[:, :, :], [modsT.name], [128, 8, 16])

        if stop == "mods":
            pass
        else:
          class Ref:
              def __init__(self, tensor, off, row):
                  self.t, self.o, self.r = tensor, off, row

              def v(self, off, dims, parts=128):
                  return bass.AP(self.t, self.o + off, [[self.r, parts]] + [list(d) for d in dims])

          def tref(tile):
              return Ref(tile.ap.tensor, tile.ap.offset, tile.ap.ap[0][0])

          def pref(i, bf=False):
              return Ref(psb[i] if bf else ps[i], 0, 1024 if bf else 512)

          def pe_fence(b):
              if os.environ.get("DBG_NOFENCE"):
                  return
              P.pe(lambda e: e.matmul(ps[b][0:1, 510:512], lhsT=ident_bf[:, 0:1], rhs=ident_bf[:, 0:2], start=True, stop=True), r=[ident_bf.name], w=[PSK[b]])

          bank_rr = [0]

          def next_bank(lo=0, hi=8):
              b = lo + (bank_rr[0] % (hi - lo))
              bank_rr[0] += 1
              return b

          TBr = tref(TB)
          statn = [0]
          STT = A.alloc("stats", [128, 16, 8], F32)

          def new_stat():
              s = statn[0] % 16
              statn[0] += 1
              key = (STT.name, s)
              P.dve(lambda e: e.memset(STT[:, s, :], 0.0), w=[key])
              return s, key

          def finish_rstd(s, key, n, c_sum, c_sq, c_mean, c_rstd, c_nmr, width=1):
              inv = 1.0 / n
              sl = lambda c: STT[:, s, c:c + width]
              P.dve(lambda e: e.tensor_scalar(out=sl(c_mean), in0=sl(c_sum), scalar1=inv, scalar2=None, op0=ALU.mult), r=[key], w=[key])
              P.dve(lambda e: e.tensor_tensor(out=sl(c_rstd), in0=sl(c_mean), in1=sl(c_mean), op=ALU.mult), r=[key], w=[key])
              P.dve(lambda e: e.tensor_scalar(out=sl(c_rstd), in0=sl(c_rstd), scalar1=-EPS, scalar2=None, op0=ALU.add), r=[key], w=[key])
              P.dve(lambda e: e.scalar_tensor_tensor(out=sl(c_rstd), in0=sl(c_sq), scalar=inv, in1=sl(c_rstd), op0=ALU.mult, op1=ALU.subtract), r=[key], w=[key])
              P.act(lambda e: e.activation(out=sl(c_rstd), in_=sl(c_rstd), func=AF.Sqrt), r=[key], w=[key])
              P.dve(lambda e: e.reciprocal(out=sl(c_rstd), in_=sl(c_rstd)), r=[key], w=[key])
              if c_nmr is not None:
                  P.dve(lambda e: e.scalar_tensor_tensor(out=sl(c_nmr), in0=sl(c_mean), scalar=-1.0, in1=sl(c_rstd), op0=ALU.mult, op1=ALU.mult), r=[key], w=[key])

          def rms_rstd(s, key, n, c_sq, c_rstd):
              P.dve(lambda e: e.tensor_scalar(out=STT[:, s, c_rstd:c_rstd + 1], in0=STT[:, s, c_sq:c_sq + 1], scalar1=1.0 / n, scalar2=EPS, op0=ALU.mult, op1=ALU.add), r=[key], w=[key])
              P.act(lambda e: e.activation(out=STT[:, s, c_rstd:c_rstd + 1], in_=STT[:, s, c_rstd:c_rstd + 1], func=AF.Sqrt), r=[key], w=[key])
              P.dve(lambda e: e.reciprocal(out=STT[:, s, c_rstd:c_rstd + 1], in_=STT[:, s, c_rstd:c_rstd + 1]), r=[key], w=[key])

          JUNK = A.alloc("junk", [128, 2048], BF16)

          def ln_stats_multi(srcs):
              n = len(srcs)
              LS = A.alloc("lnst", [128, 5, n], F32)
              ks, kq, key = (LS.name, 0), (LS.name, 1), (LS.name, 2)
              P.dve(lambda e: e.memset(LS[:, :, :], 0.0), w=[ks, kq, key])
              for i, (ap_, keys_) in enumerate(srcs):
                  P.dve(lambda e: e.tensor_reduce(out=LS[:, 0, i:i + 1], in_=ap_, axis=AX.X, op=ALU.add), r=list(keys_) + [ks], w=[ks])
              for i, (ap_, keys_) in enumerate(srcs):
                  P.act(lambda e: e.activation(out=JUNK[:, :], in_=ap_, func=AF.Square, accum_out=LS[:, 1, i:i + 1]), r=list(keys_) + [kq], w=[JUNK.name, kq])
              inv = 1.0 / 2048.0
              P.dve(lambda e: e.tensor_scalar(out=LS[:, 2, :], in0=LS[:, 0, :], scalar1=inv, scalar2=None, op0=ALU.mult), r=[ks, key], w=[key])
              P.dve(lambda e: e.tensor_tensor(out=LS[:, 3, :], in0=LS[:, 2, :], in1=LS[:, 2, :], op=ALU.mult), r=[key], w=[key])
              P.dve(lambda e: e.tensor_scalar(out=LS[:, 3, :], in0=LS[:, 3, :], scalar1=-EPS, scalar2=None, op0=ALU.add), r=[key], w=[key])
              P.dve(lambda e: e.scalar_tensor_tensor(out=LS[:, 3, :], in0=LS[:, 1, :], scalar=inv, in1=LS[:, 3, :], op0=ALU.mult, op1=ALU.subtract), r=[kq, key], w=[key])
              P.act(lambda e: e.activation(out=LS[:, 3, :], in_=LS[:, 3, :], func=AF.Sqrt), r=[key], w=[key])
              P.dve(lambda e: e.reciprocal(out=LS[:, 3, :], in_=LS[:, 3, :]), r=[key], w=[key])
              P.dve(lambda e: e.scalar_tensor_tensor(out=LS[:, 4, :], in0=LS[:, 2, :], scalar=-1.0, in1=LS[:, 3, :], op0=ALU.mult, op1=ALU.mult), r=[key], w=[key])
              return LS, [(LS[:, 3, i:i + 1], LS[:, 4, i:i + 1], key) for i in range(n)]

          def ln_apply(st, src_ap, src_keys, dst_ap, dst_keys):
              rstd_ap, nmr_ap, key = st
              P.act(lambda e: e.activation(out=dst_ap, in_=src_ap, func=AF.Identity, scale=rstd_ap, bias=nmr_ap), r=list(src_keys) + [key], w=list(dst_keys))

          def ln_stats(src_ap, src_keys):
              s, key = new_stat()
              P.dve(lambda e: e.tensor_reduce(out=STT[:, s, 0:1], in_=src_ap, axis=AX.X, op=ALU.add), r=list(src_keys) + [key], w=[key])
              P.act(lambda e: e.activation(out=JUNK[:, :], in_=src_ap, func=AF.Square, accum_out=STT[:, s, 1:2]), r=list(src_keys) + [key], w=[JUNK.name, key])
              finish_rstd(s, key, 2048.0, 0, 1, 2, 3, 4)
              return STT[:, s, 3:4], STT[:, s, 4:5], key

          def ln_norm_tile(src_ap, src_keys, dst_ap, dst_keys):
              ln_apply(ln_stats(src_ap, src_keys), src_ap, src_keys, dst_ap, dst_keys)

          def transpose_block(src_tile, ntile, dst_tile, dst_col0, j_scale, j_shift, eng_toggle):
              n = ntile * 128
              for kc in range(16):
                  b = next_bank()
                  for j in range(ntile):
                      P.pe(lambda e, b=b, j=j, kc=kc: e.transpose(psb[b][:, j * 128:(j + 1) * 128], src_tile[:, j, kc * 128:(kc + 1) * 128], ident_bf[:, :]),
                           r=[src_tile.name, ident_bf.name], w=[PSK[b]])
                  dst = dst_tile[:, kc, dst_col0:dst_col0 + n]
                  if (kc + eng_toggle) % 2 == 0:
                      P.act(lambda e, b=b, dst=dst, kc=kc: e.activation(out=dst, in_=psb[b][:, 0:n], func=AF.Identity, scale=modsT[:, j_scale, kc:kc + 1], bias=modsT[:, j_shift, kc:kc + 1]),
                            r=[PSK[b], modsT.name], w=[(dst_tile.name, kc)])
                  else:
                      P.dve(lambda e, b=b, dst=dst, kc=kc: e.tensor_scalar(out=dst, in0=psb[b][:, 0:n], scalar1=modsT[:, j_scale, kc:kc + 1], scalar2=modsT[:, j_shift, kc:kc + 1], op0=ALU.mult, op1=ALU.add),
                            r=[PSK[b], modsT.name], w=[(dst_tile.name, kc)])

          P.marks.append(("A", sum(1 for x_ in P.ins if x_.eng == "pe")))
          xmT_own = A.alloc("xmT_own", [128, 16, 1024], BF16)
          xmT_oth = A.alloc("xmT_oth", [128, 16, 1024], BF16)
          xmT_ctx = A.alloc("xmT_ctx", [128, 16, 256], BF16)
          XK = lambda t: [(t.name, kc) for kc in range(16)]

          def phase_a(src_dram, ntiles_total, dst_tile, j_scale, j_shift):
              loaded = {}

              def load(ti):
                  if ti < ntiles_total and ti not in loaded:
                      xt = A.alloc("xt", [128, 2048], F32)
                      P.dma("sp", xt[:, :], src_dram.ap()[ti * 128:(ti + 1) * 128, :], w=[xt.name])
                      loaded[ti] = xt
              blk = 0
              t0 = 0
              while t0 < ntiles_total:
                  nt = min(4, ntiles_total - t0)
                  for j in range(nt):
                      load(t0 + j)
                  load(t0 + nt)
                  load(t0 + nt + 1)
                  xn = A.alloc("xn", [128, nt, 2048], BF16)
                  xts = [loaded.pop(t0 + j) for j in range(nt)]
                  LSx, sts = ln_stats_multi([(xt[:, :], [xt.name]) for xt in xts])
                  for j in range(nt):
                      ln_apply(sts[j], xts[j][:, :], [xts[j].name], xn[:, j, :], [xn.name])
                  A.free(LSx, *xts)
                  transpose_block(xn, nt, dst_tile, t0 * 128, j_scale, j_shift, blk)
                  A.free(xn)
                  t0 += nt
                  blk += 1

          phase_a(x_oth, 8, xmT_oth, 1, 0)
          phase_a(ctx_in, 2, xmT_ctx, 5, 4)
          phase_a(x_own, 8, xmT_own, 1, 0)
          if "xmTo" in tap_out:
              for q4 in range(4):
                  tap("xmTo%d" % q4, xmT_oth[:, :, q4 * 256:(q4 + 1) * 256], XK(xmT_oth), [128, 16, 256])
          if "xmT" in tap_out:
              tap("xmT", xmT_own[:, :, 0:256], XK(xmT_own), [128, 16, 256])
              tap("xmTc", xmT_ctx[:, :, :], XK(xmT_ctx), [128, 16, 256])

          P.marks.append(("0c", sum(1 for x_ in P.ins if x_.eng == "pe")))
          LG = A.alloc("LG", [128, 16], F32)
          G128 = A.alloc("G128", [128, 16], F32)
          KZ = A.alloc("KZ", [128, 16], F32)
          lgt = A.alloc("lgt", [128, 16], F32)
          P.dma("sp", LG[:, :], bc(ret_decay, 0, [[0, 128], [1, 16]]), w=[LG.name])
          P.act(lambda e: e.activation(out=lgt[:, :], in_=LG[:, :], func=AF.Exp, scale=-1.0), r=[LG.name], w=[lgt.name])
          P.dve(lambda e: e.tensor_scalar(out=LG[:, :], in0=lgt[:, :], scalar1=-1.0 / 7.0, scalar2=1.0 / 6.0, op0=ALU.mult, op1=ALU.add), r=[lgt.name], w=[LG.name])
          for kk in (5, 4, 3, 2, 1):
              P.dve(lambda e: e.tensor_tensor(out=LG[:, :], in0=LG[:, :], in1=lgt[:, :], op=ALU.mult), r=[LG.name, lgt.name], w=[LG.name])
              P.dve(lambda e, kk=kk: e.tensor_scalar(out=LG[:, :], in0=LG[:, :], scalar1=-1.0, scalar2=1.0 / kk, op0=ALU.mult, op1=ALU.add), r=[LG.name], w=[LG.name])
          P.dve(lambda e: e.scalar_tensor_tensor(out=LG[:, :], in0=LG[:, :], scalar=-1.0, in1=lgt[:, :], op0=ALU.mult, op1=ALU.mult), r=[LG.name, lgt.name], w=[LG.name])
          P.act(lambda e: e.activation(out=G128[:, :], in_=LG[:, :], func=AF.Exp, scale=128.0), r=[LG.name], w=[G128.name])
          P.dve(lambda e: e.tensor_scalar(out=lgt[:, 0:8], in0=LG[:, 0:8], scalar1=TB[:, T_Z1:T_Z1 + 1], scalar2=None, op0=ALU.mult), r=[LG.name, TB.name], w=[lgt.name])
          P.dve(lambda e: e.tensor_scalar(out=lgt[:, 8:16], in0=LG[:, 8:16], scalar1=TB[:, T_Z2:T_Z2 + 1], scalar2=None, op0=ALU.mult), r=[LG.name, TB.name], w=[lgt.name])
          P.act(lambda e: e.activation(out=KZ[:, :], in_=lgt[:, :], func=AF.Exp), r=[lgt.name], w=[KZ.name])
          P.dve(lambda e: e.tensor_scalar(out=KZ[:, :], in0=KZ[:, :], scalar1=K_SCALE, scalar2=None, op0=ALU.mult), r=[KZ.name], w=[KZ.name])
          MT = A.alloc("MT", [128, 8, 128], F32)
          XiF = A.alloc("XiF", [128, 8, 128], BF16)
          XiB = A.alloc("XiB", [128, 8, 128], BF16)
          WF = A.alloc("WF", [128, 10, 8], F32)
          WB = A.alloc("WB", [128, 10, 8], F32)
          mtmp = A.alloc("mtmp", [128, 128], F32)
          for h in range(8):
              P.act(lambda e, h=h: e.activation(out=MT[:, h, :], in_=TB[:, T_D1:T_D1 + 128], func=AF.Exp, scale=LG[:, h:h + 1]), r=[LG.name, TB.name], w=[(MT.name, h)])
              P.dve(lambda e, h=h: e.scalar_tensor_tensor(out=MT[:, h, :], in0=MT[:, h, :], scalar=K_SCALE, in1=TB[:, T_MK1:T_MK1 + 128], op0=ALU.mult, op1=ALU.mult), r=[(MT.name, h), TB.name], w=[(MT.name, h)])
              P.act(lambda e, h=h: e.activation(out=mtmp[:, :], in_=TB[:, T_D2:T_D2 + 128], func=AF.Exp, scale=LG[:, 8 + h:9 + h]), r=[LG.name, TB.name], w=[mtmp.name])
              P.dve(lambda e, h=h: e.scalar_tensor_tensor(out=mtmp[:, :], in0=mtmp[:, :], scalar=K_SCALE, in1=TB[:, T_MK2:T_MK2 + 128], op0=ALU.mult, op1=ALU.mult), r=[mtmp.name, TB.name], w=[mtmp.name])
              P.dve(lambda e, h=h: e.tensor_tensor(out=MT[:, h, :], in0=MT[:, h, :], in1=mtmp[:, :], op=ALU.add), r=[(MT.name, h), mtmp.name], w=[(MT.name, h)])
              P.act(lambda e, h=h: e.activation(out=XiF[:, h, :], in_=TB[:, T_CP1:T_CP1 + 128], func=AF.Exp, scale=LG[:, h:h + 1]), r=[LG.name, TB.name], w=[(XiF.name, h)])
              P.act(lambda e, h=h: e.activation(out=XiB[:, h, :], in_=TB[:, T_C128:T_C128 + 128], func=AF.Exp, scale=LG[:, 8 + h:9 + h]), r=[LG.name, TB.name], w=[(XiB.name, h)])
              P.act(lambda e, h=h: e.activation(out=WF[:, :, h], in_=TB[:, T_EF:T_EF + 10], func=AF.Exp, scale=LG[:, h:h + 1]), r=[LG.name, TB.name], w=[WF.name])
              P.act(lambda e, h=h: e.activation(out=WB[:, :, h], in_=TB[:, T_EB:T_EB + 10], func=AF.Exp, scale=LG[:, 8 + h:9 + h]), r=[LG.name, TB.name], w=[WB.name])
          P.dve(lambda e: e.scalar_tensor_tensor(out=WF[:, :, :], in0=WF[:, :, :], scalar=K_SCALE, in1=TBr.v(T_MF, [[1, 10], [0, 8]]), op0=ALU.mult, op1=ALU.mult), r=[WF.name, TB.name], w=[WF.name])
          P.dve(lambda e: e.scalar_tensor_tensor(out=WB[:, :, :], in0=WB[:, :, :], scalar=K_SCALE, in1=TBr.v(T_MB, [[1, 10], [0, 8]]), op0=ALU.mult, op1=ALU.mult), r=[WB.name, TB.name], w=[WB.name])
          A.free(mtmp, lgt)
          MTK = [(MT.name, h) for h in range(8)]
          if "decay" in tap_out:
              tap("LG", LG[:, :], [LG.name], [128, 16])
              tap("MT", MT[:, :, :], MTK, [128, 8, 128])
              tap("WF", WF[:, :, :], [WF.name], [128, 10, 8])
              tap("WB", WB[:, :, :], [WB.name], [128, 10, 8])

          def load_win(col0, ncols, name="w"):
              t = A.alloc(name, [128, 16, ncols], BF16)
              P.dma("pool", t[:, :, :], bc(w_in, col0, [[IN_W, 128], [128 * IN_W, 16], [1, ncols]]), w=[t.name])
              return t

          def proj_tile(xT, col0, wt, ncols, bank, wcol0=0, wkeys=None):
              for kc in range(16):
                  P.pe(lambda e, kc=kc: e.matmul(ps[bank][:, 0:ncols], lhsT=xT[:, kc, col0:col0 + 128], rhs=wt[:, kc, wcol0:wcol0 + ncols], start=(kc == 0), stop=(kc == 15)),
                       r=[(xT.name, kc)] + (wkeys or [wt.name]), w=[PSK[bank]])

          def rope(dst_ref, src_ref, nh, W, cos_ap_ref, sin_ap_ref, rkeys, wkeys, tmpname="ropetmp", tab_stride=0):
              q = W // 4
              CH = ["ropechain"] if os.environ.get("DBG_ROPE_CHAIN") else []
              rkeys = list(rkeys)
              t1 = A.alloc(tmpname, [128, nh, W], F32)
              t2 = A.alloc(tmpname, [128, nh, W], F32)
              t1r, t2r = tref(t1), tref(t2)
              hdim = [[W, nh]] if nh > 1 else []
              bdim = [[tab_stride, nh]] if nh > 1 else []
              full = hdim + [[1, W]]
              P.dve(lambda e: e.tensor_tensor(out=t1r.v(0, full), in0=src_ref.v(0, full), in1=cos_ap_ref.v(0, bdim + [[1, W]]), op=ALU.mult),
                    r=rkeys + CH, w=[t1.name] + CH)
              if W == 64:
                  q = W // 2
                  half = hdim + [[1, q]]
                  bhalf = bdim + [[1, q]]
              else:
                  half = hdim + [[2 * q, 2], [1, q]]
                  bhalf = bdim + [[2 * q, 2], [1, q]]
              P.dve(lambda e: e.tensor_tensor(out=t2r.v(0, half), in0=src_ref.v(q, half), in1=sin_ap_ref.v(0, bhalf), op=ALU.mult),
                    r=rkeys + CH, w=[(t2.name, 0)] + CH)
              P.dve(lambda e: e.tensor_tensor(out=t2r.v(q, half), in0=src_ref.v(0, half), in1=sin_ap_ref.v(q, bhalf), op=ALU.mult),
                    r=rkeys + CH, w=[(t2.name, 1)] + CH)
              P.add(os.environ.get("DBG_ROPE_ENG", "dve"), lambda e: e.tensor_tensor(out=dst_ref.v(0, full), in0=t1r.v(0, full), in1=t2r.v(0, full), op=ALU.add),
                    [t1.name, (t2.name, 0), (t2.name, 1)], wkeys)
              A.free(t1, t2)

          RR_OTH = A.alloc("rr_oth", [128, 2, 8, 128], F32)
          P.dma("sp", RR_OTH[:, :, :, :].rearrange("p a b c -> p (a b c)"), rope_r_oth.ap(), w=[RR_OTH.name])
          RRo = tref(RR_OTH)

          P.marks.append(("B", sum(1 for x_ in P.ins if x_.eng == "pe")))
          SF0 = A.alloc("SF0", [128, 8, 128], F32)
          SB0 = A.alloc("SB0", [128, 8, 128], F32)
          for hg in range(2):
              wk = load_win(1024 + hg * 512, 512, "wk")
              wv = load_win(2048 + hg * 512, 512, "wv")
              kf_all = A.alloc("kf_all", [128, 10, 512], BF16)
              kb_all = A.alloc("kb_all", [128, 10, 512], BF16)
              v_all = A.alloc("v_all", [128, 10, 512], BF16)
              WFr, WBr = tref(WF), tref(WB)
              for t in range(10):
                  xT, c0 = (xmT_oth, t * 128) if t < 8 else (xmT_ctx, (t - 8) * 128)
                  b = next_bank()
                  proj_tile(xT, c0, wk, 512, b)
                  if t < 8:
                      kr = A.alloc("kr", [128, 4, 128], F32)
                      rope(tref(kr), pref(b), 4, 128, Ref(RRo.t, RRo.o + t * 128, RRo.r), Ref(RRo.t, RRo.o + 1024 + t * 128, RRo.r), [PSK[b], RR_OTH.name], [kr.name])
                      ksrc, kkeys = tref(kr), [kr.name]
                  else:
                      ksrc, kkeys = pref(b), [PSK[b]]
                  full = [[128, 4], [1, 128]]
                  P.dve(lambda e, ksrc=ksrc, t=t: e.tensor_tensor(out=tref(kf_all).v(t * 512, full), in0=ksrc.v(0, full), in1=WFr.v(t * 8 + hg * 4, [[1, 4], [0, 128]]), op=ALU.mult),
                        r=kkeys + [WF.name], w=[(kf_all.name, t)])
                  P.dve(lambda e, ksrc=ksrc, t=t: e.tensor_tensor(out=tref(kb_all).v(t * 512, full), in0=ksrc.v(0, full), in1=WBr.v(t * 8 + hg * 4, [[1, 4], [0, 128]]), op=ALU.mult),
                        r=kkeys + [WB.name], w=[(kb_all.name, t)])
                  if t < 8:
                      A.free(kr)
                  b2 = next_bank()
                  proj_tile(xT, c0, wv, 512, b2)
                  P.act(lambda e, b2=b2, t=t: e.activation(out=v_all[:, t, :], in_=ps[b2][:, :], func=AF.Copy), r=[PSK[b2]], w=[(v_all.name, t)])
              A.free(wk, wv)
              for (kall, S0) in ((kf_all, SF0), (kb_all, SB0)):
                  b = next_bank()
                  for h in range(4):
                      for t in range(10):
                          P.pe(lambda e, b=b, h=h, t=t, kall=kall: e.matmul(ps[b][:, h * 128:(h + 1) * 128], lhsT=kall[:, t, h * 128:(h + 1) * 128], rhs=v_all[:, t, h * 128:(h + 1) * 128], start=(t == 0), stop=(t == 9)),
                               r=[(kall.name, t), (v_all.name, t)], w=[PSK[b]])
                  P.dve(lambda e, b=b, S0=S0: e.tensor_copy(out=S0[:, hg * 4:(hg + 1) * 4, :], in_=ps[b][:, :].rearrange("p (h e) -> p h e", h=4)), r=[PSK[b]], w=[(S0.name, hg)])
              A.free(kf_all, kb_all, v_all)
          A.free(RR_OTH)
          if "SF0" in tap_out:
              tap("SF0", SF0[:, :, :], [(SF0.name, 0), (SF0.name, 1)], [128, 8, 128])
              tap("SB0", SB0[:, :, :], [(SB0.name, 0), (SB0.name, 1)], [128, 8, 128])

          nb6 = lambda: next_bank(0, 6)
          P.marks.append(("C", sum(1 for x_ in P.ins if x_.eng == "pe")))
          ckvT = A.alloc("ckvT", [128, 2, 2304], BF16)
          kpeT = A.alloc("kpeT", [64, 2304], BF16)
          RM_OWN = A.alloc("rm_own", [128, 2, 8, 64], F32)
          RM_OTH = A.alloc("rm_oth", [128, 2, 8, 64], F32)
          P.dma("sp", RM_OWN[:, :, :, :].rearrange("p a b c -> p (a b c)"), rope_m_own.ap(), w=[RM_OWN.name])
          P.dma("sp", RM_OTH[:, :, :, :].rearrange("p a b c -> p (a b c)"), rope_m_oth.ap(), w=[RM_OTH.name])
          wc = A.alloc("wc", [128, 16, 320], BF16)
          P.dma("pool", wc[:, :, :], bc(w_ckp, 0, [[320, 128], [128 * 320, 16], [1, 320]]), w=[wc.name])
          CK = lambda lo, hi: [(ckvT.name, i) for i in range(lo // 128, (hi + 127) // 128)]
          PK = lambda lo, hi: [(kpeT.name, i) for i in range(lo // 128, (hi + 127) // 128)]
          for (xT, ntiles, key0, rtab) in ((xmT_oth, 8, 1024, RM_OTH), (xmT_ctx, 2, 2048, None), (xmT_own, 8, 0, RM_OWN)):
              t0 = 0
              while t0 < ntiles:
                  nt = min(4, ntiles - t0)
                  n = nt * 128
                  ckvn = A.alloc("ckvn", [128, nt, 256], BF16)
                  kper = A.alloc("kper", [128, nt, 64], BF16)
                  kps = A.alloc("kps", [128, nt, 64], F32)
                  for j in range(nt):
                      t = t0 + j
                      b = nb6()
                      proj_tile(xT, t * 128, wc, 320, b)
                      s, key = new_stat()
                      P.act(lambda e: e.activation(out=JUNK[:, 0:256], in_=ps[b][:, 0:256], func=AF.Square, accum_out=STT[:, s, 0:1]), r=[PSK[b], key], w=[JUNK.name, key])
                      rms_rstd(s, key, 256.0, 0, 1)
                      P.act(lambda e: e.activation(out=ckvn[:, j, :], in_=ps[b][:, 0:256], func=AF.Copy, scale=STT[:, s, 1:2]), r=[PSK[b], key], w=[(ckvn.name, j)])
                      if rtab is not None:
                          P.act(lambda e: e.activation(out=kps[:, j, :], in_=ps[b][:, 256:320], func=AF.Copy), r=[PSK[b]], w=[(kps.name, j)])
                      else:
                          P.act(lambda e: e.activation(out=kper[:, j, :], in_=ps[b][:, 256:320], func=AF.Copy), r=[PSK[b]], w=[(kper.name, j)])
                  if rtab is not None:
                      rt = tref(rtab)
                      rope(tref(kper), tref(kps), nt, 64, Ref(rt.t, rt.o + t0 * 64, rt.r), Ref(rt.t, rt.o + 512 + t0 * 64, rt.r),
                           [(kps.name, j) for j in range(nt)] + [rtab.name], [(kper.name, j) for j in range(nt)], tab_stride=64)
                  A.free(kps)
                  k0 = key0 + t0 * 128
                  for lc in range(2):
                      b = nb6()
                      for j in range(nt):
                          P.pe(lambda e: e.transpose(psb[b][:, j * 128:(j + 1) * 128], ckvn[:, j, lc * 128:(lc + 1) * 128], ident_bf[:, :]), r=[(ckvn.name, j), ident_bf.name], w=[PSK[b]])
                      pe_fence(b)
                      P.dve(lambda e: e.tensor_scalar(out=ckvT[:, lc, k0:k0 + n], in0=psb[b][:, 0:n], scalar1=kvn[:, lc:lc + 1], scalar2=None, op0=ALU.mult), r=[PSK[b], kvn.name], w=CK(k0, k0 + n))
                  b = nb6()
                  if not os.environ.get("DBG_NOKPE") and not os.environ.get("DBG_NOKPET"):
                      for j in range(nt):
                          P.pe(lambda e: e.transpose(psb[b][0:64, j * 128:(j + 1) * 128], kper[:, j, :], ident_bf[:, :]), r=[(kper.name, j), ident_bf.name], w=[PSK[b]])
                      pe_fence(b)
                      P.act(lambda e: e.activation(out=kpeT[0:64, k0:k0 + n], in_=psb[b][0:64, 0:n], func=AF.Copy), r=[PSK[b]], w=PK(k0, k0 + n))
                  A.free(ckvn, kper)
                  t0 += nt
          A.free(wc, xmT_oth, xmT_ctx, RM_OTH)
          if "ckvT" in tap_out:
              tap("ckvT", ckvT[:, :, :], CK(0, 2304), [128, 2, 2304])
              tap("kpeT", kpeT[0:64, :], PK(0, 2304), [64, 2304])

          P.marks.append(("D", sum(1 for x_ in P.ins if x_.eng == "pe")))
          mixT = A.alloc("mixT", [128, 16, 1024], BF16)
          NHP = 0 if stop == "C" else (1 if stop == "D1" else 4)
          GW = A.alloc("GW", [128, 1024], F32)
          P.dma("sp", GW[:, :], bc(ret_gn_w, 0, [[0, 128], [1, 1024]]), w=[GW.name])
          RR_OWN = A.alloc("rr_own", [128, 2, 8, 128], F32)
          P.dma("sp", RR_OWN[:, :, :, :].rearrange("p a b c -> p (a b c)"), rope_r_own.ap(), w=[RR_OWN.name])
          RRw = tref(RR_OWN)
          KZr, G128r, MTr, XiFr, XiBr, GWr = tref(KZ), tref(G128), tref(MT), tref(XiF), tref(XiB), tref(GW)
          hd = [[128, 2], [1, 128]]
          for hp in range(NHP):
              wq = load_win(hp * 256, 256, "wq")
              wk = load_win(1024 + hp * 256, 256, "wk")
              wv = load_win(2048 + hp * 256, 256, "wv")
              wg = load_win(3072 + hp * 256, 256, "wg")
              qT = A.alloc("qT", [128, 2, 1024], BF16)
              kT = A.alloc("kT", [128, 2, 1024], BF16)
              kzf = A.alloc("kzf", [128, 8, 256], BF16)
              kzb = A.alloc("kzb", [128, 8, 256], BF16)
              vt = A.alloc("vt", [128, 8, 256], BF16)
              sg = A.alloc("sg", [128, 8, 256], BF16)
              pend = []

              def flush():
                  for th in pend:
                      th()
                  del pend[:]

              for t in range(8):
                  blk, j = t // 4, t % 4
                  bq = nb6()
                  proj_tile(xmT_own, t * 128, wq, 256, bq)
                  bk = nb6()
                  proj_tile(xmT_own, t * 128, wk, 256, bk)
                  bv = nb6()
                  proj_tile(xmT_own, t * 128, wv, 256, bv)
                  bg = nb6()
                  proj_tile(xmT_own, t * 128, wg, 256, bg)
                  flush()
                  qr = A.alloc("qr", [128, 2, 128], BF16)
                  rope(tref(qr), pref(bq), 2, 128, Ref(RRw.t, RRw.o + t * 128, RRw.r), Ref(RRw.t, RRw.o + 1024 + t * 128, RRw.r), [PSK[bq], RR_OWN.name], [qr.name])
                  kr = A.alloc("kr", [128, 2, 128], F32)
                  rope(tref(kr), pref(bk), 2, 128, Ref(RRw.t, RRw.o + t * 128, RRw.r), Ref(RRw.t, RRw.o + 1024 + t * 128, RRw.r), [PSK[bk], RR_OWN.name], [kr.name])
                  P.act(lambda e: e.activation(out=vt[:, t, :], in_=ps[bv][:, 0:256], func=AF.Copy), r=[PSK[bv]], w=[(vt.name, t)])
                  P.act(lambda e: e.activation(out=sg[:, t, :], in_=ps[bg][:, 0:256], func=AF.Silu), r=[PSK[bg]], w=[(sg.name, t)])
                  P.dve(lambda e: e.tensor_tensor(out=tref(kzf).v(t * 256, hd), in0=kr[:, :, :], in1=KZr.v(2 * hp, [[1, 2], [0, 128]]), op=ALU.mult), r=[kr.name, KZ.name], w=[(kzf.name, t)])
                  P.pool(lambda e: e.tensor_tensor(out=tref(kzb).v(t * 256, hd), in0=kr[:, :, :], in1=KZr.v(8 + 2 * hp, [[1, 2], [0, 128]]), op=ALU.mult), r=[kr.name, KZ.name], w=[(kzb.name, t)])
                  krb = A.alloc("krb", [128, 2, 128], BF16)
                  P.act(lambda e: e.activation(out=krb[:, :, :], in_=kr[:, :, :], func=AF.Copy), r=[kr.name], w=[krb.name])

                  def tr(qr=qr, krb=krb, kr=kr, blk=blk, j=j):
                      for h in range(2):
                          P.pe(lambda e: e.transpose(psb[6][:, h * 512 + j * 128:h * 512 + (j + 1) * 128], qr[:, h, :], ident_bf[:, :]), r=[qr.name, ident_bf.name], w=[PSK[6]])
                      for h in range(2):
                          P.pe(lambda e: e.transpose(psb[7][:, h * 512 + j * 128:h * 512 + (j + 1) * 128], krb[:, h, :], ident_bf[:, :]), r=[krb.name, ident_bf.name], w=[PSK[7]])
                      A.free(qr, kr, krb)
                      if j == 3:
                          P.dve(lambda e: e.tensor_copy(out=qT[:, :, blk * 512:(blk + 1) * 512], in_=psb[6][:, :].rearrange("p (h n) -> p h n", h=2)), r=[PSK[6]], w=[(qT.name, blk)])
                          P.act(lambda e: e.activation(out=kT[:, :, blk * 512:(blk + 1) * 512], in_=psb[7][:, :].rearrange("p (h n) -> p h n", h=2), func=AF.Copy), r=[PSK[7]], w=[(kT.name, blk)])
                  pend.append(tr)
              flush()
              A.free(wq, wk, wv, wg)
              SF = A.alloc("SF", [128, 8, 2, 128], F32)
              SBk = A.alloc("SBk", [128, 8, 2, 128], F32)
              SFb = A.alloc("SFb", [128, 8, 2, 128], BF16)
              SBb = A.alloc("SBb", [128, 8, 2, 128], BF16)
              P.dve(lambda e: e.tensor_copy(out=SF[:, 0, :, :], in_=SF0[:, 2 * hp:2 * hp + 2, :]), r=[(SF0.name, hp // 2)], w=[(SF.name, 0)])
              P.dve(lambda e: e.tensor_copy(out=SBk[:, 7, :, :], in_=SB0[:, 2 * hp:2 * hp + 2, :]), r=[(SB0.name, hp // 2)], w=[(SBk.name, 7)])
              for i in range(7):
                  b = nb6()
                  for h in range(2):
                      P.pe(lambda e: e.matmul(ps[b][:, h * 128:(h + 1) * 128], lhsT=kzf[:, i, h * 128:(h + 1) * 128], rhs=vt[:, i, h * 128:(h + 1) * 128], start=True, stop=True),
                           r=[(kzf.name, i), (vt.name, i)], w=[PSK[b]])
                      P.pe(lambda e: e.matmul(ps[b][:, 256 + h * 128:256 + (h + 1) * 128], lhsT=kzb[:, 7 - i, h * 128:(h + 1) * 128], rhs=vt[:, 7 - i, h * 128:(h + 1) * 128], start=True, stop=True),
                           r=[(kzb.name, 7 - i), (vt.name, 7 - i)], w=[PSK[b]])
                  for h in range(2):
                      P.dve(lambda e: e.scalar_tensor_tensor(out=SF[:, i + 1, h, :], in0=SF[:, i, h, :], scalar=G128[:, 2 * hp + h:2 * hp + h + 1], in1=ps[b][:, h * 128:(h + 1) * 128], op0=ALU.mult, op1=ALU.add),
                            r=[(SF.name, i), G128.name, PSK[b]], w=[(SF.name, i + 1)])
                      P.dve(lambda e: e.scalar_tensor_tensor(out=SBk[:, 6 - i, h, :], in0=SBk[:, 7 - i, h, :], scalar=G128[:, 8 + 2 * hp + h:8 + 2 * hp + h + 1], in1=ps[b][:, 256 + h * 128:256 + (h + 1) * 128], op0=ALU.mult, op1=ALU.add),
                            r=[(SBk.name, 7 - i), G128.name, PSK[b]], w=[(SBk.name, 6 - i)])
              P.act(lambda e: e.activation(out=SFb[:, :, :, :].rearrange("p a b c -> p (a b c)"), in_=SF[:, :, :, :].rearrange("p a b c -> p (a b c)"), func=AF.Copy), r=[(SF.name, i) for i in range(8)], w=[SFb.name])
              P.act(lambda e: e.activation(out=SBb[:, :, :, :].rearrange("p a b c -> p (a b c)"), in_=SBk[:, :, :, :].rearrange("p a b c -> p (a b c)"), func=AF.Copy), r=[(SBk.name, i) for i in range(8)], w=[SBb.name])
              if hp == 0 and "SF" in tap_out:
                  tap("SF", SF[:, :, :, :].rearrange("p a b c -> p (a b c)"), [(SF.name, i) for i in range(8)], [128, 2048])
                  tap("SBk", SBk[:, :, :, :].rearrange("p a b c -> p (a b c)"), [(SBk.name, i) for i in range(8)], [128, 2048])
              A.free(SF, SBk)

              def scores(t):
                  blk = t // 4
                  b = nb6()
                  for h in range(2):
                      P.pe(lambda e: e.matmul(ps[b][:, h * 128:(h + 1) * 128], lhsT=kT[:, h, t * 128:(t + 1) * 128], rhs=qT[:, h, t * 128:(t + 1) * 128], start=True, stop=True),
                           r=[(kT.name, blk), (qT.name, blk)], w=[PSK[b]])
                  PT = A.alloc("PT", [128, 2, 128], BF16)
                  P.dve(lambda e: e.tensor_tensor(out=PT[:, :, :], in0=ps[b][:, 0:256].rearrange("p (h n) -> p h n", h=2), in1=MT[:, 2 * hp:2 * hp + 2, :], op=ALU.mult),
                        r=[PSK[b], (MT.name, 2 * hp), (MT.name, 2 * hp + 1)], w=[PT.name])
                  qf = A.alloc("qf", [128, 2, 128], BF16)
                  qb_ = A.alloc("qb", [128, 2, 128], BF16)
                  P.pool(lambda e: e.tensor_tensor(out=qf[:, :, :], in0=qT[:, :, t * 128:(t + 1) * 128], in1=XiF[:, 2 * hp:2 * hp + 2, :], op=ALU.mult),
                         r=[(qT.name, blk), (XiF.name, 2 * hp), (XiF.name, 2 * hp + 1)], w=[qf.name])
                  P.pool(lambda e: e.tensor_tensor(out=qb_[:, :, :], in0=qT[:, :, t * 128:(t + 1) * 128], in1=XiB[:, 2 * hp:2 * hp + 2, :], op=ALU.mult),
                         r=[(qT.name, blk), (XiB.name, 2 * hp), (XiB.name, 2 * hp + 1)], w=[qb_.name])
                  return PT, qf, qb_

              cur = scores(0)
              for t in range(8):
                  blk, j = t // 4, t % 4
                  nxt = scores(t + 1) if t + 1 < 8 else None
                  PT, qf, qb_ = cur
                  bo = nb6()
                  for h in range(2):
                      oreg = ps[bo][:, h * 128:(h + 1) * 128]
                      P.pe(lambda e: e.matmul(oreg, lhsT=PT[:, h, :], rhs=vt[:, t, h * 128:(h + 1) * 128], start=True, stop=False), r=[PT.name, (vt.name, t)], w=[PSK[bo]])
                      P.pe(lambda e: e.matmul(oreg, lhsT=qf[:, h, :], rhs=SFb[:, t, h, :], start=False, stop=False), r=[qf.name, SFb.name], w=[PSK[bo]])
                      P.pe(lambda e: e.matmul(oreg, lhsT=qb_[:, h, :], rhs=SBb[:, t, h, :], start=False, stop=True), r=[qb_.name, SBb.name], w=[PSK[bo]])
                  flush()
                  s_, key = new_stat()
                  P.dve(lambda e: e.tensor_reduce(out=STT[:, s_, 0:2], in_=ps[bo][:, 0:256].rearrange("p (h n) -> p h n", h=2), axis=AX.X, op=ALU.add), r=[PSK[bo], key], w=[key])
                  for h in range(2):
                      P.act(lambda e: e.activation(out=JUNK[:, 0:128], in_=ps[bo][:, h * 128:(h + 1) * 128], func=AF.Square, accum_out=STT[:, s_, 2 + h:3 + h]), r=[PSK[bo], key], w=[JUNK.name, key])
                  finish_rstd(s_, key, 128.0, 0, 2, 4, 6, None, width=2)
                  y = A.alloc("y", [128, 2, 128], F32)
                  for h in range(2):
                      P.dve(lambda e: e.tensor_scalar(out=y[:, h, :], in0=ps[bo][:, h * 128:(h + 1) * 128], scalar1=STT[:, s_, 4 + h:5 + h], scalar2=STT[:, s_, 6 + h:7 + h], op0=ALU.subtract, op1=ALU.mult),
                            r=[PSK[bo], key], w=[(y.name, h)])
                  P.pool(lambda e: e.tensor_tensor(out=y[:, :, :], in0=y[:, :, :], in1=GWr.v(hp * 256, hd), op=ALU.mult), r=[(y.name, 0), (y.name, 1), GW.name], w=[(y.name, 0), (y.name, 1)])
                  ret = A.alloc("ret", [128, 2, 128], BF16)
                  P.pool(lambda e: e.tensor_tensor(out=ret[:, :, :], in0=y[:, :, :], in1=tref(sg).v(t * 256, hd), op=ALU.mult), r=[(y.name, 0), (y.name, 1), (sg.name, t)], w=[ret.name])

                  def tr2(ret=ret, PT=PT, qf=qf, qb_=qb_, y=y, blk=blk, j=j):
                      for h in range(2):
                          P.pe(lambda e: e.transpose(psb[6][:, h * 512 + j * 128:h * 512 + (j + 1) * 128], ret[:, h, :], ident_bf[:, :]), r=[ret.name, ident_bf.name], w=[PSK[6]])
                      A.free(PT, qf, qb_, y, ret)
                      if j == 3:
                          P.dve(lambda e: e.tensor_copy(out=mixT[:, 2 * hp:2 * hp + 2, blk * 512:(blk + 1) * 512], in_=psb[6][:, :].rearrange("p (h n) -> p h n", h=2)), r=[PSK[6]],
                                w=[(mixT.name, 2 * hp), (mixT.name, 2 * hp + 1)])
                  pend.append(tr2)
                  cur = nxt
              flush()
              A.free(qT, kT, kzf, kzb, vt, sg, SFb, SBb)
          A.free(RR_OWN, GW, SF0, SB0, MT, XiF, XiB, WF, WB)
          if "ret" in tap_out:
              tap("ret", mixT[:, 0:8, 0:256], [(mixT.name, i) for i in range(8)], [128, 8, 256])

          nb4 = lambda: next_bank(0, 4)
          MK = lambda lo, hi: [(mixT.name, i) for i in range(lo, hi)]
          if stop not in ("C", "D1", "D"):
              P.marks.append(("E", sum(1 for x_ in P.ins if x_.eng == "pe")))
              wcq = load_win(4096, 512, "wcq")
              cqT = A.alloc("cqT", [128, 4, 1024], BF16)
              for blk in range(2):
                  cqn = A.alloc("cqn", [128, 4, 512], BF16)
                  for j in range(4):
                      t = blk * 4 + j
                      b = nb6()
                      proj_tile(xmT_own, t * 128, wcq, 512, b)
                      s, key = new_stat()
                      P.act(lambda e: e.activation(out=JUNK[:, 0:512], in_=ps[b][:, :], func=AF.Square, accum_out=STT[:, s, 0:1]), r=[PSK[b], key], w=[JUNK.name, key])
                      rms_rstd(s, key, 512.0, 0, 1)
                      P.act(lambda e: e.activation(out=cqn[:, j, :], in_=ps[b][:, :], func=AF.Copy, scale=STT[:, s, 1:2]), r=[PSK[b], key], w=[(cqn.name, j)])
                  for lc in range(4):
                      b = nb6()
                      for j in range(4):
                          P.pe(lambda e: e.transpose(psb[b][:, j * 128:(j + 1) * 128], cqn[:, j, lc * 128:(lc + 1) * 128], ident_bf[:, :]), r=[(cqn.name, j), ident_bf.name], w=[PSK[b]])
                      P.dve(lambda e: e.tensor_scalar(out=cqT[:, lc, blk * 512:(blk + 1) * 512], in0=psb[b][:, 0:512], scalar1=qn[:, lc:lc + 1], scalar2=None, op0=ALU.mult), r=[PSK[b], qn.name], w=[(cqT.name, blk)])
                  A.free(cqn)
              A.free(wcq, xmT_own)
              CQK = [(cqT.name, 0), (cqT.name, 1)]
              wqr = A.alloc("wqr", [128, 4, 512], BF16)
              P.dma("pool", wqr[:, :, :], bc(w_uqr, 0, [[512, 128], [128 * 512, 4], [1, 512]]), w=[wqr.name])
              qrT = A.alloc("qrT", [64, 8, 1024], BF16)
              RMw = tref(RM_OWN)
              for t in range(8):
                  b = nb6()
                  for lc in range(4):
                      P.pe(lambda e: e.matmul(ps[b][:, :], lhsT=cqT[:, lc, t * 128:(t + 1) * 128], rhs=wqr[:, lc, :], start=(lc == 0), stop=(lc == 3)), r=[(cqT.name, t // 4), wqr.name], w=[PSK[b]])
                  qrr = A.alloc("qrr", [128, 8, 64], BF16)
                  rope(tref(qrr), pref(b), 8, 64, Ref(RMw.t, RMw.o + t * 64, RMw.r), Ref(RMw.t, RMw.o + 512 + t * 64, RMw.r), [PSK[b], RM_OWN.name], [qrr.name])
                  b2 = nb6()
                  for h in range(8):
                      P.pe(lambda e: e.transpose(psb[b2][0:64, h * 128:(h + 1) * 128], qrr[:, h, :], ident_bf[:, :]), r=[qrr.name, ident_bf.name], w=[PSK[b2]])
                  P.act(lambda e: e.activation(out=qrT[0:64, :, t * 128:(t + 1) * 128], in_=psb[b2][0:64, :].rearrange("p (h n) -> p h n", h=8), func=AF.Copy), r=[PSK[b2]], w=[(qrT.name, t // 4)])
                  A.free(qrr)
              A.free(wqr, RM_OWN)
              wqn = A.alloc("wqn", [128, 4, 8, 128], BF16)
              for lc in range(4):
                  P.dma("pool", wqn[:, lc, :, :], bc(w_uq, lc * 128 * 1536, [[1536, 128], [192, 8], [1, 128]]), w=[wqn.name])
              wkv = A.alloc("wkv", [128, 2, 2048], BF16)
              P.dma("pool", wkv[:, :, :], bc(w_ukv, 0, [[2048, 128], [128 * 2048, 2], [1, 2048]]), w=[wkv.name])
              late = []

              def fm_block_thunks(j_src, jd, plus_one):
                  for q in range(4):
                      def th(q=q):
                          buf = load_wada(j_src * 4 + q)
                          def run():
                              bb = next_bank(0, 4)
                              for cc in range(4):
                                  for kc in range(16):
                                      P.pe(lambda e: e.matmul(ps[bb][:, cc * 2:cc * 2 + 2], lhsT=buf[:, kc, cc * 128:(cc + 1) * 128], rhs=sT_bf[:, kc, :], start=(kc == 0), stop=(kc == 15)), r=[buf.name, sT_bf.name], w=[PSK[bb]])
                              dst = modsT[:, jd, q * 4:(q + 1) * 4]
                              src = bc(ps[bb], 0, [[512, 128], [2, 4]])
                              bsrc = badaT_sb[:, j_src * 16 + q * 4: j_src * 16 + q * 4 + 4]
                              P.dve(lambda e: e.scalar_tensor_tensor(out=dst, in0=src, scalar=(1.0 if plus_one else 0.0), in1=bsrc, op0=ALU.add, op1=ALU.add), r=[PSK[bb], badaT_sb.name], w=[modsT.name])
                              A.free(buf)
                          return run
                      late.append(th)

              Gt = {}

              def gate_block_thunks(j_src, name):
                  def alloc_g():
                      G = A.alloc(name, [128, 2048], F32)
                      P.dma("sp", G[:, :], bc(b_ada, j_src * 2048, [[0, 128], [1, 2048]]), w=[G.name])
                      Gt[name] = G
                  for q in range(4):
                      def th(q=q):
                          if name not in Gt:
                              alloc_g()
                          G = Gt[name]
                          buf = load_wada(j_src * 4 + q)
                          def run():
                              bb = next_bank(0, 4)
                              for kc in range(16):
                                  P.pe(lambda e: e.matmul(ps[bb][:, :], lhsT=s_bc[:, kc, :], rhs=buf[:, kc, :], start=(kc == 0), stop=(kc == 15)), r=[s_bc.name, buf.name], w=[PSK[bb]])
                              P.dve(lambda e: e.tensor_tensor(out=G[:, q * 512:(q + 1) * 512], in0=ps[bb][:, :], in1=G[:, q * 512:(q + 1) * 512], op=ALU.add), r=[PSK[bb], G.name], w=[G.name])
                              A.free(buf)
                          return run
                      late.append(th)

              fm_block_thunks(3, 2, False)
              fm_block_thunks(4, 3, True)
              gate_block_thunks(2, "G1")
              gate_block_thunks(5, "G2")
              pending_runs = []

              def late_step(n):
                  runs = list(pending_runs)
                  del pending_runs[:]
                  for _ in range(n):
                      if late:
                          pending_runs.append(late.pop(0)())
                  for r_ in runs:
                      r_()

              PTs = [A.alloc("PTt", [128, 512], BF16) for _ in range(4)]
              ptn = [0]
              kblocks = [(0, 512), (512, 512), (1024, 512), (1536, 512), (2048, 256)]
              hq = 0
              for h in range(8):
                  late_step(1)
                  qnT_h = A.alloc("qnT_h", [128, 1024], BF16)
                  for qb in range(2):
                      b = nb4()
                      for lc in range(4):
                          P.pe(lambda e: e.matmul(ps[b][:, :], lhsT=wqn[:, lc, h, :], rhs=cqT[:, lc, qb * 512:(qb + 1) * 512], start=(lc == 0), stop=(lc == 3)), r=[wqn.name, (cqT.name, qb)], w=[PSK[b]])
                      P.dve(lambda e: e.tensor_copy(out=qnT_h[:, qb * 512:(qb + 1) * 512], in_=ps[b][:, :]), r=[PSK[b]], w=[(qnT_h.name, qb)])
                  knT_h = A.alloc("knT_h", [128, 2304], BF16)
                  for (k0, n) in kblocks:
                      b = nb4()
                      for lc in range(2):
                          P.pe(lambda e: e.matmul(ps[b][:, 0:n], lhsT=wkv[:, lc, h * 256:h * 256 + 128], rhs=ckvT[:, lc, k0:k0 + n], start=(lc == 0), stop=(lc == 1)), r=[wkv.name] + CK(k0, k0 + n), w=[PSK[b]])
                      P.act(lambda e: e.activation(out=knT_h[:, k0:k0 + n], in_=ps[b][:, 0:n], func=AF.Copy), r=[PSK[b]], w=[(knT_h.name, k0 // 512)])
                  v_h = A.alloc("v_h", [128, 18, 128], BF16)
                  for g in range(5):
                      ntl = 4 if g < 4 else 2
                      b = nb4()
                      for j in range(ntl):
                          kt = g * 4 + j
                          for lc in range(2):
                              P.pe(lambda e: e.matmul(ps[b][:, j * 128:(j + 1) * 128], lhsT=ckvT[:, lc, kt * 128:(kt + 1) * 128], rhs=wkv[:, lc, h * 256 + 128:h * 256 + 256], start=(lc == 0), stop=(lc == 1)), r=[wkv.name, (ckvT.name, kt)], w=[PSK[b]])
                      P.dve(lambda e: e.tensor_copy(out=v_h[:, g * 4:g * 4 + ntl, :], in_=ps[b][:, 0:ntl * 128].rearrange("p (j n) -> p j n", j=ntl)), r=[PSK[b]], w=[(v_h.name, g)])
                  for qb in range(2):
                      if qb == 1:
                          late_step(1)
                      bO, bS = (4, 5) if hq % 2 == 0 else (6, 7)
                      hq += 1
                      def qk(kt):
                          b = nb4()
                          P.pe(lambda e: e.matmul(ps[b][:, :], lhsT=knT_h[:, kt * 128:(kt + 1) * 128], rhs=qnT_h[:, qb * 512:(qb + 1) * 512], start=True, stop=False), r=[(knT_h.name, kt // 4), (qnT_h.name, qb)], w=[PSK[b]])
                          P.pe(lambda e: e.matmul(ps[b][:, :], lhsT=kpeT[0:64, kt * 128:(kt + 1) * 128], rhs=qrT[0:64, h, qb * 512:(qb + 1) * 512], start=False, stop=True), r=[(kpeT.name, kt), (qrT.name, qb)], w=[PSK[b]])
                          return b
                      banks = {0: qk(0), 1: qk(1)}
                      for kt in range(18):
                          if kt + 2 < 18:
                              banks[kt + 2] = qk(kt + 2)
                          b = banks.pop(kt)
                          PTt = PTs[ptn[0] % 4]
                          ptn[0] += 1
                          P.act(lambda e: e.activation(out=PTt[:, :], in_=ps[b][:, :], func=AF.Exp, scale=ATT_SCALE), r=[PSK[b]], w=[PTt.name])
                          P.pe(lambda e: e.matmul(ps[bO][:, :], lhsT=v_h[:, kt, :], rhs=PTt[:, :], start=(kt == 0), stop=(kt == 17)), r=[(v_h.name, kt // 4), PTt.name], w=[PSK[bO]])
                          P.pe(lambda e: e.matmul(ps[bS][:, :], lhsT=ones_bf[:, :], rhs=PTt[:, :], start=(kt == 0), stop=(kt == 17)), r=[ones_bf.name, PTt.name], w=[PSK[bS]])
                      rec = A.alloc("rec", [128, 512], F32)
                      P.dve(lambda e: e.reciprocal(out=rec[:, :], in_=ps[bS][:, :]), r=[PSK[bS]], w=[rec.name])
                      P.dve(lambda e: e.tensor_tensor(out=mixT[:, 8 + h, qb * 512:(qb + 1) * 512], in0=ps[bO][:, :], in1=rec[:, :], op=ALU.mult), r=[PSK[bO], rec.name], w=[(mixT.name, 8 + h)])
                      A.free(rec)
                  A.free(qnT_h, knT_h, v_h)
              late_step(0)
              assert not late and not pending_runs
              A.free(wqn, wkv, cqT, qrT, ckvT, kpeT, *PTs)
              G1, G2 = Gt["G1"], Gt["G2"]
              if "att" in tap_out:
                  tap("att", mixT[:, 8:16, 0:256], MK(8, 16), [128, 8, 256])

              P.marks.append(("mods2", sum(1 for x_ in P.ins if x_.eng == "pe")))
              def bcast_row(dram, name):
                  T_ = A.alloc(name, [128, 2048], F32)
                  P.dma("sp", T_[:, :], bc(dram, 0, [[0, 128], [1, 2048]]), w=[T_.name])
                  return T_

              P.marks.append(("F", sum(1 for x_ in P.ins if x_.eng == "pe")))
              acc = A.alloc("acc", [128, 8, 2048], F32)
              wos = [A.alloc("wo", [128, 16, 512], BF16) for _ in range(2)]
              P.dma("pool", wos[0][:, :, :], bc(w_o, 0, [[D, 128], [128 * D, 16], [1, 512]]), w=[wos[0].name])
              def load_xr(i):
                  cb_, t_ = i // 8, i % 8
                  xr_ = A.alloc("xr", [128, 512], F32)
                  P.dma("sp", xr_[:, :], x_own.ap()[t_ * 128:(t_ + 1) * 128, cb_ * 512:(cb_ + 1) * 512], w=[xr_.name])
                  return xr_
              xq = [load_xr(i) for i in range(3)]
              tmp_prev = [None]
              for cb in range(4):
                  wo = wos[cb % 2]
                  if cb < 3:
                      wn = wos[(cb + 1) % 2]
                      P.dma("pool", wn[:, :, :], bc(w_o, (cb + 1) * 512, [[D, 128], [128 * D, 16], [1, 512]]), w=[wn.name])
                  for t in range(8):
                      b = next_bank()
                      for kc in range(16):
                          P.pe(lambda e: e.matmul(ps[b][:, :], lhsT=mixT[:, kc, t * 128:(t + 1) * 128], rhs=wo[:, kc, :], start=(kc == 0), stop=(kc == 15)), r=[(mixT.name, kc), wo.name], w=[PSK[b]])
                      xr = xq.pop(0)
                      nxt_i = cb * 8 + t + 3
                      if nxt_i < 32:
                          xq.append(load_xr(nxt_i))
                      tmp = A.alloc("tmpo", [128, 512], F32)
                      if tmp_prev[0] is not None:
                          A.free(tmp_prev[0])
                      tmp_prev[0] = tmp
                      P.dve(lambda e: e.tensor_tensor(out=tmp[:, :], in0=ps[b][:, :], in1=G1[:, cb * 512:(cb + 1) * 512], op=ALU.mult), r=[PSK[b], G1.name], w=[tmp.name])
                      P.dve(lambda e: e.scalar_tensor_tensor(out=acc[:, t, cb * 512:(cb + 1) * 512], in0=xr[:, :], scalar=ALPHA, in1=tmp[:, :], op0=ALU.mult, op1=ALU.add), r=[xr.name, tmp.name], w=[(acc.name, t)])
                      A.free(xr)
              A.free(mixT, G1, tmp_prev[0], *wos)
              if "pre1" in tap_out:
                  tap("pre1", acc[:, 0, :], [(acc.name, 0)], [128, 2048])
              L1W = bcast_row(ln1_w, "L1W")
              L1B = bcast_row(ln1_b, "L1B")
              h2T = A.alloc("h2T", [128, 16, 1024], BF16)
              hn_old = []
              for blk in range(2):
                  hn2 = A.alloc("hn2", [128, 4, 2048], BF16)
                  for pr in range(2):
                      tl = [blk * 4 + pr * 2, blk * 4 + pr * 2 + 1]
                      hnl = []
                      for t in tl:
                          hn = A.alloc("hn", [128, 2048], F32)
                          hnl.append(hn)
                      for ohn in hn_old:
                          A.free(ohn)
                      hn_old = list(hnl)
                      LSa, st1 = ln_stats_multi([(acc[:, t, :], [(acc.name, t)]) for t in tl])
                      for t, hn, st_ in zip(tl, hnl, st1):
                          ln_apply(st_, acc[:, t, :], [(acc.name, t)], hn[:, :], [hn.name])
                          P.dve(lambda e: e.tensor_tensor(out=hn[:, :], in0=hn[:, :], in1=L1W[:, :], op=ALU.mult), r=[hn.name, L1W.name], w=[hn.name])
                          P.add("pool" if t % 2 == 0 else "dve", lambda e: e.tensor_tensor(out=hn[:, :], in0=hn[:, :], in1=L1B[:, :], op=ALU.add), [hn.name, L1B.name], [hn.name])
                      LSb, st2 = ln_stats_multi([(hn[:, :], [hn.name]) for hn in hnl])
                      for t, hn, st_ in zip(tl, hnl, st2):
                          j = t % 4
                          ln_apply(st_, hn[:, :], [hn.name], hn2[:, j, :], [hn2.name])
                          P.act(lambda e: e.activation(out=acc[:, t, :], in_=hn[:, :], func=AF.Copy, scale=ALPHA), r=[hn.name], w=[(acc.name, t)])
                      A.free(LSa, LSb)
                  transpose_block(hn2, 4, h2T, blk * 512, 3, 2, blk)
                  A.free(hn2)
              A.free(L1W, L1B, *hn_old)
              H2K = [(h2T.name, kc) for kc in range(16)]
              if "h2T" in tap_out:
                  tap("h2T", h2T[:, :, 0:256], H2K, [128, 16, 256])

              P.marks.append(("G", sum(1 for x_ in P.ins if x_.eng == "pe")))
              wr = A.alloc("wr", [128, 16, 36], BF16)
              P.dma("pool", wr[:, :, :], bc(w_rt, 0, [[36, 128], [128 * 36, 16], [1, 36]]), w=[wr.name])
              brt = A.alloc("brt", [128, 36], F32)
              P.dma("sp", brt[:, :], bc(b_rt, 0, [[0, 128], [1, 36]]), w=[brt.name])
              LGT = A.alloc("LGT", [128, 8, 36], F32)
              for t in range(8):
                  b = next_bank()
                  for kc in range(16):
                      P.pe(lambda e: e.matmul(ps[b][:, 0:36], lhsT=h2T[:, kc, t * 128:(t + 1) * 128], rhs=wr[:, kc, :], start=(kc == 0), stop=(kc == 15)), r=[(h2T.name, kc), wr.name], w=[PSK[b]])
                  P.dve(lambda e: e.tensor_tensor(out=LGT[:, t, :], in0=ps[b][:, 0:36], in1=brt[:, :], op=ALU.add), r=[PSK[b], brt.name], w=[LGT.name])
              RT = A.alloc("RT", [128, 1024], F32)
              RTr = tref(RT)
              LGr = tref(LGT)
              def rv(off, dims):
                  return RTr.v(off, dims)
              o_gmax, o_ohg, o_eg, o_gsum, o_gprob, o_tmpe, o_esel, o_m1, o_oh1, o_e2, o_m2, o_oh2, o_dd, o_w1, o_w2, o_t1, o_t2, o_ew = (
                  0, 8, 40, 72, 80, 88, 344, 408, 416, 480, 544, 552, 616, 624, 632, 640, 704, 768)
              GATE = A.alloc("GATE", [128, 8, 32], F32)
              GL = LGr.v(0, [[36, 8], [1, 4]])
              EL = LGr.v(4, [[36, 8], [8, 4], [1, 8]])
              RK = [RT.name]
              def rdve(fn, extra_r=()):
                  P.dve(fn, r=RK + list(extra_r), w=RK)
              rdve(lambda e: e.tensor_reduce(out=rv(o_gmax, [[1, 8]]), in_=GL, axis=AX.X, op=ALU.max), [LGT.name])
              rdve(lambda e: e.tensor_tensor(out=rv(o_ohg, [[4, 8], [1, 4]]), in0=GL, in1=rv(o_gmax, [[1, 8], [0, 4]]), op=ALU.is_equal), [LGT.name])
              rdve(lambda e: e.tensor_tensor(out=rv(o_eg, [[4, 8], [1, 4]]), in0=GL, in1=rv(o_gmax, [[1, 8], [0, 4]]), op=ALU.subtract), [LGT.name])
              P.act(lambda e: e.activation(out=rv(o_eg, [[1, 32]]), in_=rv(o_eg, [[1, 32]]), func=AF.Exp), r=RK, w=RK)
              rdve(lambda e: e.tensor_reduce(out=rv(o_gsum, [[1, 8]]), in_=rv(o_eg, [[4, 8], [1, 4]]), axis=AX.X, op=ALU.add))
              rdve(lambda e: e.reciprocal(out=rv(o_gprob, [[1, 8]]), in_=rv(o_gsum, [[1, 8]])))
              rdve(lambda e: e.tensor_tensor(out=rv(o_tmpe, [[32, 8], [8, 4], [1, 8]]), in0=EL, in1=rv(o_ohg, [[4, 8], [1, 4], [0, 8]]), op=ALU.mult), [LGT.name])
              rdve(lambda e: e.tensor_reduce(out=rv(o_esel, [[8, 8], [1, 8]]), in_=rv(o_tmpe, [[32, 8], [1, 8], [8, 4]]), axis=AX.X, op=ALU.add))
              rdve(lambda e: e.tensor_reduce(out=rv(o_m1, [[1, 8]]), in_=rv(o_esel, [[8, 8], [1, 8]]), axis=AX.X, op=ALU.max))
              rdve(lambda e: e.tensor_tensor(out=rv(o_oh1, [[8, 8], [1, 8]]), in0=rv(o_esel, [[8, 8], [1, 8]]), in1=rv(o_m1, [[1, 8], [0, 8]]), op=ALU.is_equal))
              rdve(lambda e: e.scalar_tensor_tensor(out=rv(o_e2, [[1, 64]]), in0=rv(o_oh1, [[1, 64]]), scalar=-1.0e30, in1=rv(o_esel, [[1, 64]]), op0=ALU.mult, op1=ALU.add))
              rdve(lambda e: e.tensor_reduce(out=rv(o_m2, [[1, 8]]), in_=rv(o_e2, [[8, 8], [1, 8]]), axis=AX.X, op=ALU.max))
              rdve(lambda e: e.tensor_tensor(out=rv(o_oh2, [[8, 8], [1, 8]]), in0=rv(o_e2, [[8, 8], [1, 8]]), in1=rv(o_m2, [[1, 8], [0, 8]]), op=ALU.is_equal))
              rdve(lambda e: e.tensor_tensor(out=rv(o_dd, [[1, 8]]), in0=rv(o_m2, [[1, 8]]), in1=rv(o_m1, [[1, 8]]), op=ALU.subtract))
              P.act(lambda e: e.activation(out=rv(o_dd, [[1, 8]]), in_=rv(o_dd, [[1, 8]]), func=AF.Exp), r=RK, w=RK)
              rdve(lambda e: e.tensor_scalar(out=rv(o_w1, [[1, 8]]), in0=rv(o_dd, [[1, 8]]), scalar1=1.0, scalar2=None, op0=ALU.add))
              rdve(lambda e: e.reciprocal(out=rv(o_w1, [[1, 8]]), in_=rv(o_w1, [[1, 8]])))
              rdve(lambda e: e.tensor_tensor(out=rv(o_w2, [[1, 8]]), in0=rv(o_dd, [[1, 8]]), in1=rv(o_w1, [[1, 8]]), op=ALU.mult))
              rdve(lambda e: e.tensor_tensor(out=rv(o_w1, [[1, 8]]), in0=rv(o_w1, [[1, 8]]), in1=rv(o_gprob, [[1, 8]]), op=ALU.mult))
              rdve(lambda e: e.tensor_tensor(out=rv(o_w2, [[1, 8]]), in0=rv(o_w2, [[1, 8]]), in1=rv(o_gprob, [[1, 8]]), op=ALU.mult))
              rdve(lambda e: e.tensor_tensor(out=rv(o_t1, [[8, 8], [1, 8]]), in0=rv(o_oh1, [[8, 8], [1, 8]]), in1=rv(o_w1, [[1, 8], [0, 8]]), op=ALU.mult))
              rdve(lambda e: e.tensor_tensor(out=rv(o_t2, [[8, 8], [1, 8]]), in0=rv(o_oh2, [[8, 8], [1, 8]]), in1=rv(o_w2, [[1, 8], [0, 8]]), op=ALU.mult))
              rdve(lambda e: e.tensor_tensor(out=rv(o_ew, [[1, 64]]), in0=rv(o_t1, [[1, 64]]), in1=rv(o_t2, [[1, 64]]), op=ALU.add))
              P.dve(lambda e: e.tensor_tensor(out=tref(GATE).v(0, [[32, 8], [8, 4], [1, 8]]), in0=rv(o_ohg, [[4, 8], [1, 4], [0, 8]]), in1=rv(o_ew, [[8, 8], [0, 4], [1, 8]]), op=ALU.mult), r=RK, w=[GATE.name])
              A.free(wr, brt, LGT, RT)
              if "gate" in tap_out:
                  tap("gate", GATE[:, :, :], [GATE.name], [128, 8, 32])

              P.marks.append(("H", sum(1 for x_ in P.ins if x_.eng == "pe")))
              NEXP = int(os.environ.get("DBG_NEXP", "32")) if n_exp else 0
              items = []
              for ge in range(NEXP):
                  for fh in range(2):
                      items.append(("gu", ge, fh))
                  items.append(("wd", ge, 0))
              loaded = {}

              def load_item(i):
                  kind, ge, fh = items[i]
                  if kind == "gu":
                      wg = A.alloc("wg", [128, 16, 256], BF16)
                      wu = A.alloc("wu", [128, 16, 256], BF16)
                      P.dma("pool", wg[:, :, :], bc(ew_gate, ge * D * 512 + fh * 256, [[512, 128], [128 * 512, 16], [1, 256]]), w=[wg.name])
                      P.dma("pool", wu[:, :, :], bc(ew_up, ge * D * 512 + fh * 256, [[512, 128], [128 * 512, 16], [1, 256]]), w=[wu.name])
                      loaded[i] = (wg, wu)
                  else:
                      wd = A.alloc("wd", [128, 4, 2048], BF16)
                      P.dma("pool", wd[:, :, :], bc(ew_down, ge * 512 * D, [[D, 128], [128 * D, 4], [1, 2048]]), w=[wd.name])
                      P.pool(lambda e: e.tensor_tensor(out=wd[:, :, :], in0=wd[:, :, :], in1=tref(G2).v(0, [[0, 4], [1, 2048]]), op=ALU.mult), r=[wd.name, G2.name], w=[wd.name])
                      loaded[i] = (wd,)

              def get_item(i):
                  if i not in loaded:
                      load_item(i)
                  if i + 1 < len(items) and (i + 1) not in loaded:
                      load_item(i + 1)
                  return loaded[i]

              ii = 0
              for ge in range(NEXP):
                  hid = A.alloc("hid", [128, 4, 1024], BF16)
                  for fh in range(2):
                      wg, wu = get_item(ii)
                      for fq in range(2):
                          fc = fh * 2 + fq
                          for tb in range(2):
                              bg = next_bank()
                              for kc in range(16):
                                  P.pe(lambda e: e.matmul(ps[bg][:, :], lhsT=wg[:, kc, fq * 128:(fq + 1) * 128], rhs=h2T[:, kc, tb * 512:(tb + 1) * 512], start=(kc == 0), stop=(kc == 15)), r=[wg.name, (h2T.name, kc)], w=[PSK[bg]])
                              bu = next_bank()
                              for kc in range(16):
                                  P.pe(lambda e: e.matmul(ps[bu][:, :], lhsT=wu[:, kc, fq * 128:(fq + 1) * 128], rhs=h2T[:, kc, tb * 512:(tb + 1) * 512], start=(kc == 0), stop=(kc == 15)), r=[wu.name, (h2T.name, kc)], w=[PSK[bu]])
                              sgt = A.alloc("sgt", [128, 512], F32)
                              P.act(lambda e: e.activation(out=sgt[:, :], in_=ps[bg][:, :], func=AF.Silu), r=[PSK[bg]], w=[sgt.name])
                              P.dve(lambda e: e.tensor_tensor(out=hid[:, fc, tb * 512:(tb + 1) * 512], in0=ps[bu][:, :], in1=sgt[:, :], op=ALU.mult), r=[PSK[bu], sgt.name], w=[(hid.name, fc)])
                              A.free(sgt)
                      A.free(wg, wu)
                      del loaded[ii]
                      ii += 1
                  (wd,) = get_item(ii)
                  for t in range(8):
                      for nbk in range(4):
                          b = next_bank()
                          for fc in range(4):
                              P.pe(lambda e: e.matmul(ps[b][:, :], lhsT=hid[:, fc, t * 128:(t + 1) * 128], rhs=wd[:, fc, nbk * 512:(nbk + 1) * 512], start=(fc == 0), stop=(fc == 3)), r=[(hid.name, fc), wd.name], w=[PSK[b]])
                          P.dve(lambda e: e.scalar_tensor_tensor(out=acc[:, t, nbk * 512:(nbk + 1) * 512], in0=ps[b][:, :], scalar=GATE[:, t, ge:ge + 1], in1=acc[:, t, nbk * 512:(nbk + 1) * 512], op0=ALU.mult, op1=ALU.add),
                                r=[PSK[b], GATE.name, (acc.name, t)], w=[(acc.name, t)])
                  A.free(wd, hid)
                  del loaded[ii]
                  ii += 1
              A.free(h2T, G2)

              P.marks.append(("LN2", sum(1 for x_ in P.ins if x_.eng == "pe")))
              L2W = bcast_row(ln2_w, "L2W")
              L2B = bcast_row(ln2_b, "L2B")
              hns = [A.alloc("hn", [128, 2048], F32) for _ in range(3)]
              LS2, sts = ln_stats_multi([(acc[:, t, :], [(acc.name, t)]) for t in range(8)])
              for t in range(8):
                  hn = hns[t % 3]
                  ln_apply(sts[t], acc[:, t, :], [(acc.name, t)], hn[:, :], [hn.name])
                  P.dve(lambda e: e.tensor_tensor(out=hn[:, :], in0=hn[:, :], in1=L2W[:, :], op=ALU.mult), r=[hn.name, L2W.name], w=[hn.name])
                  P.add("pool" if t % 2 == 0 else "dve", lambda e: e.tensor_tensor(out=hn[:, :], in0=hn[:, :], in1=L2B[:, :], op=ALU.add), [hn.name, L2B.name], [hn.name])
                  P.dma("sp", out.ap()[t * 128:(t + 1) * 128, :], hn[:, :], r=[hn.name], w=[("out", t)])
                  out_keys.append(("out", t))

        P.add("sp", None, reads=out_keys)
        P.finalize(sems, dsems, {"act": FEN[:, 0:1], "dve": FEN[:, 1:2], "pool": FEN[:, 2:3]} if not os.environ.get("DBG_NOFENCE2") else None)
        import collections
        print("MARKS", P.marks, flush=True)
        print("instr counts", collections.Counter(x.eng for x in P.ins), "signals", collections.Counter(x.eng for x in P.ins if x.signal and not x.dma), "arena peak", A.peak, flush=True)
        with nc.Block() as block0:
            @block0.sync
            def _(e):
                for s_ in list(sems.values()) + [x_ for l_ in dsems.values() for x_ in l_]:
                    e.sem_clear(s_)

        with nc.Block() as block:
            @block.tensor
            def _(e):
                P.emit("pe", e)

            @block.scalar
            def _(e):
                P.emit("act", e)

            @block.vector
            def _(e):
                P.emit("dve", e)

            @block.gpsimd
            def _(e):
                P.emit("pool", e)

            @block.sync
            def _(e):
                P.emit("sp", e)
    return nc


def _rope_tables(pos, dim):
    half = dim // 2
    inv = 10000.0 ** (-np.arange(0, half, 2, dtype=np.float32) / np.float32(half))
    inv = inv.astype(np.float32)
    rows = (pos // GRID_W).astype(np.float32)
    cols = (pos % GRID_W).astype(np.float32)
    ar = (rows[:, None] * inv[None, :]).astype(np.float32)
    ac = (cols[:, None] * inv[None, :]).astype(np.float32)
    cr, sr, cc, sc = np.cos(ar), np.sin(ar), np.cos(ac), np.sin(ac)
    cosx = np.concatenate([cr, cr, cc, cc], 1).astype(np.float32)
    sinx = np.concatenate([-sr, sr, -sc, sc], 1).astype(np.float32)
    return cosx, sinx


_PERM64 = np.concatenate([np.arange(0, 16), np.arange(32, 48), np.arange(16, 32), np.arange(48, 64)])


def _tile_major(a):
    n = a.shape[0] // 128
    return np.ascontiguousarray(a.reshape(n, 128, a.shape[1]).transpose(1, 0, 2).reshape(128, -1))


def _core_tables(hf):
    own = np.arange(NTOK) + hf * NTOK
    oth = np.arange(NTOK) + (1 - hf) * NTOK
    t = {}
    for nm, pos in (("own", own), ("oth", oth)):
        c, s = _rope_tables(pos, 128)
        t["rope_r_" + nm] = np.concatenate([_tile_major(c), _tile_major(s)], 1)
        c, s = _rope_tables(pos, 64)
        c, s = c[:, _PERM64], s[:, _PERM64]
        t["rope_m_" + nm] = np.concatenate([_tile_major(c), _tile_major(s)], 1)
    tb = np.zeros((128, NTAB), np.float32)
    sidx = np.arange(128, dtype=np.float32)[:, None]
    cidx = np.arange(128, dtype=np.float32)[None, :]
    tb[:, T_D1:T_D1 + 128] = np.maximum(cidx - sidx, 0)
    tb[:, T_MK1:T_MK1 + 128] = (cidx >= sidx)
    tb[:, T_D2:T_D2 + 128] = np.maximum(sidx - cidx, 0)
    tb[:, T_MK2:T_MK2 + 128] = (sidx >= cidx)
    tb[:, T_CP1:T_CP1 + 128] = cidx + 1
    tb[:, T_C128:T_C128 + 128] = 128 - cidx
    tb[:, T_Z1] = 127 - sidx[:, 0]
    tb[:, T_Z2] = sidx[:, 0]
    j = np.arange(NTOK, dtype=np.float32)
    m = np.arange(CTX, dtype=np.float32)
    if hf == 0:
        ef_o, mf_o = np.zeros(NTOK), np.zeros(NTOK)
        ef_c, mf_c = CTX - 1 - m, np.ones(CTX)
        eb_o, mb_o = j, np.ones(NTOK)
        eb_c, mb_c = NTOK + m, np.ones(CTX)
    else:
        ef_o, mf_o = NTOK - 1 - j, np.ones(NTOK)
        ef_c, mf_c = NTOK + CTX - 1 - m, np.ones(CTX)
        eb_o, mb_o = np.zeros(NTOK), np.zeros(NTOK)
        eb_c, mb_c = m, np.ones(CTX)
    def tm(o, c):
        return np.concatenate([o.reshape(8, 128).T, c.reshape(2, 128).T], 1)
    tb[:, T_EF:T_EF + 10] = tm(ef_o, ef_c)
    tb[:, T_MF:T_MF + 10] = tm(mf_o, mf_c)
    tb[:, T_EB:T_EB + 10] = tm(eb_o, eb_c)
    tb[:, T_MB:T_MB + 10] = tm(mb_o, mb_c)
    t["tabs"] = tb
    return t


def _colT(v, n):
    return np.ascontiguousarray(np.asarray(v, np.float32).reshape(n, 128).T)


def prep(inputs, cores=None, n_exp=32):
    f = lambda k: np.asarray(inputs[k], np.float32)
    x, c, ctx, c_ctx = f("x"), f("c"), f("ctx"), f("c_ctx")
    shared = {
        "w_ada": np.ascontiguousarray(f("w_ada")[0]),
        "b_ada": np.ascontiguousarray(f("b_ada")[0][None]),
        "badaT": _colT(f("b_ada")[0], 96),
        "w_in": np.ascontiguousarray(f("w_in")[0]),
        "ret_decay": np.ascontiguousarray(f("ret_decay")[0].reshape(1, 16)),
        "ret_gn_w": np.ascontiguousarray(f("ret_gn_w")[0][None]),
        "qnT": _colT(f("mla_q_norm")[0], 4),
        "kvnT": _colT(f("mla_kv_norm")[0], 2),
        "w_uq": np.ascontiguousarray(f("w_uq")[0]),
        "w_ckp": np.ascontiguousarray(np.concatenate([f("w_in")[0][:, 4608:4864], f("w_in")[0][:, 4864:4928][:, _PERM64]], 1)),
        "w_uqr": np.ascontiguousarray(np.concatenate(
            [f("w_uq")[0][:, h * 192 + 128:h * 192 + 192][:, _PERM64] for h in range(8)], 1)),
        "w_ukv": np.ascontiguousarray(f("w_ukv")[0]),
        "w_o": np.ascontiguousarray(f("w_o")[0]),
        "ln1_w": f("ln1_w")[0][None], "ln1_b": f("ln1_b")[0][None],
        "ln2_w": f("ln2_w")[0][None], "ln2_b": f("ln2_b")[0][None],
        "w_rt": np.ascontiguousarray(np.concatenate(
            [f("router_group_w")[0]] + [f("router_expert_w")[0][g] for g in range(4)], 1)),
        "b_rt": np.ascontiguousarray(np.concatenate(
            [f("router_group_b")[0].reshape(-1), f("router_expert_b")[0].reshape(-1)])[None]),
        "ew_gate": f("expert_w_gate")[0].reshape(32, D, 512),
        "ew_up": f("expert_w_up")[0].reshape(32, D, 512),
        "ew_down": f("expert_w_down")[0].reshape(32, 512, D),
        "ident": np.eye(128, dtype=np.float32),
    }
    if not n_exp:
        for k in ("ew_gate", "ew_up", "ew_down"):
            del shared[k]
    tables = [_core_tables(0), _core_tables(1)]
    maps = []
    for core in (range(8) if cores is None else cores):
        b, hf = core // 2, core % 2
        m = dict(shared)
        m["x_own"] = np.ascontiguousarray(x[b, hf * NTOK:(hf + 1) * NTOK])
        m["x_oth"] = np.ascontiguousarray(x[b, (1 - hf) * NTOK:(2 - hf) * NTOK])
        m["ctx"] = np.ascontiguousarray(ctx[b])
        cv = np.stack([c[b], c_ctx], 0)
        m["cT"] = np.ascontiguousarray(cv.reshape(2, 16, 128).transpose(2, 1, 0).reshape(128, 32))
        m.update(tables[hf])
        maps.append(m)
    return maps


_NC_CACHE = {}


def kernel(**inputs):
    maps = prep(inputs)
    if "nc" not in _NC_CACHE:
        _NC_CACHE["nc"] = build()
    res = run_bass_kernel_spmd(_NC_CACHE["nc"], maps, core_ids=list(range(8)))
    outp = np.empty((4, 2048, D), np.float32)
    for core in range(8):
        b, hf = core // 2, core % 2
        outp[b, hf * NTOK:(hf + 1) * NTOK] = res.results[core]["out"]
    return outp
```

```python
import math
import os
from contextlib import ExitStack

import numpy as np
import concourse.bass as bass
import concourse.mybir as mybir
from concourse.bass_utils import run_bass_kernel_spmd

F32 = mybir.dt.float32
BF16 = mybir.dt.bfloat16
AF = mybir.ActivationFunctionType
ALU = mybir.AluOpType
AX = mybir.AxisListType

D = 2048
NTOK = 1024
NT = 8
GRID_W = 64
CTX = 256
IN_W = 4928
ALPHA = 2.0 ** 0.25
EPS = 1e-6
K_SCALE = 128.0 ** -0.5
ATT_SCALE = 192.0 ** -0.5
COMPUTE = ("pe", "act", "dve", "pool")
NDMA_SEM = 24
ARENA_KB = 200
T_D1, T_MK1, T_D2, T_MK2, T_CP1, T_C128, T_Z1, T_Z2, T_EF, T_MF, T_EB, T_MB = (
    0, 128, 256, 384, 512, 640, 768, 769, 770, 780, 790, 800)
NTAB = 810


class _I:
    __slots__ = ("eng", "fn", "dma", "deps", "signal", "sem", "val", "waits")

    def __init__(self, eng, fn, dma):
        self.eng = eng
        self.fn = fn
        self.dma = dma
        self.deps = ()
        self.signal = False
        self.sem = None
        self.val = 0
        self.waits = ()


class _Rec:
    def __init__(self):
        self.calls = []

    def __getattr__(self, name):
        def f(*args, **kw):
            self.calls.append((name, args, kw))
            return None
        return f


class Prog:
    def __init__(self):
        self.marks = []
        self.ins = []
        self.last_w = {}
        self.readers = {}
        self.seen = set()
        self.tile_pending = {}
        self.tile_acc = {}

    def _touch(self, k, i):
        base = k if isinstance(k, str) else k[0]
        if k not in self.seen:
            self.seen.add(k)
            p = self.tile_pending.get(base)
            if p:
                self.readers[k] = list(p)
        acc = self.tile_acc.setdefault(base, {"c": {}, "d": set()})
        x = self.ins[i] if i < len(self.ins) else None
        return acc

    def add(self, eng, fn, reads=(), writes=(), dma=False):
        if fn is not None:
            rec = _Rec()
            fn(rec)
            assert len(rec.calls) == 1
            fn = rec.calls[0]
        i = len(self.ins)
        ins = _I(eng, fn, dma)
        self.ins.append(ins)
        for k in tuple(reads) + tuple(writes):
            acc = self._touch(k, i)
            if dma:
                acc["d"].add(i)
            else:
                acc["c"][eng] = i
        deps = set()
        for k in reads:
            w = self.last_w.get(k)
            if w is not None:
                deps.add(w)
        for k in writes:
            w = self.last_w.get(k)
            if w is not None:
                deps.add(w)
            deps.update(self.readers.get(k, ()))
        for k in reads:
            self.readers.setdefault(k, []).append(i)
        for k in writes:
            self.last_w[k] = i
            self.readers[k] = []
        deps.discard(i)
        ins.deps = deps
        return i

    def pe(self, fn, r=(), w=()):
        return self.add("pe", fn, r, w)

    def act(self, fn, r=(), w=()):
        return self.add("act", fn, r, w)

    def dve(self, fn, r=(), w=()):
        return self.add("dve", fn, r, w)

    def pool(self, fn, r=(), w=()):
        return self.add("pool", fn, r, w)

    def dma(self, q, out, in_, r=(), w=()):
        return self.add(q, lambda e: e.dma_start(out=out, in_=in_), r, w, dma=True)

    def finalize(self, sems, dma_sems, fence_aps=None):
        ins = self.ins

        def prune(x):
            best = {}
            dmas = []
            for d in x.deps:
                p = ins[d]
                if p.dma:
                    dmas.append(d)
                else:
                    if p.eng == "pe" and x.eng == "pe" and not x.dma:
                        continue
                    if p.eng not in best or best[p.eng] < d:
                        best[p.eng] = d
            return sorted(list(best.values()) + dmas)

        for x in ins:
            x.deps = prune(x)
        if fence_aps and os.environ.get("K_FENCE"):
            F = set()
            for x in ins:
                if x.eng == "pe" and not x.dma:
                    for d in x.deps:
                        p = ins[d]
                        if not p.dma and p.eng in fence_aps:
                            F.add(d)
            new = []
            remap = {}
            fence_of = {}
            for i, x in enumerate(ins):
                remap[i] = len(new)
                new.append(x)
                if i in F:
                    if x.eng == "act":
                        f = _I(x.eng, ("activation", (), dict(out=fence_aps[x.eng], in_=fence_aps[x.eng], func=AF.Copy)), False)
                    else:
                        f = _I(x.eng, ("memset", (fence_aps[x.eng], 0.0), {}), False)
                    f.deps = [i]
                    fence_of[i] = len(new)
                    new.append(f)
            for j, x in enumerate(new):
                is_pe = (x.eng == "pe" and not x.dma)
                nd = []
                for d in x.deps:
                    if is_pe and d in fence_of:
                        nd.append(fence_of[d])
                    else:
                        nd.append(remap[d])
                x.deps = nd
            self.ins = ins = new
        prev = {}
        for i, x in enumerate(ins):
            if x.dma or x.fn is None or x.eng == "pe" or not os.environ.get("K_CHAIN"):
                continue
            p = prev.get(x.eng)
            if p is not None:
                x.deps = sorted(set(x.deps) | {p})
            prev[x.eng] = i
        for x in ins:
            x.deps = prune(x)
            for d in x.deps:
                ins[d].signal = True
        cnt = {e: 0 for e in COMPUTE}
        dcnt = {q: 0 for q in dma_sems}
        for x in ins:
            if x.dma:
                j = dcnt[x.eng]
                dcnt[x.eng] += 1
                pool = dma_sems[x.eng]
                x.sem = pool[j % len(pool)]
                x.val = 16 * (j // len(pool) + 1)
                x.signal = True
            elif x.signal:
                cnt[x.eng] += 1
                x.sem = sems[x.eng]
                x.val = cnt[x.eng]
        waited = {}
        for x in ins:
            ws = []
            if x.dma and x.val > 16:
                ws.append((x.sem, x.val - 16))
            for d in x.deps:
                p = ins[d]
                ws.append((p.sem, p.val))
            out = []
            for sem, val in ws:
                key = (x.eng, id(sem))
                if waited.get(key, 0) >= val:
                    continue
                waited[key] = val
                out.append((sem, val))
            x.waits = out

    def emit(self, eng_name, e):
        for x in self.ins:
            if x.eng != eng_name:
                continue
            for sem, val in x.waits:
                e.wait_ge(sem, val)
            if x.fn is None:
                continue
            name, args, kw = x.fn
            bi = getattr(e, name)(*args, **kw)
            if x.signal:
                bi.then_inc(x.sem, 16 if x.dma else 1)


class Tile:
    def __init__(self, name, ap, start, end):
        self.name = name
        self.ap = ap
        self.start = start
        self.end = end

    def __getitem__(self, key):
        return self.ap[key]


class Arena:
    def __init__(self, prog, a32, a16, nbytes):
        self.P = prog
        self.a32 = a32
        self.a16 = a16
        self.nbytes = nbytes
        self.used = []
        self.freed = []
        self.n = 0
        self.peak = 0

    def alloc(self, name, shape, dt, parts=None):
        esz = 4 if dt == F32 else 2
        free = 1
        for s in shape[1:]:
            free *= s
        nb = (free * esz + 511) // 512 * 512
        self.used.sort()
        pos = 0
        for (s, e, _) in self.used:
            if s - pos >= nb:
                break
            pos = max(pos, e)
        if pos + nb > self.nbytes:
            raise RuntimeError(f"arena full allocating {name} ({nb} B); used={[(u[2], u[1]-u[0]) for u in self.used]}")
        self.n += 1
        uname = f"{name}#{self.n}"
        self.used.append((pos, pos + nb, uname))
        self.peak = max(self.peak, pos + nb)
        pend = {}
        dm = set()
        keep = []
        for (s, e, acc) in self.freed:
            if s < pos + nb and e > pos:
                for k, v in acc["c"].items():
                    if pend.get(k, -1) < v:
                        pend[k] = v
                dm |= acc["d"]
                if s >= pos and e <= pos + nb:
                    continue
            keep.append((s, e, acc))
        self.freed = keep
        self.P.tile_pending[uname] = list(pend.values()) + sorted(dm)
        handle = self.a32 if dt == F32 else self.a16
        rowlen = self.nbytes // esz
        dims = [[rowlen, shape[0]]]
        st = free
        for s in shape[1:]:
            st //= s
            dims.append([st, s])
        ap = bass.AP(handle, pos // esz, dims)
        return Tile(uname, ap, pos, pos + nb)

    def free(self, *tiles):
        for t in tiles:
            self.used = [u for u in self.used if u[2] != t.name]
            acc = self.P.tile_acc.get(t.name, {"c": {}, "d": set()})
            self.freed.append((t.start, t.end, {"c": dict(acc["c"]), "d": set(acc["d"])}))


def bc(ap_tensor, offset, dims):
    return bass.AP(ap_tensor, offset, [list(d) for d in dims])


def build(stop=None, taps=(), n_exp=32):
    nc = bass.Bass("TRN2", target_bir_lowering=False)
    P = Prog()

    def din(name, shape):
        return nc.dram_tensor(name, list(shape), F32, kind="ExternalInput")

    x_own = din("x_own", [NTOK, D])
    x_oth = din("x_oth", [NTOK, D])
    ctx_in = din("ctx", [CTX, D])
    cT = din("cT", [128, 32])
    w_ada = din("w_ada", [D, 6 * D])
    badaT = din("badaT", [128, 96])
    b_ada = din("b_ada", [1, 6 * D])
    w_in = din("w_in", [D, IN_W])
    ret_decay = din("ret_decay", [1, 16])
    ret_gn_w = din("ret_gn_w", [1, 1024])
    qnT = din("qnT", [128, 4])
    kvnT = din("kvnT", [128, 2])
    w_uq = din("w_uq", [512, 1536])
    w_ckp = din("w_ckp", [D, 320])
    w_uqr = din("w_uqr", [512, 512])
    w_ukv = din("w_ukv", [256, 2048])
    w_o = din("w_o", [D, D])
    ln1_w = din("ln1_w", [1, D])
    ln1_b = din("ln1_b", [1, D])
    ln2_w = din("ln2_w", [1, D])
    ln2_b = din("ln2_b", [1, D])
    w_rt = din("w_rt", [D, 36])
    b_rt = din("b_rt", [1, 36])
    if n_exp:
        ew_gate = din("ew_gate", [32, D, 512])
        ew_up = din("ew_up", [32, D, 512])
        ew_down = din("ew_down", [32, 512, D])
    ident_in = din("ident", [128, 128])
    rope_r_own = din("rope_r_own", [128, 2048])
    rope_r_oth = din("rope_r_oth", [128, 2048])
    rope_m_own = din("rope_m_own", [128, 1024])
    rope_m_oth = din("rope_m_oth", [128, 1024])
    tabs = din("tabs", [128, NTAB])
    out = nc.dram_tensor("out", [NTOK, D], F32, kind="ExternalOutput")
    tap_out = {}
    taps = list(taps)
    if any(t[0] == "xmTo" for t in taps):
        taps += [("xmTo%d" % q4, [128, 16, 256]) for q4 in range(4)]
    for (tname, tshape) in taps:
        tap_out[tname] = nc.dram_tensor("tap_" + tname, [tshape[0], int(np.prod(tshape[1:]))], F32, kind="ExternalOutput")

    st = ExitStack()
    with st:
        a32 = st.enter_context(nc.sbuf_tensor("arena", [128, ARENA_KB * 256], F32))
        a16 = a32.bitcast(BF16)
        ps = [st.enter_context(nc.psum_tensor(f"ps{i}", [128, 512], F32)) for i in range(8)]
        psb = [p.bitcast(BF16) for p in ps]
        sems = {e: st.enter_context(nc.semaphore(f"s_{e}")) for e in COMPUTE}
        dsems = {q: [st.enter_context(nc.semaphore(f"d_{q}{i}")) for i in range(NDMA_SEM)] for q in ("sp", "pool")}
        A = Arena(P, a32, a16, ARENA_KB * 1024)
        PSK = [f"ps{i}" for i in range(8)]
        out_keys = []

        def tap(name, src_ap, rkeys, shape):
            if name not in tap_out:
                return
            t = A.alloc("tapst", list(shape), F32)
            idx = tuple([slice(0, shape[0])] + [slice(None)] * (len(shape) - 1))
            P.dve(lambda e: e.tensor_copy(out=t[idx], in_=src_ap), r=rkeys, w=[t.name])
            flat = 1
            for d_ in shape[1:]:
                flat *= d_
            src = bass.AP(t.ap.tensor, t.ap.offset, [[t.ap.ap[0][0], shape[0]], [1, flat]])
            P.dma("sp", tap_out[name].ap(), src, r=[t.name], w=["tap_" + name])
            out_keys.append("tap_" + name)
            A.free(t)

        FEN = A.alloc("fence", [128, 4], F32)
        ident_bf = A.alloc("ident_bf", [128, 128], BF16)
        ident_f = A.alloc("ident_f", [128, 128], F32)
        ones_bf = A.alloc("ones_bf", [128, 128], BF16)
        TB = A.alloc("tabs", [128, NTAB], F32)
        P.dma("pool", ident_bf[:, :], ident_in.ap(), w=[ident_bf.name])
        P.dma("sp", ident_f[:, :], ident_in.ap(), w=[ident_f.name])
        P.dma("sp", TB[:, :], tabs.ap(), w=[TB.name])
        P.dve(lambda e: e.memset(ones_bf[:, :], 1.0), w=[ones_bf.name])
        kvn = A.alloc("kvn", [128, 128], F32)
        qn = A.alloc("qn", [128, 128], F32)
        P.dma("sp", kvn[:, 0:2], kvnT.ap(), w=[kvn.name])
        P.dma("sp", qn[:, 0:4], qnT.ap(), w=[qn.name])

        cT_sb = A.alloc("cT", [128, 32], F32)
        sT = A.alloc("sT", [128, 32], F32)
        sT_bf = A.alloc("sT_bf", [128, 16, 2], BF16)
        s_bc = A.alloc("s_bc", [128, 16, 128], BF16)
        modsT = A.alloc("modsT", [128, 8, 16], F32)
        badaT_sb = A.alloc("badaT", [128, 96], F32)
        P.dma("sp", cT_sb[:, :], cT.ap(), w=[cT_sb.name])
        P.dma("sp", badaT_sb[:, :], badaT.ap(), w=[badaT_sb.name])
        P.act(lambda e: e.activation(out=sT[:, :], in_=cT_sb[:, :], func=AF.Silu), r=[cT_sb.name], w=[sT.name])
        P.dve(lambda e: e.tensor_copy(out=sT_bf[:, :, :], in_=sT[:, :].rearrange("p (k r) -> p k r", r=2)), r=[sT.name], w=[sT_bf.name])
        P.dve(lambda e: e.tensor_copy(out=s_bc[:, :, :], in_=bc(sT_bf.ap.tensor, sT_bf.ap.offset, [[A.nbytes // 2, 128], [2, 16], [0, 128]])),
              r=[sT_bf.name], w=[s_bc.name])

        def load_wada(blk):
            buf = A.alloc("wada", [128, 16, 512], BF16)
            src = bc(w_ada, blk * 512, [[6 * D, 128], [128 * 6 * D, 16], [1, 512]])
            P.dma("pool", buf[:, :, :], src, w=[buf.name])
            return buf

        def mods_fm_multi(specs):
            blocks = [(j_src, q, rows, p1) for (j_src, rows, p1) in specs for q in range(4)]
            nxt = load_wada(blocks[0][0] * 4 + blocks[0][1])
            for bi, (j_src, q, rows, plus_one) in enumerate(blocks):
                buf = nxt
                if bi + 1 < len(blocks):
                    nxt = load_wada(blocks[bi + 1][0] * 4 + blocks[bi + 1][1])
                for cc in range(4):
                    for kc in range(16):
                        P.pe(lambda e: e.matmul(
                            ps[0][:, cc * 2:cc * 2 + 2], lhsT=buf[:, kc, cc * 128:(cc + 1) * 128], rhs=sT_bf[:, kc, :],
                            start=(kc == 0), stop=(kc == 15)), r=[buf.name, sT_bf.name], w=[PSK[0]])
                for (row, jd) in rows:
                    dst = modsT[:, jd, q * 4:(q + 1) * 4]
                    src = bc(ps[0], row, [[512, 128], [2, 4]])
                    bsrc = badaT_sb[:, j_src * 16 + q * 4: j_src * 16 + q * 4 + 4]
                    P.dve(lambda e: e.scalar_tensor_tensor(
                        out=dst, in0=src, scalar=(1.0 if plus_one else 0.0), in1=bsrc, op0=ALU.add, op1=ALU.add),
                        r=[PSK[0], badaT_sb.name], w=[modsT.name])
                A.free(buf)

        mods_fm_multi([(0, [(0, 0), (1, 4)], False), (1, [(0, 1), (1, 5)], True)])

        if "modsT" in tap_out:
            tap("modsT", modsT[:, :, :], [modsT.name], [128, 8, 16])

        if stop == "mods":
            pass
        else:
          class Ref:
              def __init__(self, tensor, off, row):
                  self.t, self.o, self.r = tensor, off, row

              def v(self, off, dims, parts=128):
                  return bass.AP(self.t, self.o + off, [[self.r, parts]] + [list(d) for d in dims])

          def tref(tile):
              return Ref(tile.ap.tensor, tile.ap.offset, tile.ap.ap[0][0])

          def pref(i, bf=False):
              return Ref(psb[i] if bf else ps[i], 0, 1024 if bf else 512)

          def pe_fence(b):
              if os.environ.get("DBG_NOFENCE"):
                  return
              P.pe(lambda e: e.matmul(ps[b][0:1, 510:512], lhsT=ident_bf[:, 0:1], rhs=ident_bf[:, 0:2], start=True, stop=True), r=[ident_bf.name], w=[PSK[b]])

          bank_rr = [0]

          def next_bank(lo=0, hi=8):
              b = lo + (bank_rr[0] % (hi - lo))
              bank_rr[0] += 1
              return b

          TBr = tref(TB)
          statn = [0]
          STT = A.alloc("stats", [128, 16, 8], F32)

          def new_stat():
              s = statn[0] % 16
              statn[0] += 1
              key = (STT.name, s)
              P.dve(lambda e: e.memset(STT[:, s, :], 0.0), w=[key])
              return s, key

          def finish_rstd(s, key, n, c_sum, c_sq, c_mean, c_rstd, c_nmr, width=1):
              inv = 1.0 / n
              sl = lambda c: STT[:, s, c:c + width]
              P.dve(lambda e: e.tensor_scalar(out=sl(c_mean), in0=sl(c_sum), scalar1=inv, scalar2=None, op0=ALU.mult), r=[key], w=[key])
              P.dve(lambda e: e.tensor_tensor(out=sl(c_rstd), in0=sl(c_mean), in1=sl(c_mean), op=ALU.mult), r=[key], w=[key])
              P.dve(lambda e: e.tensor_scalar(out=sl(c_rstd), in0=sl(c_rstd), scalar1=-EPS, scalar2=None, op0=ALU.add), r=[key], w=[key])
              P.dve(lambda e: e.scalar_tensor_tensor(out=sl(c_rstd), in0=sl(c_sq), scalar=inv, in1=sl(c_rstd), op0=ALU.mult, op1=ALU.subtract), r=[key], w=[key])
              P.act(lambda e: e.activation(out=sl(c_rstd), in_=sl(c_rstd), func=AF.Sqrt), r=[key], w=[key])
              P.dve(lambda e: e.reciprocal(out=sl(c_rstd), in_=sl(c_rstd)), r=[key], w=[key])
              if c_nmr is not None:
                  P.dve(lambda e: e.scalar_tensor_tensor(out=sl(c_nmr), in0=sl(c_mean), scalar=-1.0, in1=sl(c_rstd), op0=ALU.mult, op1=ALU.mult), r=[key], w=[key])

          def rms_rstd(s, key, n, c_sq, c_rstd):
              P.dve(lambda e: e.tensor_scalar(out=STT[:, s, c_rstd:c_rstd + 1], in0=STT[:, s, c_sq:c_sq + 1], scalar1=1.0 / n, scalar2=EPS, op0=ALU.mult, op1=ALU.add), r=[key], w=[key])
              P.act(lambda e: e.activation(out=STT[:, s, c_rstd:c_rstd + 1], in_=STT[:, s, c_rstd:c_rstd + 1], func=AF.Sqrt), r=[key], w=[key])
              P.dve(lambda e: e.reciprocal(out=STT[:, s, c_rstd:c_rstd + 1], in_=STT[:, s, c_rstd:c_rstd + 1]), r=[key], w=[key])

          JUNK = A.alloc("junk", [128, 2048], BF16)

          def ln_stats_multi(srcs):
              n = len(srcs)
              LS = A.alloc("lnst", [128, 5, n], F32)
              ks, kq, key = (LS.name, 0), (LS.name, 1), (LS.name, 2)
              P.dve(lambda e: e.memset(LS[:, :, :], 0.0), w=[ks, kq, key])
              for i, (ap_, keys_) in enumerate(srcs):
                  P.dve(lambda e: e.tensor_reduce(out=LS[:, 0, i:i + 1], in_=ap_, axis=AX.X, op=ALU.add), r=list(keys_) + [ks], w=[ks])
              for i, (ap_, keys_) in enumerate(srcs):
                  P.act(lambda e: e.activation(out=JUNK[:, :], in_=ap_, func=AF.Square, accum_out=LS[:, 1, i:i + 1]), r=list(keys_) + [kq], w=[JUNK.name, kq])
              inv = 1.0 / 2048.0
              P.dve(lambda e: e.tensor_scalar(out=LS[:, 2, :], in0=LS[:, 0, :], scalar1=inv, scalar2=None, op0=ALU.mult), r=[ks, key], w=[key])
              P.dve(lambda e: e.tensor_tensor(out=LS[:, 3, :], in0=LS[:, 2, :], in1=LS[:, 2, :], op=ALU.mult), r=[key], w=[key])
              P.dve(lambda e: e.tensor_scalar(out=LS[:, 3, :], in0=LS[:, 3, :], scalar1=-EPS, scalar2=None, op0=ALU.add), r=[key], w=[key])
              P.dve(lambda e: e.scalar_tensor_tensor(out=LS[:, 3, :], in0=LS[:, 1, :], scalar=inv, in1=LS[:, 3, :], op0=ALU.mult, op1=ALU.subtract), r=[kq, key], w=[key])
              P.act(lambda e: e.activation(out=LS[:, 3, :], in_=LS[:, 3, :], func=AF.Sqrt), r=[key], w=[key])
              P.dve(lambda e: e.reciprocal(out=LS[:, 3, :], in_=LS[:, 3, :]), r=[key], w=[key])
              P.dve(lambda e: e.scalar_tensor_tensor(out=LS[:, 4, :], in0=LS[:, 2, :], scalar=-1.0, in1=LS[:, 3, :], op0=ALU.mult, op1=ALU.mult), r=[key], w=[key])
              return LS, [(LS[:, 3, i:i + 1], LS[:, 4, i:i + 1], key) for i in range(n)]

          def ln_apply(st, src_ap, src_keys, dst_ap, dst_keys):
              rstd_ap, nmr_ap, key = st
              P.act(lambda e: e.activation(out=dst_ap, in_=src_ap, func=AF.Identity, scale=rstd_ap, bias=nmr_ap), r=list(src_keys) + [key], w=list(dst_keys))

          def ln_stats(src_ap, src_keys):
              s, key = new_stat()
              P.dve(lambda e: e.tensor_reduce(out=STT[:, s, 0:1], in_=src_ap, axis=AX.X, op=ALU.add), r=list(src_keys) + [key], w=[key])
              P.act(lambda e: e.activation(out=JUNK[:, :], in_=src_ap, func=AF.Square, accum_out=STT[:, s, 1:2]), r=list(src_keys) + [key], w=[JUNK.name, key])
              finish_rstd(s, key, 2048.0, 0, 1, 2, 3, 4)
              return STT[:, s, 3:4], STT[:, s, 4:5], key

          def ln_norm_tile(src_ap, src_keys, dst_ap, dst_keys):
              ln_apply(ln_stats(src_ap, src_keys), src_ap, src_keys, dst_ap, dst_keys)

          def transpose_block(src_tile, ntile, dst_tile, dst_col0, j_scale, j_shift, eng_toggle):
              n = ntile * 128
              for kc in range(16):
                  b = next_bank()
                  for j in range(ntile):
                      P.pe(lambda e, b=b, j=j, kc=kc: e.transpose(psb[b][:, j * 128:(j + 1) * 128], src_tile[:, j, kc * 128:(kc + 1) * 128], ident_bf[:, :]),
                           r=[src_tile.name, ident_bf.name], w=[PSK[b]])
                  dst = dst_tile[:, kc, dst_col0:dst_col0 + n]
                  if (kc + eng_toggle) % 2 == 0:
                      P.act(lambda e, b=b, dst=dst, kc=kc: e.activation(out=dst, in_=psb[b][:, 0:n], func=AF.Identity, scale=modsT[:, j_scale, kc:kc + 1], bias=modsT[:, j_shift, kc:kc + 1]),
                            r=[PSK[b], modsT.name], w=[(dst_tile.name, kc)])
                  else:
                      P.dve(lambda e, b=b, dst=dst, kc=kc: e.tensor_scalar(out=dst, in0=psb[b][:, 0:n], scalar1=modsT[:, j_scale, kc:kc + 1], scalar2=modsT[:, j_shift, kc:kc + 1], op0=ALU.mult, op1=ALU.add),
                            r=[PSK[b], modsT.name], w=[(dst_tile.name, kc)])

          P.marks.append(("A", sum(1 for x_ in P.ins if x_.eng == "pe")))
          xmT_own = A.alloc("xmT_own", [128, 16, 1024], BF16)
          xmT_oth = A.alloc("xmT_oth", [128, 16, 1024], BF16)
          xmT_ctx = A.alloc("xmT_ctx", [128, 16, 256], BF16)
          XK = lambda t: [(t.name, kc) for kc in range(16)]

          def phase_a(src_dram, ntiles_total, dst_tile, j_scale, j_shift):
              loaded = {}

              def load(ti):
                  if ti < ntiles_total and ti not in loaded:
                      xt = A.alloc("xt", [128, 2048], F32)
                      P.dma("sp", xt[:, :], src_dram.ap()[ti * 128:(ti + 1) * 128, :], w=[xt.name])
                      loaded[ti] = xt
              blk = 0
              t0 = 0
              while t0 < ntiles_total:
                  nt = min(4, ntiles_total - t0)
                  for j in range(nt):
                      load(t0 + j)
                  load(t0 + nt)
                  load(t0 + nt + 1)
                  xn = A.alloc("xn", [128, nt, 2048], BF16)
                  xts = [loaded.pop(t0 + j) for j in range(nt)]
                  LSx, sts = ln_stats_multi([(xt[:, :], [xt.name]) for xt in xts])
                  for j in range(nt):
                      ln_apply(sts[j], xts[j][:, :], [xts[j].name], xn[:, j, :], [xn.name])
                  A.free(LSx, *xts)
                  transpose_block(xn, nt, dst_tile, t0 * 128, j_scale, j_shift, blk)
                  A.free(xn)
                  t0 += nt
                  blk += 1

          phase_a(x_oth, 8, xmT_oth, 1, 0)
          phase_a(ctx_in, 2, xmT_ctx, 5, 4)
          phase_a(x_own, 8, xmT_own, 1, 0)
          if "xmTo" in tap_out:
              for q4 in range(4):
                  tap("xmTo%d" % q4, xmT_oth[:, :, q4 * 256:(q4 + 1) * 256], XK(xmT_oth), [128, 16, 256])
          if "xmT" in tap_out:
              tap("xmT", xmT_own[:, :, 0:256], XK(xmT_own), [128, 16, 256])
              tap("xmTc", xmT_ctx[:, :, :], XK(xmT_ctx), [128, 16, 256])

          P.marks.append(("0c", sum(1 for x_ in P.ins if x_.eng == "pe")))
          LG = A.alloc("LG", [128, 16], F32)
          G128 = A.alloc("G128", [128, 16], F32)
          KZ = A.alloc("KZ", [128, 16], F32)
          lgt = A.alloc("lgt", [128, 16], F32)
          P.dma("sp", LG[:, :], bc(ret_decay, 0, [[0, 128], [1, 16]]), w=[LG.name])
          P.act(lambda e: e.activation(out=lgt[:, :], in_=LG[:, :], func=AF.Exp, scale=-1.0), r=[LG.name], w=[lgt.name])
          P.dve(lambda e: e.tensor_scalar(out=LG[:, :], in0=lgt[:, :], scalar1=-1.0 / 7.0, scalar2=1.0 / 6.0, op0=ALU.mult, op1=ALU.add), r=[lgt.name], w=[LG.name])
          for kk in (5, 4, 3, 2, 1):
              P.dve(lambda e: e.tensor_tensor(out=LG[:, :], in0=LG[:, :], in1=lgt[:, :], op=ALU.mult), r=[LG.name, lgt.name], w=[LG.name])
              P.dve(lambda e, kk=kk: e.tensor_scalar(out=LG[:, :], in0=LG[:, :], scalar1=-1.0, scalar2=1.0 / kk, op0=ALU.mult, op1=ALU.add), r=[LG.name], w=[LG.name])
          P.dve(lambda e: e.scalar_tensor_tensor(out=LG[:, :], in0=LG[:, :], scalar=-1.0, in1=lgt[:, :], op0=ALU.mult, op1=ALU.mult), r=[LG.name, lgt.name], w=[LG.name])
          P.act(lambda e: e.activation(out=G128[:, :], in_=LG[:, :], func=AF.Exp, scale=128.0), r=[LG.name], w=[G128.name])
          P.dve(lambda e: e.tensor_scalar(out=lgt[:, 0:8], in0=LG[:, 0:8], scalar1=TB[:, T_Z1:T_Z1 + 1], scalar2=None, op0=ALU.mult), r=[LG.name, TB.name], w=[lgt.name])
          P.dve(lambda e: e.tensor_scalar(out=lgt[:, 8:16], in0=LG[:, 8:16], scalar1=TB[:, T_Z2:T_Z2 + 1], scalar2=None, op0=ALU.mult), r=[LG.name, TB.name], w=[lgt.name])
          P.act(lambda e: e.activation(out=KZ[:, :], in_=lgt[:, :], func=AF.Exp), r=[lgt.name], w=[KZ.name])
          P.dve(lambda e: e.tensor_scalar(out=KZ[:, :], in0=KZ[:, :], scalar1=K_SCALE, scalar2=None, op0=ALU.mult), r=[KZ.name], w=[KZ.name])
          MT = A.alloc("MT", [128, 8, 128], F32)
          XiF = A.alloc("XiF", [128, 8, 128], BF16)
          XiB = A.alloc("XiB", [128, 8, 128], BF16)
          WF = A.alloc("WF", [128, 10, 8], F32)
          WB = A.alloc("WB", [128, 10, 8], F32)
          mtmp = A.alloc("mtmp", [128, 128], F32)
          for h in range(8):
              P.act(lambda e, h=h: e.activation(out=MT[:, h, :], in_=TB[:, T_D1:T_D1 + 128], func=AF.Exp, scale=LG[:, h:h + 1]), r=[LG.name, TB.name], w=[(MT.name, h)])
              P.dve(lambda e, h=h: e.scalar_tensor_tensor(out=MT[:, h, :], in0=MT[:, h, :], scalar=K_SCALE, in1=TB[:, T_MK1:T_MK1 + 128], op0=ALU.mult, op1=ALU.mult), r=[(MT.name, h), TB.name], w=[(MT.name, h)])
              P.act(lambda e, h=h: e.activation(out=mtmp[:, :], in_=TB[:, T_D2:T_D2 + 128], func=AF.Exp, scale=LG[:, 8 + h:9 + h]), r=[LG.name, TB.name], w=[mtmp.name])
              P.dve(lambda e, h=h: e.scalar_tensor_tensor(out=mtmp[:, :], in0=mtmp[:, :], scalar=K_SCALE, in1=TB[:, T_MK2:T_MK2 + 128], op0=ALU.mult, op1=ALU.mult), r=[mtmp.name, TB.name], w=[mtmp.name])
              P.dve(lambda e, h=h: e.tensor_tensor(out=MT[:, h, :], in0=MT[:, h, :], in1=mtmp[:, :], op=ALU.add), r=[(MT.name, h), mtmp.name], w=[(MT.name, h)])
              P.act(lambda e, h=h: e.activation(out=XiF[:, h, :], in_=TB[:, T_CP1:T_CP1 + 128], func=AF.Exp, scale=LG[:, h:h + 1]), r=[LG.name, TB.name], w=[(XiF.name, h)])
              P.act(lambda e, h=h: e.activation(out=XiB[:, h, :], in_=TB[:, T_C128:T_C128 + 128], func=AF.Exp, scale=LG[:, 8 + h:9 + h]), r=[LG.name, TB.name], w=[(XiB.name, h)])
              P.act(lambda e, h=h: e.activation(out=WF[:, :, h], in_=TB[:, T_EF:T_EF + 10], func=AF.Exp, scale=LG[:, h:h + 1]), r=[LG.name, TB.name], w=[WF.name])
              P.act(lambda e, h=h: e.activation(out=WB[:, :, h], in_=TB[:, T_EB:T_EB + 10], func=AF.Exp, scale=LG[:, 8 + h:9 + h]), r=[LG.name, TB.name], w=[WB.name])
          P.dve(lambda e: e.scalar_tensor_tensor(out=WF[:, :, :], in0=WF[:, :, :], scalar=K_SCALE, in1=TBr.v(T_MF, [[1, 10], [0, 8]]), op0=ALU.mult, op1=ALU.mult), r=[WF.name, TB.name], w=[WF.name])
          P.dve(lambda e: e.scalar_tensor_tensor(out=WB[:, :, :], in0=WB[:, :, :], scalar=K_SCALE, in1=TBr.v(T_MB, [[1, 10], [0, 8]]), op0=ALU.mult, op1=ALU.mult), r=[WB.name, TB.name], w=[WB.name])
          A.free(mtmp, lgt)
          MTK = [(MT.name, h) for h in range(8)]
          if "decay" in tap_out:
              tap("LG", LG[:, :], [LG.name], [128, 16])
              tap("MT", MT[:, :, :], MTK, [128, 8, 128])
              tap("WF", WF[:, :, :], [WF.name], [128, 10, 8])
              tap("WB", WB[:, :, :], [WB.name], [128, 10, 8])

          def load_win(col0, ncols, name="w"):
              t = A.alloc(name, [128, 16, ncols], BF16)
              P.dma("pool", t[:, :, :], bc(w_in, col0, [[IN_W, 128], [128 * IN_W, 16], [1, ncols]]), w=[t.name])
              return t

          def proj_tile(xT, col0, wt, ncols, bank, wcol0=0, wkeys=None):
              for kc in range(16):
                  P.pe(lambda e, kc=kc: e.matmul(ps[bank][:, 0:ncols], lhsT=xT[:, kc, col0:col0 + 128], rhs=wt[:, kc, wcol0:wcol0 + ncols], start=(kc == 0), stop=(kc == 15)),
                       r=[(xT.name, kc)] + (wkeys or [wt.name]), w=[PSK[bank]])

          def rope(dst_ref, src_ref, nh, W, cos_ap_ref, sin_ap_ref, rkeys, wkeys, tmpname="ropetmp", tab_stride=0):
              q = W // 4
              CH = ["ropechain"] if os.environ.get("DBG_ROPE_CHAIN") else []
              rkeys = list(rkeys)
              t1 = A.alloc(tmpname, [128, nh, W], F32)
              t2 = A.alloc(tmpname, [128, nh, W], F32)
              t1r, t2r = tref(t1), tref(t2)
              hdim = [[W, nh]] if nh > 1 else []
              bdim = [[tab_stride, nh]] if nh > 1 else []
              full = hdim + [[1, W]]
              P.dve(lambda e: e.tensor_tensor(out=t1r.v(0, full), in0=src_ref.v(0, full), in1=cos_ap_ref.v(0, bdim + [[1, W]]), op=ALU.mult),
                    r=rkeys + CH, w=[t1.name] + CH)
              if W == 64:
                  q = W // 2
                  half = hdim + [[1, q]]
                  bhalf = bdim + [[1, q]]
              else:
                  half = hdim + [[2 * q, 2], [1, q]]
                  bhalf = bdim + [[2 * q, 2], [1, q]]
              P.dve(lambda e: e.tensor_tensor(out=t2r.v(0, half), in0=src_ref.v(q, half), in1=sin_ap_ref.v(0, bhalf), op=ALU.mult),
                    r=rkeys + CH, w=[(t2.name, 0)] + CH)
              P.dve(lambda e: e.tensor_tensor(out=t2r.v(q, half), in0=src_ref.v(0, half), in1=sin_ap_ref.v(q, bhalf), op=ALU.mult),
                    r=rkeys + CH, w=[(t2.name, 1)] + CH)
              P.add(os.environ.get("DBG_ROPE_ENG", "dve"), lambda e: e.tensor_tensor(out=dst_ref.v(0, full), in0=t1r.v(0, full), in1=t2r.v(0, full), op=ALU.add),
                    [t1.name, (t2.name, 0), (t2.name, 1)], wkeys)
              A.free(t1, t2)

          RR_OTH = A.alloc("rr_oth", [128, 2, 8, 128], F32)
          P.dma("sp", RR_OTH[:, :, :, :].rearrange("p a b c -> p (a b c)"), rope_r_oth.ap(), w=[RR_OTH.name])
          RRo = tref(RR_OTH)

          P.marks.append(("B", sum(1 for x_ in P.ins if x_.eng == "pe")))
          SF0 = A.alloc("SF0", [128, 8, 128], F32)
          SB0 = A.alloc("SB0", [128, 8, 128], F32)
          for hg in range(2):
              wk = load_win(1024 + hg * 512, 512, "wk")
              wv = load_win(2048 + hg * 512, 512, "wv")
              kf_all = A.alloc("kf_all", [128, 10, 512], BF16)
              kb_all = A.alloc("kb_all", [128, 10, 512], BF16)
              v_all = A.alloc("v_all", [128, 10, 512], BF16)
              WFr, WBr = tref(WF), tref(WB)
              for t in range(10):
                  xT, c0 = (xmT_oth, t * 128) if t < 8 else (xmT_ctx, (t - 8) * 128)
                  b = next_bank()
                  proj_tile(xT, c0, wk, 512, b)
                  if t < 8:
                      kr = A.alloc("kr", [128, 4, 128], F32)
                      rope(tref(kr), pref(b), 4, 128, Ref(RRo.t, RRo.o + t * 128, RRo.r), Ref(RRo.t, RRo.o + 1024 + t * 128, RRo.r), [PSK[b], RR_OTH.name], [kr.name])
                      ksrc, kkeys = tref(kr), [kr.name]
                  else:
                      ksrc, kkeys = pref(b), [PSK[b]]
                  full = [[128, 4], [1, 128]]
                  P.dve(lambda e, ksrc=ksrc, t=t: e.tensor_tensor(out=tref(kf_all).v(t * 512, full), in0=ksrc.v(0, full), in1=WFr.v(t * 8 + hg * 4, [[1, 4], [0, 128]]), op=ALU.mult),
                        r=kkeys + [WF.name], w=[(kf_all.name, t)])
                  P.dve(lambda e, ksrc=ksrc, t=t: e.tensor_tensor(out=tref(kb_all).v(t * 512, full), in0=ksrc.v(0, full), in1=WBr.v(t * 8 + hg * 4, [[1, 4], [0, 128]]), op=ALU.mult),
                        r=kkeys + [WB.name], w=[(kb_all.name, t)])
                  if t < 8:
                      A.free(kr)
                  b2 = next_bank()
                  proj_tile(xT, c0, wv, 512, b2)
                  P.act(lambda e, b2=b2, t=t: e.activation(out=v_all[:, t, :], in_=ps[b2][:, :], func=AF.Copy), r=[PSK[b2]], w=[(v_all.name, t)])
              A.free(wk, wv)
              for (kall, S0) in ((kf_all, SF0), (kb_all, SB0)):
                  b = next_bank()
                  for h in range(4):
                      for t in range(10):
                          P.pe(lambda e, b=b, h=h, t=t, kall=kall: e.matmul(ps[b][:, h * 128:(h + 1) * 128], lhsT=kall[:, t, h * 128:(h + 1) * 128], rhs=v_all[:, t, h * 128:(h + 1) * 128], start=(t == 0), stop=(t == 9)),
                               r=[(kall.name, t), (v_all.name, t)], w=[PSK[b]])
                  P.dve(lambda e, b=b, S0=S0: e.tensor_copy(out=S0[:, hg * 4:(hg + 1) * 4, :], in_=ps[b][:, :].rearrange("p (h e) -> p h e", h=4)), r=[PSK[b]], w=[(S0.name, hg)])
              A.free(kf_all, kb_all, v_all)
          A.free(RR_OTH)
          if "SF0" in tap_out:
              tap("SF0", SF0[:, :, :], [(SF0.name, 0), (SF0.name, 1)], [128, 8, 128])
              tap("SB0", SB0[:, :, :], [(SB0.name, 0), (SB0.name, 1)], [128, 8, 128])

          nb6 = lambda: next_bank(0, 6)
          P.marks.append(("C", sum(1 for x_ in P.ins if x_.eng == "pe")))
          ckvT = A.alloc("ckvT", [128, 2, 2304], BF16)
          kpeT = A.alloc("kpeT", [64, 2304], BF16)
          RM_OWN = A.alloc("rm_own", [128, 2, 8, 64], F32)
          RM_OTH = A.alloc("rm_oth", [128, 2, 8, 64], F32)
          P.dma("sp", RM_OWN[:, :, :, :].rearrange("p a b c -> p (a b c)"), rope_m_own.ap(), w=[RM_OWN.name])
          P.dma("sp", RM_OTH[:, :, :, :].rearrange("p a b c -> p (a b c)"), rope_m_oth.ap(), w=[RM_OTH.name])
          wc = A.alloc("wc", [128, 16, 320], BF16)
          P.dma("pool", wc[:, :, :], bc(w_ckp, 0, [[320, 128], [128 * 320, 16], [1, 320]]), w=[wc.name])
          CK = lambda lo, hi: [(ckvT.name, i) for i in range(lo // 128, (hi + 127) // 128)]
          PK = lambda lo, hi: [(kpeT.name, i) for i in range(lo // 128, (hi + 127) // 128)]
          for (xT, ntiles, key0, rtab) in ((xmT_oth, 8, 1024, RM_OTH), (xmT_ctx, 2, 2048, None), (xmT_own, 8, 0, RM_OWN)):
              t0 = 0
              while t0 < ntiles:
                  nt = min(4, ntiles - t0)
                  n = nt * 128
                  ckvn = A.alloc("ckvn", [128, nt, 256], BF16)
                  kper = A.alloc("kper", [128, nt, 64], BF16)
                  kps = A.alloc("kps", [128, nt, 64], F32)
                  for j in range(nt):
                      t = t0 + j
                      b = nb6()
                      proj_tile(xT, t * 128, wc, 320, b)
                      s, key = new_stat()
                      P.act(lambda e: e.activation(out=JUNK[:, 0:256], in_=ps[b][:, 0:256], func=AF.Square, accum_out=STT[:, s, 0:1]), r=[PSK[b], key], w=[JUNK.name, key])
                      rms_rstd(s, key, 256.0, 0, 1)
                      P.act(lambda e: e.activation(out=ckvn[:, j, :], in_=ps[b][:, 0:256], func=AF.Copy, scale=STT[:, s, 1:2]), r=[PSK[b], key], w=[(ckvn.name, j)])
                      if rtab is not None:
                          P.act(lambda e: e.activation(out=kps[:, j, :], in_=ps[b][:, 256:320], func=AF.Copy), r=[PSK[b]], w=[(kps.name, j)])
                      else:
                          P.act(lambda e: e.activation(out=kper[:, j, :], in_=ps[b][:, 256:320], func=AF.Copy), r=[PSK[b]], w=[(kper.name, j)])
                  if rtab is not None:
                      rt = tref(rtab)
                      rope(tref(kper), tref(kps), nt, 64, Ref(rt.t, rt.o + t0 * 64, rt.r), Ref(rt.t, rt.o + 512 + t0 * 64, rt.r),
                           [(kps.name, j) for j in range(nt)] + [rtab.name], [(kper.name, j) for j in range(nt)], tab_stride=64)
                  A.free(kps)
                  k0 = key0 + t0 * 128
                  for lc in range(2):
                      b = nb6()
                      for j in range(nt):
                          P.pe(lambda e: e.transpose(psb[b][:, j * 128:(j + 1) * 128], ckvn[:, j, lc * 128:(lc + 1) * 128], ident_bf[:, :]), r=[(ckvn.name, j), ident_bf.name], w=[PSK[b]])
                      pe_fence(b)
                      P.dve(lambda e: e.tensor_scalar(out=ckvT[:, lc, k0:k0 + n], in0=psb[b][:, 0:n], scalar1=kvn[:, lc:lc + 1], scalar2=None, op0=ALU.mult), r=[PSK[b], kvn.name], w=CK(k0, k0 + n))
                  b = nb6()
                  if not os.environ.get("DBG_NOKPE") and not os.environ.get("DBG_NOKPET"):
                      for j in range(nt):
                          P.pe(lambda e: e.transpose(psb[b][0:64, j * 128:(j + 1) * 128], kper[:, j, :], ident_bf[:, :]), r=[(kper.name, j), ident_bf.name], w=[PSK[b]])
                      pe_fence(b)
                      P.act(lambda e: e.activation(out=kpeT[0:64, k0:k0 + n], in_=psb[b][0:64, 0:n], func=AF.Copy), r=[PSK[b]], w=PK(k0, k0 + n))
                  A.free(ckvn, kper)
                  t0 += nt
          A.free(wc, xmT_oth, xmT_ctx, RM_OTH)
          if "ckvT" in tap_out:
              tap("ckvT", ckvT[:, :, :], CK(0, 2304), [128, 2, 2304])
              tap("kpeT", kpeT[0:64, :], PK(0, 2304), [64, 2304])

          P.marks.append(("D", sum(1 for x_ in P.ins if x_.eng == "pe")))
          mixT = A.alloc("mixT", [128, 16, 1024], BF16)
          NHP = 0 if stop == "C" else (1 if stop == "D1" else 4)
          GW = A.alloc("GW", [128, 1024], F32)
          P.dma("sp", GW[:, :], bc(ret_gn_w, 0, [[0, 128], [1, 1024]]), w=[GW.name])
          RR_OWN = A.alloc("rr_own", [128, 2, 8, 128], F32)
          P.dma("sp", RR_OWN[:, :, :, :].rearrange("p a b c -> p (a b c)"), rope_r_own.ap(), w=[RR_OWN.name])
          RRw = tref(RR_OWN)
          KZr, G128r, MTr, XiFr, XiBr, GWr = tref(KZ), tref(G128), tref(MT), tref(XiF), tref(XiB), tref(GW)
          hd = [[128, 2], [1, 128]]
          for hp in range(NHP):
              wq = load_win(hp * 256, 256, "wq")
              wk = load_win(1024 + hp * 256, 256, "wk")
              wv = load_win(2048 + hp * 256, 256, "wv")
              wg = load_win(3072 + hp * 256, 256, "wg")
              qT = A.alloc("qT", [128, 2, 1024], BF16)
              kT = A.alloc("kT", [128, 2, 1024], BF16)
              kzf = A.alloc("kzf", [128, 8, 256], BF16)
              kzb = A.alloc("kzb", [128, 8, 256], BF16)
              vt = A.alloc("vt", [128, 8, 256], BF16)
              sg = A.alloc("sg", [128, 8, 256], BF16)
              pend = []

              def flush():
                  for th in pend:
                      th()
                  del pend[:]

              for t in range(8):
                  blk, j = t // 4, t % 4
                  bq = nb6()
                  proj_tile(xmT_own, t * 128, wq, 256, bq)
                  bk = nb6()
                  proj_tile(xmT_own, t * 128, wk, 256, bk)
                  bv = nb6()
                  proj_tile(xmT_own, t * 128, wv, 256, bv)
                  bg = nb6()
                  proj_tile(xmT_own, t * 128, wg, 256, bg)
                  flush()
                  qr = A.alloc("qr", [128, 2, 128], BF16)
                  rope(tref(qr), pref(bq), 2, 128, Ref(RRw.t, RRw.o + t * 128, RRw.r), Ref(RRw.t, RRw.o + 1024 + t * 128, RRw.r), [PSK[bq], RR_OWN.name], [qr.name])
                  kr = A.alloc("kr", [128, 2, 128], F32)
                  rope(tref(kr), pref(bk), 2, 128, Ref(RRw.t, RRw.o + t * 128, RRw.r), Ref(RRw.t, RRw.o + 1024 + t * 128, RRw.r), [PSK[bk], RR_OWN.name], [kr.name])
                  P.act(lambda e: e.activation(out=vt[:, t, :], in_=ps[bv][:, 0:256], func=AF.Copy), r=[PSK[bv]], w=[(vt.name, t)])
                  P.act(lambda e: e.activation(out=sg[:, t, :], in_=ps[bg][:, 0:256], func=AF.Silu), r=[PSK[bg]], w=[(sg.name, t)])
                  P.dve(lambda e: e.tensor_tensor(out=tref(kzf).v(t * 256, hd), in0=kr[:, :, :], in1=KZr.v(2 * hp, [[1, 2], [0, 128]]), op=ALU.mult), r=[kr.name, KZ.name], w=[(kzf.name, t)])
                  P.pool(lambda e: e.tensor_tensor(out=tref(kzb).v(t * 256, hd), in0=kr[:, :, :], in1=KZr.v(8 + 2 * hp, [[1, 2], [0, 128]]), op=ALU.mult), r=[kr.name, KZ.name], w=[(kzb.name, t)])
                  krb = A.alloc("krb", [128, 2, 128], BF16)
                  P.act(lambda e: e.activation(out=krb[:, :, :], in_=kr[:, :, :], func=AF.Copy), r=[kr.name], w=[krb.name])

                  def tr(qr=qr, krb=krb, kr=kr, blk=blk, j=j):
                      for h in range(2):
                          P.pe(lambda e: e.transpose(psb[6][:, h * 512 + j * 128:h * 512 + (j + 1) * 128], qr[:, h, :], ident_bf[:, :]), r=[qr.name, ident_bf.name], w=[PSK[6]])
                      for h in range(2):
                          P.pe(lambda e: e.transpose(psb[7][:, h * 512 + j * 128:h * 512 + (j + 1) * 128], krb[:, h, :], ident_bf[:, :]), r=[krb.name, ident_bf.name], w=[PSK[7]])
                      A.free(qr, kr, krb)
                      if j == 3:
                          P.dve(lambda e: e.tensor_copy(out=qT[:, :, blk * 512:(blk + 1) * 512], in_=psb[6][:, :].rearrange("p (h n) -> p h n", h=2)), r=[PSK[6]], w=[(qT.name, blk)])
                          P.act(lambda e: e.activation(out=kT[:, :, blk * 512:(blk + 1) * 512], in_=psb[7][:, :].rearrange("p (h n) -> p h n", h=2), func=AF.Copy), r=[PSK[7]], w=[(kT.name, blk)])
                  pend.append(tr)
              flush()
              A.free(wq, wk, wv, wg)
              SF = A.alloc("SF", [128, 8, 2, 128], F32)
              SBk = A.alloc("SBk", [128, 8, 2, 128], F32)
              SFb = A.alloc("SFb", [128, 8, 2, 128], BF16)
              SBb = A.alloc("SBb", [128, 8, 2, 128], BF16)
              P.dve(lambda e: e.tensor_copy(out=SF[:, 0, :, :], in_=SF0[:, 2 * hp:2 * hp + 2, :]), r=[(SF0.name, hp // 2)], w=[(SF.name, 0)])
              P.dve(lambda e: e.tensor_copy(out=SBk[:, 7, :, :], in_=SB0[:, 2 * hp:2 * hp + 2, :]), r=[(SB0.name, hp // 2)], w=[(SBk.name, 7)])
              for i in range(7):
                  b = nb6()
                  for h in range(2):
                      P.pe(lambda e: e.matmul(ps[b][:, h * 128:(h + 1) * 128], lhsT=kzf[:, i, h * 128:(h + 1) * 128], rhs=vt[:, i, h * 128:(h + 1) * 128], start=True, stop=True),
                           r=[(kzf.name, i), (vt.name, i)], w=[PSK[b]])
                      P.pe(lambda e: e.matmul(ps[b][:, 256 + h * 128:256 + (h + 1) * 128], lhsT=kzb[:, 7 - i, h * 128:(h + 1) * 128], rhs=vt[:, 7 - i, h * 128:(h + 1) * 128], start=True, stop=True),
                           r=[(kzb.name, 7 - i), (vt.name, 7 - i)], w=[PSK[b]])
                  for h in range(2):
                      P.dve(lambda e: e.scalar_tensor_tensor(out=SF[:, i + 1, h, :], in0=SF[:, i, h, :], scalar=G128[:, 2 * hp + h:2 * hp + h + 1], in1=ps[b][:, h * 128:(h + 1) * 128], op0=ALU.mult, op1=ALU.add),
                            r=[(SF.name, i), G128.name, PSK[b]], w=[(SF.name, i + 1)])
                      P.dve(lambda e: e.scalar_tensor_tensor(out=SBk[:, 6 - i, h, :], in0=SBk[:, 7 - i, h, :], scalar=G128[:, 8 + 2 * hp + h:8 + 2 * hp + h + 1], in1=ps[b][:, 256 + h * 128:256 + (h + 1) * 128], op0=ALU.mult, op1=ALU.add),
                            r=[(SBk.name, 7 - i), G128.name, PSK[b]], w=[(SBk.name, 6 - i)])
              P.act(lambda e: e.activation(out=SFb[:, :, :, :].rearrange("p a b c -> p (a b c)"), in_=SF[:, :, :, :].rearrange("p a b c -> p (a b c)"), func=AF.Copy), r=[(SF.name, i) for i in range(8)], w=[SFb.name])
              P.act(lambda e: e.activation(out=SBb[:, :, :, :].rearrange("p a b c -> p (a b c)"), in_=SBk[:, :, :, :].rearrange("p a b c -> p (a b c)"), func=AF.Copy), r=[(SBk.name, i) for i in range(8)], w=[SBb.name])
              if hp == 0 and "SF" in tap_out:
                  tap("SF", SF[:, :, :, :].rearrange("p a b c -> p (a b c)"), [(SF.name, i) for i in range(8)], [128, 2048])
                  tap("SBk", SBk[:, :, :, :].rearrange("p a b c -> p (a b c)"), [(SBk.name, i) for i in range(8)], [128, 2048])
              A.free(SF, SBk)

              def scores(t):
                  blk = t // 4
                  b = nb6()
                  for h in range(2):
                      P.pe(lambda e: e.matmul(ps[b][:, h * 128:(h + 1) * 128], lhsT=kT[:, h, t * 128:(t + 1) * 128], rhs=qT[:, h, t * 128:(t + 1) * 128], start=True, stop=True),
                           r=[(kT.name, blk), (qT.name, blk)], w=[PSK[b]])
                  PT = A.alloc("PT", [128, 2, 128], BF16)
                  P.dve(lambda e: e.tensor_tensor(out=PT[:, :, :], in0=ps[b][:, 0:256].rearrange("p (h n) -> p h n", h=2), in1=MT[:, 2 * hp:2 * hp + 2, :], op=ALU.mult),
                        r=[PSK[b], (MT.name, 2 * hp), (MT.name, 2 * hp + 1)], w=[PT.name])
                  qf = A.alloc("qf", [128, 2, 128], BF16)
                  qb_ = A.alloc("qb", [128, 2, 128], BF16)
                  P.pool(lambda e: e.tensor_tensor(out=qf[:, :, :], in0=qT[:, :, t * 128:(t + 1) * 128], in1=XiF[:, 2 * hp:2 * hp + 2, :], op=ALU.mult),
                         r=[(qT.name, blk), (XiF.name, 2 * hp), (XiF.name, 2 * hp + 1)], w=[qf.name])
                  P.pool(lambda e: e.tensor_tensor(out=qb_[:, :, :], in0=qT[:, :, t * 128:(t + 1) * 128], in1=XiB[:, 2 * hp:2 * hp + 2, :], op=ALU.mult),
                         r=[(qT.name, blk), (XiB.name, 2 * hp), (XiB.name, 2 * hp + 1)], w=[qb_.name])
                  return PT, qf, qb_

              cur = scores(0)
              for t in range(8):
                  blk, j = t // 4, t % 4
                  nxt = scores(t + 1) if t + 1 < 8 else None
                  PT, qf, qb_ = cur
                  bo = nb6()
                  for h in range(2):
                      oreg = ps[bo][:, h * 128:(h + 1) * 128]
                      P.pe(lambda e: e.matmul(oreg, lhsT=PT[:, h, :], rhs=vt[:, t, h * 128:(h + 1) * 128], start=True, stop=False), r=[PT.name, (vt.name, t)], w=[PSK[bo]])
                      P.pe(lambda e: e.matmul(oreg, lhsT=qf[:, h, :], rhs=SFb[:, t, h, :], start=False, stop=False), r=[qf.name, SFb.name], w=[PSK[bo]])
                      P.pe(lambda e: e.matmul(oreg, lhsT=qb_[:, h, :], rhs=SBb[:, t, h, :], start=False, stop=True), r=[qb_.name, SBb.name], w=[PSK[bo]])
                  flush()
                  s_, key = new_stat()
                  P.dve(lambda e: e.tensor_reduce(out=STT[:, s_, 0:2], in_=ps[bo][:, 0:256].rearrange("p (h n) -> p h n", h=2), axis=AX.X, op=ALU.add), r=[PSK[bo], key], w=[key])
                  for h in range(2):
                      P.act(lambda e: e.activation(out=JUNK[:, 0:128], in_=ps[bo][:, h * 128:(h + 1) * 128], func=AF.Square, accum_out=STT[:, s_, 2 + h:3 + h]), r=[PSK[bo], key], w=[JUNK.name, key])
                  finish_rstd(s_, key, 128.0, 0, 2, 4, 6, None, width=2)
                  y = A.alloc("y", [128, 2, 128], F32)
                  for h in range(2):
                      P.dve(lambda e: e.tensor_scalar(out=y[:, h, :], in0=ps[bo][:, h * 128:(h + 1) * 128], scalar1=STT[:, s_, 4 + h:5 + h], scalar2=STT[:, s_, 6 + h:7 + h], op0=ALU.subtract, op1=ALU.mult),
                            r=[PSK[bo], key], w=[(y.name, h)])
                  P.pool(lambda e: e.tensor_tensor(out=y[:, :, :], in0=y[:, :, :], in1=GWr.v(hp * 256, hd), op=ALU.mult), r=[(y.name, 0), (y.name, 1), GW.name], w=[(y.name, 0), (y.name, 1)])
                  ret = A.alloc("ret", [128, 2, 128], BF16)
                  P.pool(lambda e: e.tensor_tensor(out=ret[:, :, :], in0=y[:, :, :], in1=tref(sg).v(t * 256, hd), op=ALU.mult), r=[(y.name, 0), (y.name, 1), (sg.name, t)], w=[ret.name])

                  def tr2(ret=ret, PT=PT, qf=qf, qb_=qb_, y=y, blk=blk, j=j):
                      for h in range(2):
                          P.pe(lambda e: e.transpose(psb[6][:, h * 512 + j * 128:h * 512 + (j + 1) * 128], ret[:, h, :], ident_bf[:, :]), r=[ret.name, ident_bf.name], w=[PSK[6]])
                      A.free(PT, qf, qb_, y, ret)
                      if j == 3:
                          P.dve(lambda e: e.tensor_copy(out=mixT[:, 2 * hp:2 * hp + 2, blk * 512:(blk + 1) * 512], in_=psb[6][:, :].rearrange("p (h n) -> p h n", h=2)), r=[PSK[6]],
                                w=[(mixT.name, 2 * hp), (mixT.name, 2 * hp + 1)])
                  pend.append(tr2)
                  cur = nxt
              flush()
              A.free(qT, kT, kzf, kzb, vt, sg, SFb, SBb)
          A.free(RR_OWN, GW, SF0, SB0, MT, XiF, XiB, WF, WB)
          if "ret" in tap_out:
              tap("ret", mixT[:, 0:8, 0:256], [(mixT.name, i) for i in range(8)], [128, 8, 256])

          nb4 = lambda: next_bank(0, 4)
          MK = lambda lo, hi: [(mixT.name, i) for i in range(lo, hi)]
          if stop not in ("C", "D1", "D"):
              P.marks.append(("E", sum(1 for x_ in P.ins if x_.eng == "pe")))
              wcq = load_win(4096, 512, "wcq")
              cqT = A.alloc("cqT", [128, 4, 1024], BF16)
              for blk in range(2):
                  cqn = A.alloc("cqn", [128, 4, 512], BF16)
                  for j in range(4):
                      t = blk * 4 + j
                      b = nb6()
                      proj_tile(xmT_own, t * 128, wcq, 512, b)
                      s, key = new_stat()
                      P.act(lambda e: e.activation(out=JUNK[:, 0:512], in_=ps[b][:, :], func=AF.Square, accum_out=STT[:, s, 0:1]), r=[PSK[b], key], w=[JUNK.name, key])
                      rms_rstd(s, key, 512.0, 0, 1)
                      P.act(lambda e: e.activation(out=cqn[:, j, :], in_=ps[b][:, :], func=AF.Copy, scale=STT[:, s, 1:2]), r=[PSK[b], key], w=[(cqn.name, j)])
                  for lc in range(4):
                      b = nb6()
                      for j in range(4):
                          P.pe(lambda e: e.transpose(psb[b][:, j * 128:(j + 1) * 128], cqn[:, j, lc * 128:(lc + 1) * 128], ident_bf[:, :]), r=[(cqn.name, j), ident_bf.name], w=[PSK[b]])
                      P.dve(lambda e: e.tensor_scalar(out=cqT[:, lc, blk * 512:(blk + 1) * 512], in0=psb[b][:, 0:512], scalar1=qn[:, lc:lc + 1], scalar2=None, op0=ALU.mult), r=[PSK[b], qn.name], w=[(cqT.name, blk)])
                  A.free(cqn)
              A.free(wcq, xmT_own)
              CQK = [(cqT.name, 0), (cqT.name, 1)]
              wqr = A.alloc("wqr", [128, 4, 512], BF16)
              P.dma("pool", wqr[:, :, :], bc(w_uqr, 0, [[512, 128], [128 * 512, 4], [1, 512]]), w=[wqr.name])
              qrT = A.alloc("qrT", [64, 8, 1024], BF16)
              RMw = tref(RM_OWN)
              for t in range(8):
                  b = nb6()
                  for lc in range(4):
                      P.pe(lambda e: e.matmul(ps[b][:, :], lhsT=cqT[:, lc, t * 128:(t + 1) * 128], rhs=wqr[:, lc, :], start=(lc == 0), stop=(lc == 3)), r=[(cqT.name, t // 4), wqr.name], w=[PSK[b]])
                  qrr = A.alloc("qrr", [128, 8, 64], BF16)
                  rope(tref(qrr), pref(b), 8, 64, Ref(RMw.t, RMw.o + t * 64, RMw.r), Ref(RMw.t, RMw.o + 512 + t * 64, RMw.r), [PSK[b], RM_OWN.name], [qrr.name])
                  b2 = nb6()
                  for h in range(8):
                      P.pe(lambda e: e.transpose(psb[b2][0:64, h * 128:(h + 1) * 128], qrr[:, h, :], ident_bf[:, :]), r=[qrr.name, ident_bf.name], w=[PSK[b2]])
                  P.act(lambda e: e.activation(out=qrT[0:64, :, t * 128:(t + 1) * 128], in_=psb[b2][0:64, :].rearrange("p (h n) -> p h n", h=8), func=AF.Copy), r=[PSK[b2]], w=[(qrT.name, t // 4)])
                  A.free(qrr)
              A.free(wqr, RM_OWN)
              wqn = A.alloc("wqn", [128, 4, 8, 128], BF16)
              for lc in range(4):
                  P.dma("pool", wqn[:, lc, :, :], bc(w_uq, lc * 128 * 1536, [[1536, 128], [192, 8], [1, 128]]), w=[wqn.name])
              wkv = A.alloc("wkv", [128, 2, 2048], BF16)
              P.dma("pool", wkv[:, :, :], bc(w_ukv, 0, [[2048, 128], [128 * 2048, 2], [1, 2048]]), w=[wkv.name])
              late = []

              def fm_block_thunks(j_src, jd, plus_one):
                  for q in range(4):
                      def th(q=q):
                          buf = load_wada(j_src * 4 + q)
                          def run():
                              bb = next_bank(0, 4)
                              for cc in range(4):
                                  for kc in range(16):
                                      P.pe(lambda e: e.matmul(ps[bb][:, cc * 2:cc * 2 + 2], lhsT=buf[:, kc, cc * 128:(cc + 1) * 128], rhs=sT_bf[:, kc, :], start=(kc == 0), stop=(kc == 15)), r=[buf.name, sT_bf.name], w=[PSK[bb]])
                              dst = modsT[:, jd, q * 4:(q + 1) * 4]
                              src = bc(ps[bb], 0, [[512, 128], [2, 4]])
                              bsrc = badaT_sb[:, j_src * 16 + q * 4: j_src * 16 + q * 4 + 4]
                              P.dve(lambda e: e.scalar_tensor_tensor(out=dst, in0=src, scalar=(1.0 if plus_one else 0.0), in1=bsrc, op0=ALU.add, op1=ALU.add), r=[PSK[bb], badaT_sb.name], w=[modsT.name])
                              A.free(buf)
                          return run
                      late.append(th)

              Gt = {}

              def gate_block_thunks(j_src, name):
                  def alloc_g():
                      G = A.alloc(name, [128, 2048], F32)
                      P.dma("sp", G[:, :], bc(b_ada, j_src * 2048, [[0, 128], [1, 2048]]), w=[G.name])
                      Gt[name] = G
                  for q in range(4):
                      def th(q=q):
                          if name not in Gt:
                              alloc_g()
                          G = Gt[name]
                          buf = load_wada(j_src * 4 + q)
                          def run():
                              bb = next_bank(0, 4)
                              for kc in range(16):
                                  P.pe(lambda e: e.matmul(ps[bb][:, :], lhsT=s_bc[:, kc, :], rhs=buf[:, kc, :], start=(kc == 0), stop=(kc == 15)), r=[s_bc.name, buf.name], w=[PSK[bb]])
                              P.dve(lambda e: e.tensor_tensor(out=G[:, q * 512:(q + 1) * 512], in0=ps[bb][:, :], in1=G[:, q * 512:(q + 1) * 512], op=ALU.add), r=[PSK[bb], G.name], w=[G.name])
                              A.free(buf)
                          return run
                      late.append(th)

              fm_block_thunks(3, 2, False)
              fm_block_thunks(4, 3, True)
              gate_block_thunks(2, "G1")
              gate_block_thunks(5, "G2")
              pending_runs = []

              def late_step(n):
                  runs = list(pending_runs)
                  del pending_runs[:]
                  for _ in range(n):
                      if late:
                          pending_runs.append(late.pop(0)())
                  for r_ in runs:
                      r_()

              PTs = [A.alloc("PTt", [128, 512], BF16) for _ in range(4)]
              ptn = [0]
              kblocks = [(0, 512), (512, 512), (1024, 512), (1536, 512), (2048, 256)]
              hq = 0
              for h in range(8):
                  late_step(1)
                  qnT_h = A.alloc("qnT_h", [128, 1024], BF16)
                  for qb in range(2):
                      b = nb4()
                      for lc in range(4):
                          P.pe(lambda e: e.matmul(ps[b][:, :], lhsT=wqn[:, lc, h, :], rhs=cqT[:, lc, qb * 512:(qb + 1) * 512], start=(lc == 0), stop=(lc == 3)), r=[wqn.name, (cqT.name, qb)], w=[PSK[b]])
                      P.dve(lambda e: e.tensor_copy(out=qnT_h[:, qb * 512:(qb + 1) * 512], in_=ps[b][:, :]), r=[PSK[b]], w=[(qnT_h.name, qb)])
                  knT_h = A.alloc("knT_h", [128, 2304], BF16)
                  for (k0, n) in kblocks:
                      b = nb4()
                      for lc in range(2):
                          P.pe(lambda e: e.matmul(ps[b][:, 0:n], lhsT=wkv[:, lc, h * 256:h * 256 + 128], rhs=ckvT[:, lc, k0:k0 + n], start=(lc == 0), stop=(lc == 1)), r=[wkv.name] + CK(k0, k0 + n), w=[PSK[b]])
                      P.act(lambda e: e.activation(out=knT_h[:, k0:k0 + n], in_=ps[b][:, 0:n], func=AF.Copy), r=[PSK[b]], w=[(knT_h.name, k0 // 512)])
                  v_h = A.alloc("v_h", [128, 18, 128], BF16)
                  for g in range(5):
                      ntl = 4 if g < 4 else 2
                      b = nb4()
                      for j in range(ntl):
                          kt = g * 4 + j
                          for lc in range(2):
                              P.pe(lambda e: e.matmul(ps[b][:, j * 128:(j + 1) * 128], lhsT=ckvT[:, lc, kt * 128:(kt + 1) * 128], rhs=wkv[:, lc, h * 256 + 128:h * 256 + 256], start=(lc == 0), stop=(lc == 1)), r=[wkv.name, (ckvT.name, kt)], w=[PSK[b]])
                      P.dve(lambda e: e.tensor_copy(out=v_h[:, g * 4:g * 4 + ntl, :], in_=ps[b][:, 0:ntl * 128].rearrange("p (j n) -> p j n", j=ntl)), r=[PSK[b]], w=[(v_h.name, g)])
                  for qb in range(2):
                      if qb == 1:
                          late_step(1)
                      bO, bS = (4, 5) if hq % 2 == 0 else (6, 7)
                      hq += 1
                      def qk(kt):
                          b = nb4()
                          P.pe(lambda e: e.matmul(ps[b][:, :], lhsT=knT_h[:, kt * 128:(kt + 1) * 128], rhs=qnT_h[:, qb * 512:(qb + 1) * 512], start=True, stop=False), r=[(knT_h.name, kt // 4), (qnT_h.name, qb)], w=[PSK[b]])
                          P.pe(lambda e: e.matmul(ps[b][:, :], lhsT=kpeT[0:64, kt * 128:(kt + 1) * 128], rhs=qrT[0:64, h, qb * 512:(qb + 1) * 512], start=False, stop=True), r=[(kpeT.name, kt), (qrT.name, qb)], w=[PSK[b]])
                          return b
                      banks = {0: qk(0), 1: qk(1)}
                      for kt in range(18):
                          if kt + 2 < 18:
                              banks[kt + 2] = qk(kt + 2)
                          b = banks.pop(kt)
                          PTt = PTs[ptn[0] % 4]
                          ptn[0] += 1
                          P.act(lambda e: e.activation(out=PTt[:, :], in_=ps[b][:, :], func=AF.Exp, scale=ATT_SCALE), r=[PSK[b]], w=[PTt.name])
                          P.pe(lambda e: e.matmul(ps[bO][:, :], lhsT=v_h[:, kt, :], rhs=PTt[:, :], start=(kt == 0), stop=(kt == 17)), r=[(v_h.name, kt // 4), PTt.name], w=[PSK[bO]])
                          P.pe(lambda e: e.matmul(ps[bS][:, :], lhsT=ones_bf[:, :], rhs=PTt[:, :], start=(kt == 0), stop=(kt == 17)), r=[ones_bf.name, PTt.name], w=[PSK[bS]])
                      rec = A.alloc("rec", [128, 512], F32)
                      P.dve(lambda e: e.reciprocal(out=rec[:, :], in_=ps[bS][:, :]), r=[PSK[bS]], w=[rec.name])
                      P.dve(lambda e: e.tensor_tensor(out=mixT[:, 8 + h, qb * 512:(qb + 1) * 512], in0=ps[bO][:, :], in1=rec[:, :], op=ALU.mult), r=[PSK[bO], rec.name], w=[(mixT.name, 8 + h)])
                      A.free(rec)
                  A.free(qnT_h, knT_h, v_h)
              late_step(0)
              assert not late and not pending_runs
              A.free(wqn, wkv, cqT, qrT, ckvT, kpeT, *PTs)
              G1, G2 = Gt["G1"], Gt["G2"]
              if "att" in tap_out:
                  tap("att", mixT[:, 8:16, 0:256], MK(8, 16), [128, 8, 256])

              P.marks.append(("mods2", sum(1 for x_ in P.ins if x_.eng == "pe")))
              def bcast_row(dram, name):
                  T_ = A.alloc(name, [128, 2048], F32)
                  P.dma("sp", T_[:, :], bc(dram, 0, [[0, 128], [1, 2048]]), w=[T_.name])
                  return T_

              P.marks.append(("F", sum(1 for x_ in P.ins if x_.eng == "pe")))
              acc = A.alloc("acc", [128, 8, 2048], F32)
              wos = [A.alloc("wo", [128, 16, 512], BF16) for _ in range(2)]
              P.dma("pool", wos[0][:, :, :], bc(w_o, 0, [[D, 128], [128 * D, 16], [1, 512]]), w=[wos[0].name])
              def load_xr(i):
                  cb_, t_ = i // 8, i % 8
                  xr_ = A.alloc("xr", [128, 512], F32)
                  P.dma("sp", xr_[:, :], x_own.ap()[t_ * 128:(t_ + 1) * 128, cb_ * 512:(cb_ + 1) * 512], w=[xr_.name])
                  return xr_
              xq = [load_xr(i) for i in range(3)]
              tmp_prev = [None]
              for cb in range(4):
                  wo = wos[cb % 2]
                  if cb < 3:
                      wn = wos[(cb + 1) % 2]
                      P.dma("pool", wn[:, :, :], bc(w_o, (cb + 1) * 512, [[D, 128], [128 * D, 16], [1, 512]]), w=[wn.name])
                  for t in range(8):
                      b = next_bank()
                      for kc in range(16):
                          P.pe(lambda e: e.matmul(ps[b][:, :], lhsT=mixT[:, kc, t * 128:(t + 1) * 128], rhs=wo[:, kc, :], start=(kc == 0), stop=(kc == 15)), r=[(mixT.name, kc), wo.name], w=[PSK[b]])
                      xr = xq.pop(0)
                      nxt_i = cb * 8 + t + 3
                      if nxt_i < 32:
                          xq.append(load_xr(nxt_i))
                      tmp = A.alloc("tmpo", [128, 512], F32)
                      if tmp_prev[0] is not None:
                          A.free(tmp_prev[0])
                      tmp_prev[0] = tmp
                      P.dve(lambda e: e.tensor_tensor(out=tmp[:, :], in0=ps[b][:, :], in1=G1[:, cb * 512:(cb + 1) * 512], op=ALU.mult), r=[PSK[b], G1.name], w=[tmp.name])
                      P.dve(lambda e: e.scalar_tensor_tensor(out=acc[:, t, cb * 512:(cb + 1) * 512], in0=xr[:, :], scalar=ALPHA, in1=tmp[:, :], op0=ALU.mult, op1=ALU.add), r=[xr.name, tmp.name], w=[(acc.name, t)])
                      A.free(xr)
              A.free(mixT, G1, tmp_prev[0], *wos)
              if "pre1" in tap_out:
                  tap("pre1", acc[:, 0, :], [(acc.name, 0)], [128, 2048])
              L1W = bcast_row(ln1_w, "L1W")
              L1B = bcast_row(ln1_b, "L1B")
              h2T = A.alloc("h2T", [128, 16, 1024], BF16)
              hn_old = []
              for blk in range(2):
                  hn2 = A.alloc("hn2", [128, 4, 2048], BF16)
                  for pr in range(2):
                      tl = [blk * 4 + pr * 2, blk * 4 + pr * 2 + 1]
                      hnl = []
                      for t in tl:
                          hn = A.alloc("hn", [128, 2048], F32)
                          hnl.append(hn)
                      for ohn in hn_old:
                          A.free(ohn)
                      hn_old = list(hnl)
                      LSa, st1 = ln_stats_multi([(acc[:, t, :], [(acc.name, t)]) for t in tl])
                      for t, hn, st_ in zip(tl, hnl, st1):
                          ln_apply(st_, acc[:, t, :], [(acc.name, t)], hn[:, :], [hn.name])
                          P.dve(lambda e: e.tensor_tensor(out=hn[:, :], in0=hn[:, :], in1=L1W[:, :], op=ALU.mult), r=[hn.name, L1W.name], w=[hn.name])
                          P.add("pool" if t % 2 == 0 else "dve", lambda e: e.tensor_tensor(out=hn[:, :], in0=hn[:, :], in1=L1B[:, :], op=ALU.add), [hn.name, L1B.name], [hn.name])
                      LSb, st2 = ln_stats_multi([(hn[:, :], [hn.name]) for hn in hnl])
                      for t, hn, st_ in zip(tl, hnl, st2):
                          j = t % 4
                          ln_apply(st_, hn[:, :], [hn.name], hn2[:, j, :], [hn2.name])
                          P.act(lambda e: e.activation(out=acc[:, t, :], in_=hn[:, :], func=AF.Copy, scale=ALPHA), r=[hn.name], w=[(acc.name, t)])
                      A.free(LSa, LSb)
                  transpose_block(hn2, 4, h2T, blk * 512, 3, 2, blk)
                  A.free(hn2)
              A.free(L1W, L1B, *hn_old)
              H2K = [(h2T.name, kc) for kc in range(16)]
              if "h2T" in tap_out:
                  tap("h2T", h2T[:, :, 0:256], H2K, [128, 16, 256])

              P.marks.append(("G", sum(1 for x_ in P.ins if x_.eng == "pe")))
              wr = A.alloc("wr", [128, 16, 36], BF16)
              P.dma("pool", wr[:, :, :], bc(w_rt, 0, [[36, 128], [128 * 36, 16], [1, 36]]), w=[wr.name])
              brt = A.alloc("brt", [128, 36], F32)
              P.dma("sp", brt[:, :], bc(b_rt, 0, [[0, 128], [1, 36]]), w=[brt.name])
              LGT = A.alloc("LGT", [128, 8, 36], F32)
              for t in range(8):
                  b = next_bank()
                  for kc in range(16):
                      P.pe(lambda e: e.matmul(ps[b][:, 0:36], lhsT=h2T[:, kc, t * 128:(t + 1) * 128], rhs=wr[:, kc, :], start=(kc == 0), stop=(kc == 15)), r=[(h2T.name, kc), wr.name], w=[PSK[b]])
                  P.dve(lambda e: e.tensor_tensor(out=LGT[:, t, :], in0=ps[b][:, 0:36], in1=brt[:, :], op=ALU.add), r=[PSK[b], brt.name], w=[LGT.name])
              RT = A.alloc("RT", [128, 1024], F32)
              RTr = tref(RT)
              LGr = tref(LGT)
              def rv(off, dims):
                  return RTr.v(off, dims)
              o_gmax, o_ohg, o_eg, o_gsum, o_gprob, o_tmpe, o_esel, o_m1, o_oh1, o_e2, o_m2, o_oh2, o_dd, o_w1, o_w2, o_t1, o_t2, o_ew = (
                  0, 8, 40, 72, 80, 88, 344, 408, 416, 480, 544, 552, 616, 624, 632, 640, 704, 768)
              GATE = A.alloc("GATE", [128, 8, 32], F32)
              GL = LGr.v(0, [[36, 8], [1, 4]])
              EL = LGr.v(4, [[36, 8], [8, 4], [1, 8]])
              RK = [RT.name]
              def rdve(fn, extra_r=()):
                  P.dve(fn, r=RK + list(extra_r), w=RK)
              rdve(lambda e: e.tensor_reduce(out=rv(o_gmax, [[1, 8]]), in_=GL, axis=AX.X, op=ALU.max), [LGT.name])
              rdve(lambda e: e.tensor_tensor(out=rv(o_ohg, [[4, 8], [1, 4]]), in0=GL, in1=rv(o_gmax, [[1, 8], [0, 4]]), op=ALU.is_equal), [LGT.name])
              rdve(lambda e: e.tensor_tensor(out=rv(o_eg, [[4, 8], [1, 4]]), in0=GL, in1=rv(o_gmax, [[1, 8], [0, 4]]), op=ALU.subtract), [LGT.name])
              P.act(lambda e: e.activation(out=rv(o_eg, [[1, 32]]), in_=rv(o_eg, [[1, 32]]), func=AF.Exp), r=RK, w=RK)
              rdve(lambda e: e.tensor_reduce(out=rv(o_gsum, [[1, 8]]), in_=rv(o_eg, [[4, 8], [1, 4]]), axis=AX.X, op=ALU.add))
              rdve(lambda e: e.reciprocal(out=rv(o_gprob, [[1, 8]]), in_=rv(o_gsum, [[1, 8]])))
              rdve(lambda e: e.tensor_tensor(out=rv(o_tmpe, [[32, 8], [8, 4], [1, 8]]), in0=EL, in1=rv(o_ohg, [[4, 8], [1, 4], [0, 8]]), op=ALU.mult), [LGT.name])
              rdve(lambda e: e.tensor_reduce(out=rv(o_esel, [[8, 8], [1, 8]]), in_=rv(o_tmpe, [[32, 8], [1, 8], [8, 4]]), axis=AX.X, op=ALU.add))
              rdve(lambda e: e.tensor_reduce(out=rv(o_m1, [[1, 8]]), in_=rv(o_esel, [[8, 8], [1, 8]]), axis=AX.X, op=ALU.max))
              rdve(lambda e: e.tensor_tensor(out=rv(o_oh1, [[8, 8], [1, 8]]), in0=rv(o_esel, [[8, 8], [1, 8]]), in1=rv(o_m1, [[1, 8], [0, 8]]), op=ALU.is_equal))
              rdve(lambda e: e.scalar_tensor_tensor(out=rv(o_e2, [[1, 64]]), in0=rv(o_oh1, [[1, 64]]), scalar=-1.0e30, in1=rv(o_esel, [[1, 64]]), op0=ALU.mult, op1=ALU.add))
              rdve(lambda e: e.tensor_reduce(out=rv(o_m2, [[1, 8]]), in_=rv(o_e2, [[8, 8], [1, 8]]), axis=AX.X, op=ALU.max))
              rdve(lambda e: e.tensor_tensor(out=rv(o_oh2, [[8, 8], [1, 8]]), in0=rv(o_e2, [[8, 8], [1, 8]]), in1=rv(o_m2, [[1, 8], [0, 8]]), op=ALU.is_equal))
              rdve(lambda e: e.tensor_tensor(out=rv(o_dd, [[1, 8]]), in0=rv(o_m2, [[1, 8]]), in1=rv(o_m1, [[1, 8]]), op=ALU.subtract))
              P.act(lambda e: e.activation(out=rv(o_dd, [[1, 8]]), in_=rv(o_dd, [[1, 8]]), func=AF.Exp), r=RK, w=RK)
              rdve(lambda e: e.tensor_scalar(out=rv(o_w1, [[1, 8]]), in0=rv(o_dd, [[1, 8]]), scalar1=1.0, scalar2=None, op0=ALU.add))
              rdve(lambda e: e.reciprocal(out=rv(o_w1, [[1, 8]]), in_=rv(o_w1, [[1, 8]])))
              rdve(lambda e: e.tensor_tensor(out=rv(o_w2, [[1, 8]]), in0=rv(o_dd, [[1, 8]]), in1=rv(o_w1, [[1, 8]]), op=ALU.mult))
              rdve(lambda e: e.tensor_tensor(out=rv(o_w1, [[1, 8]]), in0=rv(o_w1, [[1, 8]]), in1=rv(o_gprob, [[1, 8]]), op=ALU.mult))
              rdve(lambda e: e.tensor_tensor(out=rv(o_w2, [[1, 8]]), in0=rv(o_w2, [[1, 8]]), in1=rv(o_gprob, [[1, 8]]), op=ALU.mult))
              rdve(lambda e: e.tensor_tensor(out=rv(o_t1, [[8, 8], [1, 8]]), in0=rv(o_oh1, [[8, 8], [1, 8]]), in1=rv(o_w1, [[1, 8], [0, 8]]), op=ALU.mult))
              rdve(lambda e: e.tensor_tensor(out=rv(o_t2, [[8, 8], [1, 8]]), in0=rv(o_oh2, [[8, 8], [1, 8]]), in1=rv(o_w2, [[1, 8], [0, 8]]), op=ALU.mult))
              rdve(lambda e: e.tensor_tensor(out=rv(o_ew, [[1, 64]]), in0=rv(o_t1, [[1, 64]]), in1=rv(o_t2, [[1, 64]]), op=ALU.add))
              P.dve(lambda e: e.tensor_tensor(out=tref(GATE).v(0, [[32, 8], [8, 4], [1, 8]]), in0=rv(o_ohg, [[4, 8], [1, 4], [0, 8]]), in1=rv(o_ew, [[8, 8], [0, 4], [1, 8]]), op=ALU.mult), r=RK, w=[GATE.name])
              A.free(wr, brt, LGT, RT)
              if "gate" in tap_out:
                  tap("gate", GATE[:, :, :], [GATE.name], [128, 8, 32])

              P.marks.append(("H", sum(1 for x_ in P.ins if x_.eng == "pe")))
              NEXP = int(os.environ.get("DBG_NEXP", "32")) if n_exp else 0
              items = []
              for ge in range(NEXP):
                  for fh in range(2):
                      items.append(("gu", ge, fh))
                  items.append(("wd", ge, 0))
              loaded = {}

              def load_item(i):
                  kind, ge, fh = items[i]
                  if kind == "gu":
                      wg = A.alloc("wg", [128, 16, 256], BF16)
                      wu = A.alloc("wu", [128, 16, 256], BF16)
                      P.dma("pool", wg[:, :, :], bc(ew_gate, ge * D * 512 + fh * 256, [[512, 128], [128 * 512, 16], [1, 256]]), w=[wg.name])
                      P.dma("pool", wu[:, :, :], bc(ew_up, ge * D * 512 + fh * 256, [[512, 128], [128 * 512, 16], [1, 256]]), w=[wu.name])
                      loaded[i] = (wg, wu)
                  else:
                      wd = A.alloc("wd", [128, 4, 2048], BF16)
                      P.dma("pool", wd[:, :, :], bc(ew_down, ge * 512 * D, [[D, 128], [128 * D, 4], [1, 2048]]), w=[wd.name])
                      P.pool(lambda e: e.tensor_tensor(out=wd[:, :, :], in0=wd[:, :, :], in1=tref(G2).v(0, [[0, 4], [1, 2048]]), op=ALU.mult), r=[wd.name, G2.name], w=[wd.name])
                      loaded[i] = (wd,)

              def get_item(i):
                  if i not in loaded:
                      load_item(i)
                  if i + 1 < len(items) and (i + 1) not in loaded:
                      load_item(i + 1)
                  return loaded[i]

              ii = 0
              for ge in range(NEXP):
                  hid = A.alloc("hid", [128, 4, 1024], BF16)
                  for fh in range(2):
                      wg, wu = get_item(ii)
                      for fq in range(2):
                          fc = fh * 2 + fq
                          for tb in range(2):
                              bg = next_bank()
                              for kc in range(16):
                                  P.pe(lambda e: e.matmul(ps[bg][:, :], lhsT=wg[:, kc, fq * 128:(fq + 1) * 128], rhs=h2T[:, kc, tb * 512:(tb + 1) * 512], start=(kc == 0), stop=(kc == 15)), r=[wg.name, (h2T.name, kc)], w=[PSK[bg]])
                              bu = next_bank()
                              for kc in range(16):
                                  P.pe(lambda e: e.matmul(ps[bu][:, :], lhsT=wu[:, kc, fq * 128:(fq + 1) * 128], rhs=h2T[:, kc, tb * 512:(tb + 1) * 512], start=(kc == 0), stop=(kc == 15)), r=[wu.name, (h2T.name, kc)], w=[PSK[bu]])
                              sgt = A.alloc("sgt", [128, 512], F32)
                              P.act(lambda e: e.activation(out=sgt[:, :], in_=ps[bg][:, :], func=AF.Silu), r=[PSK[bg]], w=[sgt.name])
                              P.dve(lambda e: e.tensor_tensor(out=hid[:, fc, tb * 512:(tb + 1) * 512], in0=ps[bu][:, :], in1=sgt[:, :], op=ALU.mult), r=[PSK[bu], sgt.name], w=[(hid.name, fc)])
                              A.free(sgt)
                      A.free(wg, wu)
                      del loaded[ii]
                      ii += 1
                  (wd,) = get_item(ii)
                  for t in range(8):
                      for nbk in range(4):
                          b = next_bank()
                          for fc in range(4):
                              P.pe(lambda e: e.matmul(ps[b][:, :], lhsT=hid[:, fc, t * 128:(t + 1) * 128], rhs=wd[:, fc, nbk * 512:(nbk + 1) * 512], start=(fc == 0), stop=(fc == 3)), r=[(hid.name, fc), wd.name], w=[PSK[b]])
                          P.dve(lambda e: e.scalar_tensor_tensor(out=acc[:, t, nbk * 512:(nbk + 1) * 512], in0=ps[b][:, :], scalar=GATE[:, t, ge:ge + 1], in1=acc[:, t, nbk * 512:(nbk + 1) * 512], op0=ALU.mult, op1=ALU.add),
                                r=[PSK[b], GATE.name, (acc.name, t)], w=[(acc.name, t)])
                  A.free(wd, hid)
                  del loaded[ii]
                  ii += 1
              A.free(h2T, G2)

              P.marks.append(("LN2", sum(1 for x_ in P.ins if x_.eng == "pe")))
              L2W = bcast_row(ln2_w, "L2W")
              L2B = bcast_row(ln2_b, "L2B")
              hns = [A.alloc("hn", [128, 2048], F32) for _ in range(3)]
              LS2, sts = ln_stats_multi([(acc[:, t, :], [(acc.name, t)]) for t in range(8)])
              for t in range(8):
                  hn = hns[t % 3]
                  ln_apply(sts[t], acc[:, t, :], [(acc.name, t)], hn[:, :], [hn.name])
                  P.dve(lambda e: e.tensor_tensor(out=hn[:, :], in0=hn[:, :], in1=L2W[:, :], op=ALU.mult), r=[hn.name, L2W.name], w=[hn.name])
                  P.add("pool" if t % 2 == 0 else "dve", lambda e: e.tensor_tensor(out=hn[:, :], in0=hn[:, :], in1=L2B[:, :], op=ALU.add), [hn.name, L2B.name], [hn.name])
                  P.dma("sp", out.ap()[t * 128:(t + 1) * 128, :], hn[:, :], r=[hn.name], w=[("out", t)])
                  out_keys.append(("out", t))

        P.add("sp", None, reads=out_keys)
        P.finalize(sems, dsems, {"act": FEN[:, 0:1], "dve": FEN[:, 1:2], "pool": FEN[:, 2:3]} if not os.environ.get("DBG_NOFENCE2") else None)
        with nc.Block() as block0:
            @block0.sync
            def _(e):
                for s_ in list(sems.values()) + [x_ for l_ in dsems.values() for x_ in l_]:
                    e.sem_clear(s_)

        with nc.Block() as block:
            @block.tensor
            def _(e):
                P.emit("pe", e)

            @block.scalar
            def _(e):
                P.emit("act", e)

            @block.vector
            def _(e):
                P.emit("dve", e)

            @block.gpsimd
            def _(e):
                P.emit("pool", e)

            @block.sync
            def _(e):
                P.emit("sp", e)
    return nc


def _rope_tables(pos, dim):
    half = dim // 2
    inv = 10000.0 ** (-np.arange(0, half, 2, dtype=np.float32) / np.float32(half))
    inv = inv.astype(np.float32)
    rows = (pos // GRID_W).astype(np.float32)
    cols = (pos % GRID_W).astype(np.float32)
    ar = (rows[:, None] * inv[None, :]).astype(np.float32)
    ac = (cols[:, None] * inv[None, :]).astype(np.float32)
    cr, sr, cc, sc = np.cos(ar), np.sin(ar), np.cos(ac), np.sin(ac)
    cosx = np.concatenate([cr, cr, cc, cc], 1).astype(np.float32)
    sinx = np.concatenate([-sr, sr, -sc, sc], 1).astype(np.float32)
    return cosx, sinx


_PERM64 = np.concatenate([np.arange(0, 16), np.arange(32, 48), np.arange(16, 32), np.arange(48, 64)])


def _tile_major(a):
    n = a.shape[0] // 128
    return np.ascontiguousarray(a.reshape(n, 128, a.shape[1]).transpose(1, 0, 2).reshape(128, -1))


def _core_tables(hf):
    own = np.arange(NTOK) + hf * NTOK
    oth = np.arange(NTOK) + (1 - hf) * NTOK
    t = {}
    for nm, pos in (("own", own), ("oth", oth)):
        c, s = _rope_tables(pos, 128)
        t["rope_r_" + nm] = np.concatenate([_tile_major(c), _tile_major(s)], 1)
        c, s = _rope_tables(pos, 64)
        c, s = c[:, _PERM64], s[:, _PERM64]
        t["rope_m_" + nm] = np.concatenate([_tile_major(c), _tile_major(s)], 1)
    tb = np.zeros((128, NTAB), np.float32)
    sidx = np.arange(128, dtype=np.float32)[:, None]
    cidx = np.arange(128, dtype=np.float32)[None, :]
    tb[:, T_D1:T_D1 + 128] = np.maximum(cidx - sidx, 0)
    tb[:, T_MK1:T_MK1 + 128] = (cidx >= sidx)
    tb[:, T_D2:T_D2 + 128] = np.maximum(sidx - cidx, 0)
    tb[:, T_MK2:T_MK2 + 128] = (sidx >= cidx)
    tb[:, T_CP1:T_CP1 + 128] = cidx + 1
    tb[:, T_C128:T_C128 + 128] = 128 - cidx
    tb[:, T_Z1] = 127 - sidx[:, 0]
    tb[:, T_Z2] = sidx[:, 0]
    j = np.arange(NTOK, dtype=np.float32)
    m = np.arange(CTX, dtype=np.float32)
    if hf == 0:
        ef_o, mf_o = np.zeros(NTOK), np.zeros(NTOK)
        ef_c, mf_c = CTX - 1 - m, np.ones(CTX)
        eb_o, mb_o = j, np.ones(NTOK)
        eb_c, mb_c = NTOK + m, np.ones(CTX)
    else:
        ef_o, mf_o = NTOK - 1 - j, np.ones(NTOK)
        ef_c, mf_c = NTOK + CTX - 1 - m, np.ones(CTX)
        eb_o, mb_o = np.zeros(NTOK), np.zeros(NTOK)
        eb_c, mb_c = m, np.ones(CTX)
    def tm(o, c):
        return np.concatenate([o.reshape(8, 128).T, c.reshape(2, 128).T], 1)
    tb[:, T_EF:T_EF + 10] = tm(ef_o, ef_c)
    tb[:, T_MF:T_MF + 10] = tm(mf_o, mf_c)
    tb[:, T_EB:T_EB + 10] = tm(eb_o, eb_c)
    tb[:, T_MB:T_MB + 10] = tm(mb_o, mb_c)
    t["tabs"] = tb
    return t


def _colT(v, n):
    return np.ascontiguousarray(np.asarray(v, np.float32).reshape(n, 128).T)


def prep(inputs, cores=None, n_exp=32):
    f = lambda k: np.asarray(inputs[k], np.float32)
    x, c, ctx, c_ctx = f("x"), f("c"), f("ctx"), f("c_ctx")
    shared = {
        "w_ada": np.ascontiguousarray(f("w_ada")[0]),
        "b_ada": np.ascontiguousarray(f("b_ada")[0][None]),
        "badaT": _colT(f("b_ada")[0], 96),
        "w_in": np.ascontiguousarray(f("w_in")[0]),
        "ret_decay": np.ascontiguousarray(f("ret_decay")[0].reshape(1, 16)),
        "ret_gn_w": np.ascontiguousarray(f("ret_gn_w")[0][None]),
        "qnT": _colT(f("mla_q_norm")[0], 4),
        "kvnT": _colT(f("mla_kv_norm")[0], 2),
        "w_uq": np.ascontiguousarray(f("w_uq")[0]),
        "w_ckp": np.ascontiguousarray(np.concatenate([f("w_in")[0][:, 4608:4864], f("w_in")[0][:, 4864:4928][:, _PERM64]], 1)),
        "w_uqr": np.ascontiguousarray(np.concatenate(
            [f("w_uq")[0][:, h * 192 + 128:h * 192 + 192][:, _PERM64] for h in range(8)], 1)),
        "w_ukv": np.ascontiguousarray(f("w_ukv")[0]),
        "w_o": np.ascontiguousarray(f("w_o")[0]),
        "ln1_w": f("ln1_w")[0][None], "ln1_b": f("ln1_b")[0][None],
        "ln2_w": f("ln2_w")[0][None], "ln2_b": f("ln2_b")[0][None],
        "w_rt": np.ascontiguousarray(np.concatenate(
            [f("router_group_w")[0]] + [f("router_expert_w")[0][g] for g in range(4)], 1)),
        "b_rt": np.ascontiguousarray(np.concatenate(
            [f("router_group_b")[0].reshape(-1), f("router_expert_b")[0].reshape(-1)])[None]),
        "ew_gate": f("expert_w_gate")[0].reshape(32, D, 512),
        "ew_up": f("expert_w_up")[0].reshape(32, D, 512),
        "ew_down": f("expert_w_down")[0].reshape(32, 512, D),
        "ident": np.eye(128, dtype=np.float32),
    }
    if not n_exp:
        for k in ("ew_gate", "ew_up", "ew_down"):
            del shared[k]
    tables = [_core_tables(0), _core_tables(1)]
    maps = []
    for core in (range(8) if cores is None else cores):
        b, hf = core // 2, core % 2
        m = dict(shared)
        m["x_own"] = np.ascontiguousarray(x[b, hf * NTOK:(hf + 1) * NTOK])
        m["x_oth"] = np.ascontiguousarray(x[b, (1 - hf) * NTOK:(2 - hf) * NTOK])
        m["ctx"] = np.ascontiguousarray(ctx[b])
        cv = np.stack([c[b], c_ctx], 0)
        m["cT"] = np.ascontiguousarray(cv.reshape(2, 16, 128).transpose(2, 1, 0).reshape(128, 32))
        m.update(tables[hf])
        maps.append(m)
    return maps


_NC_CACHE = {}


def kernel(**inputs):
    maps = prep(inputs)
    if "nc" not in _NC_CACHE:
        _NC_CACHE["nc"] = build()
    res = run_bass_kernel_spmd(_NC_CACHE["nc"], maps, core_ids=list(range(8)))
    outp = np.empty((4, 2048, D), np.float32)
    for core in range(8):
        b, hf = core // 2, core % 2
        outp[b, hf * NTOK:(hf + 1) * NTOK] = res.results[core]["out"]
    return outp
```
